# Optimizing a Trainium2 kernel written in Bass

```python
import jax, jax.numpy as jnp
from jax import lax
import numpy as np


D_MODEL = 4096
BATCH = 4
SEQ = 2048
DEPTH = 1

CHUNK = 64
MIX_WIDTH = D_MODEL
CONV_WIDTH = MIX_WIDTH // 2
CONV_GROUPS = 16
CONV_KERNEL = 31
ATTN_WIDTH = MIX_WIDTH - CONV_WIDTH
SB_HEADS = 16
SB_HEAD_DIM = ATTN_WIDTH // SB_HEADS
Q_BLOCK = 128
IN_COLS = 2 * CONV_WIDTH + 3 * ATTN_WIDTH
N_EXPERTS = 32
TOP_K = 4
EXPERT_FF = 3 * D_MODEL // 8
SWIGLU_LIMIT = 7.0
SWIGLU_ALPHA = 1.702
EXPERT_BLOCK = 128
NORM_EPS = 1e-6
LN_EPS = 1e-5

kernel_name = "hybrid_conv_stickbreak_moe_block"


def rms_norm(x, g):
    xf = x.astype(jnp.float32)
    y = xf * lax.rsqrt(jnp.mean(xf * xf, axis=-1, keepdims=True) + NORM_EPS)
    return (y * g.astype(jnp.float32)).astype(x.dtype)


def layer_norm(x, g, b):
    xf = x.astype(jnp.float32)
    mu = jnp.mean(xf, axis=-1, keepdims=True)
    var = jnp.mean(jnp.square(xf - mu), axis=-1, keepdims=True)
    y = (xf - mu) * lax.rsqrt(var + LN_EPS) * g.astype(jnp.float32) + b.astype(jnp.float32)
    return y.astype(x.dtype)


def modulate(h, shift, scale):
    return h * (1 + scale[:, None, :]) + shift[:, None, :]


def conformer_conv(val, gate, b_glu, conv_w, conv_b, ln_g, ln_b):
    u = (val + b_glu[:CONV_WIDTH]) * jax.nn.sigmoid(gate + b_glu[CONV_WIDTH:])
    u = lax.conv_general_dilated(
        u, conv_w, window_strides=(1,), padding=[(CONV_KERNEL - 1, 0)],
        dimension_numbers=('NWC', 'WIO', 'NWC'), feature_group_count=CONV_WIDTH) + conv_b
    u = layer_norm(u, ln_g, ln_b)
    return jax.nn.silu(u)


def _stick_breaking_block(q_blk, k_pre, v_pre, q_start):
    n_q = q_blk.shape[2]
    n_k = k_pre.shape[2]
    z = jnp.einsum('bhqd,bhkd->bhqk', q_blk, k_pre).astype(jnp.float32) * (SB_HEAD_DIM ** -0.5)
    t = q_start + jnp.arange(n_q, dtype=jnp.int32)[:, None]
    s = jnp.arange(n_k, dtype=jnp.int32)[None, :]
    mask = s < t
    log_fail = jnp.where(mask, jax.nn.log_sigmoid(-z), 0.0)
    suffix = lax.cumsum(log_fail, axis=3, reverse=True) - log_fail
    log_a = jax.nn.log_sigmoid(z) + suffix
    a = jnp.where(mask, jnp.exp(log_a), 0.0)
    return jnp.einsum('bhqk,bhkd->bhqd', a.astype(v_pre.dtype), v_pre)


def stick_breaking_attention(q, k, v):
    b_, s_ = q.shape[:2]
    q = q.transpose(0, 2, 1, 3)
    k = k.transpose(0, 2, 1, 3)
    v = v.transpose(0, 2, 1, 3)
    outs = []
    for start in range(0, s_, Q_BLOCK):
        stop = start + Q_BLOCK
        outs.append(_stick_breaking_block(q[:, :, start:stop], k[:, :, :stop], v[:, :, :stop], start))
    o = jnp.concatenate(outs, axis=2)
    return o.transpose(0, 2, 1, 3).reshape(b_, s_, ATTN_WIDTH)


def moe_ffn(h, w_router, b_router, w_gate_up, b_gate_up, w_down, b_down):
    b_, s_, d_ = h.shape
    n_tok = b_ * s_
    hf = h.reshape(n_tok, d_)
    logits = (hf @ w_router + b_router).astype(jnp.float32)
    top_val, top_idx = lax.top_k(logits, TOP_K)
    gates = jax.nn.softmax(top_val, axis=-1)
    n_assign = n_tok * TOP_K
    flat_e = top_idx.reshape(n_assign).astype(jnp.int32)
    flat_tok = jnp.repeat(jnp.arange(n_tok, dtype=jnp.int32), TOP_K)
    flat_g = gates.reshape(n_assign)
    order = jnp.argsort(flat_e, stable=True)
    sorted_e = flat_e[order]
    sorted_tok = flat_tok[order]
    sorted_g = flat_g[order]
    counts = jnp.bincount(flat_e, length=N_EXPERTS).astype(jnp.int32)
    padded = (counts + EXPERT_BLOCK - 1) // EXPERT_BLOCK * EXPERT_BLOCK
    pad_end = jnp.cumsum(padded)
    pad_start = pad_end - padded
    grp_start = jnp.cumsum(counts) - counts
    dest = pad_start[sorted_e] + jnp.arange(n_assign, dtype=jnp.int32) - grp_start[sorted_e]
    n_blocks = -(-n_assign // EXPERT_BLOCK) + N_EXPERTS
    rows = jnp.zeros((n_blocks * EXPERT_BLOCK, d_), h.dtype).at[dest].set(hf[sorted_tok])
    block_e = jnp.minimum(
        jnp.searchsorted(pad_end, jnp.arange(n_blocks, dtype=jnp.int32) * EXPERT_BLOCK, side='right'),
        N_EXPERTS - 1).astype(jnp.int32)

    def expert_block(args):
        xb, e = args
        gu = xb @ w_gate_up[e] + b_gate_up[e]
        g_, u_ = gu[:, :EXPERT_FF], gu[:, EXPERT_FF:]
        g_ = jnp.minimum(g_, SWIGLU_LIMIT)
        u_ = jnp.clip(u_, -SWIGLU_LIMIT, SWIGLU_LIMIT)
        act = (u_ + 1) * (g_ * jax.nn.sigmoid(SWIGLU_ALPHA * g_))
        return act @ w_down[e] + b_down[e]

    out_rows = lax.map(expert_block, (rows.reshape(n_blocks, EXPERT_BLOCK, d_), block_e))
    out_rows = out_rows.reshape(n_blocks * EXPERT_BLOCK, d_)
    y = out_rows[dest] * sorted_g[:, None].astype(h.dtype)
    out = jax.ops.segment_sum(y, sorted_tok, num_segments=n_tok)
    return out.reshape(b_, s_, d_)


def setup_inputs(seed: int = 0) -> dict:
    key = jax.random.key(seed)
    ks = jax.random.split(key, 24)
    f32 = jnp.float32
    L, D, E, F = DEPTH, D_MODEL, N_EXPERTS, EXPERT_FF

    def nrm(k, shape, scale):
        return jax.random.normal(k, shape, f32) * scale

    return {
        'x': nrm(ks[0], (BATCH, SEQ, D), 1.0),
        'c': nrm(ks[1], (BATCH, D), 1.0),
        'w_ada': nrm(ks[2], (L, D, 6 * D), 0.5 * D ** -0.5),
        'b_ada': nrm(ks[3], (L, 6 * D), 0.01),
        'g_pre_mix': 1.0 + nrm(ks[4], (L, D), 0.01),
        'g_post_mix': 1.0 + nrm(ks[5], (L, D), 0.01),
        'w_in': nrm(ks[6], (L, D, IN_COLS), D ** -0.5),
        'b_glu': nrm(ks[7], (L, 2 * CONV_WIDTH), 0.01),
        'conv_w': nrm(ks[8], (L, CONV_KERNEL, 1, CONV_WIDTH), CONV_KERNEL ** -0.5),
        'conv_b': nrm(ks[9], (L, CONV_WIDTH), 0.01),
        'conv_ln_g': 1.0 + nrm(ks[10], (L, CONV_WIDTH), 0.01),
        'conv_ln_b': nrm(ks[11], (L, CONV_WIDTH), 0.01),
        'w_out': nrm(ks[12], (L, MIX_WIDTH, D), MIX_WIDTH ** -0.5),
        'g_pre_ffn': 1.0 + nrm(ks[13], (L, D), 0.01),
        'g_post_ffn': 1.0 + nrm(ks[14], (L, D), 0.01),
        'w_router': nrm(ks[15], (L, D, E), D ** -0.5),
        'b_router': nrm(ks[16], (L, E), 0.01),
        'w_gate_up': nrm(ks[17], (L, E, D, 2 * F), D ** -0.5),
        'b_gate_up': nrm(ks[18], (L, E, 2 * F), 0.01),
        'w_down': nrm(ks[19], (L, E, F, D), F ** -0.5),
        'b_down': nrm(ks[20], (L, E, D), 0.01),
    }


def reference(x, c, w_ada, b_ada, g_pre_mix, g_post_mix, w_in, b_glu, conv_w, conv_b,
              conv_ln_g, conv_ln_b, w_out, g_pre_ffn, g_post_ffn, w_router, b_router,
              w_gate_up, b_gate_up, w_down, b_down):
    b_, s_, _ = x.shape
    for l in range(DEPTH):
        mod = jax.nn.silu(c) @ w_ada[l] + b_ada[l]
        shift1, scale1, gate1, shift2, scale2, gate2 = jnp.split(mod, 6, axis=-1)

        h = modulate(rms_norm(x, g_pre_mix[l]), shift1, scale1)
        proj = h @ w_in[l]
        c0 = 2 * CONV_WIDTH
        val = proj[..., :CONV_WIDTH]
        gte = proj[..., CONV_WIDTH:c0]
        q = proj[..., c0:c0 + ATTN_WIDTH].reshape(b_, s_, SB_HEADS, SB_HEAD_DIM)
        k = proj[..., c0 + ATTN_WIDTH:c0 + 2 * ATTN_WIDTH].reshape(b_, s_, SB_HEADS, SB_HEAD_DIM)
        v = proj[..., c0 + 2 * ATTN_WIDTH:].reshape(b_, s_, SB_HEADS, SB_HEAD_DIM)
        conv_out = conformer_conv(val, gte, b_glu[l], conv_w[l], conv_b[l], conv_ln_g[l], conv_ln_b[l])
        attn_out = stick_breaking_attention(q, k, v)
        m = jnp.concatenate([conv_out, attn_out], axis=-1) @ w_out[l]
        x = x + gate1[:, None, :] * rms_norm(m, g_post_mix[l])

        h = modulate(rms_norm(x, g_pre_ffn[l]), shift2, scale2)
        f = moe_ffn(h, w_router[l], b_router[l], w_gate_up[l], b_gate_up[l], w_down[l], b_down[l])
        x = x + gate2[:, None, :] * rms_norm(f, g_post_ffn[l])
    return x
```

```python
import numpy as np
import concourse.bass as bass
import concourse.mybir as mybir
from concourse.bass_utils import run_bass_kernel_spmd
from contextlib import ExitStack

F32 = mybir.dt.float32
BF16 = mybir.dt.bfloat16
I32 = mybir.dt.int32
AF = mybir.ActivationFunctionType
ALU = mybir.AluOpType

ENGS = ["pe", "act", "dve", "pool", "sp"]
TRUST_SAME = {"pe": True, "act": False, "dve": False, "pool": False, "sp": True}


class Op:
    __slots__ = ("eng", "fn", "deps", "signal", "sigval", "dma_key", "dma_val")

    def __init__(self, eng, fn):
        self.eng = eng
        self.fn = fn
        self.deps = ()
        self.signal = False
        self.sigval = 0
        self.dma_key = None
        self.dma_val = 0


class Prog:
    def __init__(self):
        self.ops = {e: [] for e in ENGS}
        self.last_w = {}
        self.readers = {}
        self.dma_cnt = {}

    def add(self, eng, fn, reads=(), writes=(), dma=None):
        op = Op(eng, fn)
        deps = set()
        for r in reads:
            lw = self.last_w.get(r)
            if lw is not None:
                deps.add(lw)
        for w in writes:
            lw = self.last_w.get(w)
            if lw is not None:
                deps.add(lw)
            rs = self.readers.get(w)
            if rs:
                deps.update(rs)
        op.deps = tuple(deps)
        for r in reads:
            self.readers.setdefault(r, []).append(op)
        for w in writes:
            self.last_w[w] = op
            self.readers[w] = []
        if dma is not None:
            op.dma_key = dma
            self.dma_cnt[dma] = self.dma_cnt.get(dma, 0) + 16
            op.dma_val = self.dma_cnt[dma]
        self.ops[eng].append(op)
        return op

    def emit(self, nc):
        for e in ENGS:
            for op in self.ops[e]:
                for d in op.deps:
                    if d.dma_key is None:
                        if d.eng == op.eng and TRUST_SAME[e]:
                            continue
                        d.signal = True
        for e in ENGS:
            c = 0
            for op in self.ops[e]:
                if op.signal and op.dma_key is None:
                    c += 1
                    op.sigval = c
        with ExitStack() as st:
            sem = {}
            for e in ENGS:
                sem[("eng", e)] = st.enter_context(nc.semaphore("s_" + e))
            for i, k in enumerate(self.dma_cnt):
                sem[("dma", k)] = st.enter_context(nc.semaphore("d%d" % i))
            block = st.enter_context(nc.Block())

            def run(e, eng):
                waited = {}
                for op in self.ops[e]:
                    need = {}
                    for d in op.deps:
                        if d.dma_key is not None:
                            k = ("dma", d.dma_key)
                            v = d.dma_val
                        else:
                            if d.eng == e and TRUST_SAME[e]:
                                continue
                            k = ("eng", d.eng)
                            v = d.sigval
                        if need.get(k, 0) < v:
                            need[k] = v
                    for k, v in need.items():
                        if waited.get(k, 0) < v:
                            eng.wait_ge(sem[k], v)
                            waited[k] = v
                    inst = op.fn(eng)
                    if op.dma_key is not None:
                        inst.then_inc(sem[("dma", op.dma_key)], 16)
                    elif op.signal:
                        inst.then_inc(sem[("eng", e)], 1)

            block.tensor(lambda eng: run("pe", eng))
            block.scalar(lambda eng: run("act", eng))
            block.vector(lambda eng: run("dve", eng))
            block.gpsimd(lambda eng: run("pool", eng))
            block.sync(lambda eng: run("sp", eng))


SB_BASE = 16512 + 2048
SB_END = 229344


class Arena:
    def __init__(self, nc):
        self.nc = nc
        self.top = SB_BASE
        self.n = 0

    def alloc(self, shape, dt, name=None):
        nbytes = int(np.prod(shape[1:])) * (4 if dt in (F32, I32) else 2)
        nbytes = (nbytes + 31) // 32 * 32
        off = self.top
        assert off + nbytes <= SB_END, ("SBUF overflow", name, off + nbytes - SB_END)
        self.top += nbytes
        self.n += 1
        return self.nc.alloc_sbuf_tensor_at("%s_%d" % (name or "t", self.n), list(shape), dt, offset=off)


NTOK = 1024
D = 4096
DH_SCALE = 128 ** -0.5
HALO = 128
VGW = NTOK + HALO
CAP = 384


def build(stage=99, dbg=(), nada=96, ntb=16, nexp=32):
    nc = bass.Bass("TRN2", target_bir_lowering=False)
    di = lambda name, shape, dt=F32: nc.dram_tensor(name, list(shape), dt, kind="ExternalInput").ap()
    ds = lambda name, shape, dt=F32: nc.dram_tensor(name, list(shape), dt).ap()
    xo = di("xo", [NTOK, D])
    xp = di("xp", [NTOK, D])
    flag_d = di("flag", [128, 1])
    cT_d = di("cT", [128, 32])
    w_ada = di("w_ada", [D, 6 * D])
    b_adaT_d = di("b_adaT", [128, 64])
    vec_bc = di("vec_bc", [7, 128, D])
    gpreT_d = di("gpreT", [128, 32])
    w_in = di("w_in", [D, 10240])
    out_d = nc.dram_tensor("out", [NTOK, D], F32, kind="ExternalOutput").ap()
    w_out = di("w_out", [D, D])
    bgluT_d = di("bgluT", [128, 32])
    cwT_d = di("cwT", [128, 16 * 31])
    cvec_d = di("cvec", [128, 48])
    consts_d = di("consts", [6, 128, 128])
    w_router = di("w_router", [D, 32])
    brt_d = di("brt", [128, 32])
    iota_d = di("iota", [128, CAP])
    tid_d = di("tid", [128, 8 * 5])
    w_gu = di("w_gu", [32, D, 3072]) if stage >= 6 else None
    bguT_d = di("bguT", [32, 128, 24])
    w_dn = di("w_dn", [32, 1536, D]) if stage >= 6 else None
    b_dn = di("b_dn", [32, D])
    Md = ds("Md", [NTOK, D])
    X1d = ds("X1d", [NTOK, D])
    Fd = [ds("Fd%d" % c, [NTOK + 1, 512]) for c in range(8)]

    BC = ds("BC", [4, 128, D])
    VG = ds("VG", [D, VGW])
    QT = ds("QT", [16, 128, NTOK], BF16)
    KT = ds("KT", [16, 128, 2 * NTOK], BF16)
    Vd = ds("Vd", [2 * NTOK, 2048], BF16)

    dbg_out = {}

    def dbg_tensor(name, shape, dt):
        dbg_out[name] = nc.dram_tensor("dbg_" + name, list(shape), dt, kind="ExternalOutput").ap()
        return dbg_out[name]

    P = Prog()
    A = Arena(nc)
    out_keys = []
    with ExitStack() as st:
        bank = [st.enter_context(nc.psum_tensor("bank%d" % i, [128, 512], F32)) for i in range(8)]

        ident = A.alloc([128, 128], BF16, "ident")
        flag = A.alloc([128, 1], F32, "flag")
        cT = A.alloc([128, 32], F32, "cT")
        sc = A.alloc([128, 32], BF16, "sc")
        b_adaT = A.alloc([128, 64], F32, "b_adaT")
        gpreT = A.alloc([128, 32], F32, "gpreT")
        modT = A.alloc([128, 64], F32, "modT")
        A1 = A.alloc([128, 32], F32, "A1")
        small = A.alloc([128, 64], F32, "small")
        wbuf = [A.alloc([128, 32, 256], BF16, "wbuf%d" % i) for i in range(2)]
        hT_off = A.top
        hT = A.alloc([128, 32, 2 * NTOK], BF16, "hT")
        regionT = A.top

        P.add("pool", lambda e: e.memset(ident[:], 1.0), writes=["ident"])
        P.add("pool", lambda e: e.affine_select(out=ident[:], in_=ident[:], pattern=[[-1, 128]],
                                                compare_op=ALU.is_equal, fill=0.0, base=0, channel_multiplier=1),
              reads=["ident"], writes=["ident"])
        P.add("sp", lambda e: e.dma_start(out=flag[:], in_=flag_d), writes=["flag"], dma="c0")
        P.add("sp", lambda e: e.dma_start(out=cT[:], in_=cT_d), writes=["cT"], dma="c1")
        P.add("sp", lambda e: e.dma_start(out=b_adaT[:], in_=b_adaT_d), writes=["b_adaT"], dma="c2")
        P.add("sp", lambda e: e.dma_start(out=gpreT[:], in_=gpreT_d), writes=["gpreT"], dma="c3")

        screp = A.alloc([128, 32, 128], BF16, "screp")
        bch = [A.alloc([128, 256], F32, "bch%d" % i) for i in range(2)]
        gch = [A.alloc([128, 256], F32, "gch%d" % i) for i in range(2)]
        och = [A.alloc([128, 256], F32, "och%d" % i) for i in range(2)]
        tch = [A.alloc([128, 256], F32, "tch%d" % i) for i in range(2)]
        P.add("act", lambda e: e.activation(out=sc[:], in_=cT[:], func=AF.Silu), reads=["cT"], writes=["sc"])
        P.add("dve", lambda e: e.tensor_copy(out=screp[:], in_=sc[:].unsqueeze(2).to_broadcast([128, 32, 128])),
              reads=["sc"], writes=["screp"])
        wv_ada = w_ada.rearrange("(kc p) n -> p kc n", p=128)
        NCH_ADA = nada
        for ci in range(NCH_ADA):
            b = ci % 2
            P.add("pool", lambda e, b=b, ci=ci: e.dma_start(out=wbuf[b][:], in_=wv_ada[:, :, ci * 256:(ci + 1) * 256]),
                  writes=[("wbuf", b)], dma=("wbuf", b))
            if ci < 32:
                for sub in range(2):
                    cc = ci * 2 + sub
                    for kd in range(32):
                        P.add("pe", lambda e, b=b, sub=sub, kd=kd, cc=cc: e.matmul(
                            bank[0][:, cc:cc + 1], lhsT=wbuf[b][:, kd, sub * 128:(sub + 1) * 128], rhs=sc[:, kd:kd + 1],
                            start=(kd == 0), stop=(kd == 31)),
                            reads=[("wbuf", b), "sc"], writes=["bank0"])
                if ci == 31:
                    P.add("dve", lambda e: e.tensor_tensor(out=modT[:], in0=bank[0][:, 0:64], in1=b_adaT[:], op=ALU.add),
                          reads=["bank0", "b_adaT"], writes=["modT"])
                    P.add("dve", lambda e: e.scalar_tensor_tensor(out=A1[:], in0=modT[:, 32:64], scalar=1.0, in1=gpreT[:],
                                                                  op0=ALU.add, op1=ALU.mult),
                          reads=["modT", "gpreT"], writes=["A1"])
            else:
                j = (ci - 32) // 16
                c0 = ((ci - 32) % 16) * 256
                pb = 1 + (ci % 2)
                for kd in range(32):
                    P.add("pe", lambda e, b=b, kd=kd, pb=pb: e.matmul(
                        bank[pb][:, 0:256], lhsT=screp[:, kd, :], rhs=wbuf[b][:, kd, :],
                        start=(kd == 0), stop=(kd == 31)),
                        reads=[("wbuf", b), "screp"], writes=[("bank", pb)])
                P.add("sp", lambda e, b=b, j=j, c0=c0: e.dma_start(out=bch[b][:], in_=vec_bc[j, :, c0:c0 + 256]),
                      writes=[("bch", b)], dma=("bch", b))
                if j != 1:
                    gi = {0: 4, 2: 5, 3: 6}[j]
                    P.add("sp", lambda e, b=b, gi=gi, c0=c0: e.dma_start(out=gch[b][:], in_=vec_bc[gi, :, c0:c0 + 256]),
                          writes=[("gch", b)], dma=("gch", b))
                if j == 1:
                    P.add("dve", lambda e, b=b, pb=pb: e.tensor_tensor(out=och[b][:], in0=bank[pb][:, 0:256], in1=bch[b][:],
                                                                      op=ALU.add),
                          reads=[("bank", pb), ("bch", b)], writes=[("och", b)])
                else:
                    addc = 1.0 if j == 2 else 0.0
                    P.add("dve", lambda e, b=b, pb=pb, addc=addc: e.scalar_tensor_tensor(
                        out=tch[b][:], in0=bank[pb][:, 0:256], scalar=addc, in1=bch[b][:], op0=ALU.add, op1=ALU.add),
                        reads=[("bank", pb), ("bch", b)], writes=[("tch", b)])
                    P.add("dve", lambda e, b=b: e.tensor_tensor(out=och[b][:], in0=tch[b][:], in1=gch[b][:], op=ALU.mult),
                          reads=[("tch", b), ("gch", b)], writes=[("och", b)])
                P.add("sp", lambda e, b=b, j=j, c0=c0: e.dma_start(out=BC[j, :, c0:c0 + 256], in_=och[b][:]),
                      reads=[("och", b)], writes=[("BC", j, c0)], dma=("och", b))
        A.top = regionT

        xt = A.alloc([128, D], F32, "xt")
        xs = A.alloc([128, D], BF16, "xs")
        P.add("dve", lambda e: e.engine_nop(),
              writes=[("bch", 0), ("bch", 1), ("gch", 0), ("gch", 1), ("och", 0), ("och", 1), ("tch", 0), ("tch", 1), "screp",
                      "xt", "xs"])
        ev = 0
        for tb in range(ntb):
            src = xp if tb < 8 else xo
            r0 = (tb % 8) * 128
            P.add("sp", lambda e, src=src, r0=r0: e.dma_start(out=xt[:], in_=src[r0:r0 + 128, :]), writes=["xt"], dma="xt")
            P.add("act", lambda e: e.activation(out=xs[:], in_=xt[:], func=AF.Square, accum_out=small[:, 0:1]),
                  reads=["xt"], writes=["xs", "ss"])
            P.add("act", lambda e: e.activation(out=small[:, 1:2], in_=small[:, 0:1], func=AF.Sqrt, scale=1.0 / D, bias=1e-6),
                  reads=["ss"], writes=["sq"])
            P.add("dve", lambda e: e.reciprocal(out=small[:, 2:3], in_=small[:, 1:2]), reads=["sq"], writes=["rstd"])
            P.add("act", lambda e: e.activation(out=xs[:], in_=xt[:], func=AF.Copy, scale=small[:, 2:3]),
                  reads=["xt", "rstd"], writes=["xs"])
            for kc in range(32):
                pb = 3 + kc % 4
                slot = 0
                pt = bank[pb][:, 0:64].bitcast(BF16)
                P.add("pe", lambda e, kc=kc, pt=pt: e.transpose(out=pt, in_=xs[:, kc * 128:(kc + 1) * 128], identity=ident[:]),
                      reads=["xs", "ident"], writes=[("pt", pb, slot)])
                dst = hT[:, kc, tb * 128:(tb + 1) * 128]
                if ev % 2 == 0:
                    P.add("act", lambda e, kc=kc, pt=pt, dst=dst: e.activation(
                        out=dst, in_=pt, func=AF.Identity, scale=A1[:, kc:kc + 1], bias=modT[:, kc:kc + 1]),
                        reads=[("pt", pb, slot), "A1", "modT"], writes=[("hT", tb)])
                else:
                    P.add("dve", lambda e, kc=kc, pt=pt, dst=dst: e.tensor_scalar(
                        out=dst, in0=pt, scalar1=A1[:, kc:kc + 1], scalar2=modT[:, kc:kc + 1], op0=ALU.mult, op1=ALU.add),
                        reads=[("pt", pb, slot), "A1", "modT"], writes=[("hT", tb)])
                ev += 1
        A.top = regionT

        def dump(nm, shp, dt, parts, keys):
            o = dbg_tensor(nm, shp, dt)
            for i, (dst_fn, src_fn) in enumerate(parts):
                P.add("sp", lambda e, o=o, dst_fn=dst_fn, src_fn=src_fn: e.dma_start(out=dst_fn(o), in_=src_fn()),
                      reads=keys, writes=[("dbg_" + nm, i)], dma=("dbg", i % 2))
                out_keys.append(("dbg_" + nm, i))

        if "modT" in dbg:
            dump("modT", [128, 64], F32, [((lambda o: o), (lambda: modT[:]))], ["modT"])
        if "hT" in dbg:
            dump("hT", [128, 32, 2 * NTOK], BF16,
                 [((lambda o, kc=kc: o[:, kc, :]), (lambda kc=kc: hT[:, kc, :])) for kc in range(32)],
                 [("hT", tb) for tb in range(16)])
        if "BC" in dbg:
            dump("BC", [4, 128, D], F32,
                 [((lambda o, j=j: o[j]), (lambda j=j: BC[j])) for j in range(4)],
                 [("BC", j, c0) for j in range(4) for c0 in range(0, D, 256)])

        if stage >= 2:
            stg = [A.alloc([128, VGW], F32, "stg%d" % i) for i in range(2)]
            stgk = [A.alloc([128, 2 * NTOK], BF16, "stgk%d" % i) for i in range(2)]
            stgv = [A.alloc([128, 256], BF16, "stgv%d" % i) for i in range(2)]
            P.add("dve", lambda e: e.engine_nop(),
                  writes=["xt", "xs", ("stg", 0), ("stg", 1), ("stgk", 0), ("stgk", 1), ("stgv", 0), ("stgv", 1)])
            wv_in = w_in.rearrange("(kc p) n -> p kc n", p=128)
            allh = [("hT", tb) for tb in range(16)]
            pbc = 0
            sg = 0
            sgk = 0
            sgv = 0
            ev = 0

            def evac(dst, srcp, rk, wk, scale=None):
                nonlocal ev
                if ev % 2 == 0:
                    if scale is None:
                        P.add("act", lambda e: e.copy(out=dst, in_=srcp), reads=rk, writes=wk)
                    elif isinstance(scale, float):
                        P.add("act", lambda e: e.activation(out=dst, in_=srcp, func=AF.Copy, scale=scale), reads=rk, writes=wk)
                    else:
                        P.add("act", lambda e: e.activation(out=dst, in_=srcp, func=AF.Copy, scale=scale),
                              reads=rk + ["flag"], writes=wk)
                else:
                    if scale is None:
                        P.add("dve", lambda e: e.tensor_copy(out=dst, in_=srcp), reads=rk, writes=wk)
                    elif isinstance(scale, float):
                        P.add("dve", lambda e: e.tensor_single_scalar(out=dst, in_=srcp, scalar=scale, op=ALU.mult),
                              reads=rk, writes=wk)
                    else:
                        P.add("dve", lambda e: e.tensor_scalar(out=dst, in0=srcp, scalar1=scale, scalar2=None, op0=ALU.mult),
                              reads=rk + ["flag"], writes=wk)
                ev += 1

            for ci in range(40):
                b = ci % 2
                P.add("pool", lambda e, b=b, ci=ci: e.dma_start(out=wbuf[b][:], in_=wv_in[:, :, ci * 256:(ci + 1) * 256]),
                      writes=[("wbuf", b)], dma=("wbuf", b))
                kind = ci // 8
                if kind <= 1:
                    for sub in range(2):
                        s_ = sg % 2
                        sg += 1
                        row0 = ci * 256 + sub * 128
                        for (t0, n, o0) in ((NTOK - HALO, HALO, 0), (NTOK, 512, HALO), (NTOK + 512, 512, HALO + 512)):
                            pb = pbc % 8
                            pbc += 1
                            for kc in range(32):
                                P.add("pe", lambda e, b=b, sub=sub, kc=kc, pb=pb, t0=t0, n=n: e.matmul(
                                    bank[pb][:, 0:n], lhsT=wbuf[b][:, kc, sub * 128:(sub + 1) * 128], rhs=hT[:, kc, t0:t0 + n],
                                    start=(kc == 0), stop=(kc == 31)),
                                    reads=[("wbuf", b)] + allh, writes=[("bank", pb)])
                            evac(stg[s_][:, o0:o0 + n], bank[pb][:, 0:n], [("bank", pb)], [("stg", s_)])
                        P.add("sp", lambda e, s_=s_, row0=row0: e.dma_start(out=VG[row0:row0 + 128, :], in_=stg[s_][:]),
                              reads=[("stg", s_)], writes=[("VG", row0)], dma=("stg", s_))
                elif kind <= 3:
                    isq = kind == 2
                    for sub in range(2):
                        head = (ci % 8) * 2 + sub
                        s_ = sgk % 2
                        sgk += 1
                        groups = ((NTOK, 0), (NTOK + 512, 512)) if isq else ((0, 0), (512, 512), (1024, 1024), (1536, 1536))
                        for (t0, o0) in groups:
                            pb = pbc % 8
                            pbc += 1
                            for kc in range(32):
                                P.add("pe", lambda e, b=b, sub=sub, kc=kc, pb=pb, t0=t0: e.matmul(
                                    bank[pb][:, 0:512], lhsT=wbuf[b][:, kc, sub * 128:(sub + 1) * 128], rhs=hT[:, kc, t0:t0 + 512],
                                    start=(kc == 0), stop=(kc == 31)),
                                    reads=[("wbuf", b)] + allh, writes=[("bank", pb)])
                            evac(stgk[s_][:, o0:o0 + 512], bank[pb][:, 0:512], [("bank", pb)], [("stgk", s_)],
                                 scale=(DH_SCALE if isq else None))
                        if isq:
                            P.add("sp", lambda e, s_=s_, head=head: e.dma_start(out=QT[head], in_=stgk[s_][:, 0:NTOK]),
                                  reads=[("stgk", s_)], writes=[("QT", head)], dma=("stgk", s_))
                        else:
                            P.add("sp", lambda e, s_=s_, head=head: e.dma_start(out=KT[head], in_=stgk[s_][:]),
                                  reads=[("stgk", s_)], writes=[("KT", head)], dma=("stgk", s_))
                else:
                    c0 = (ci - 32) * 256
                    for tb in range(16):
                        pb = pbc % 8
                        pbc += 1
                        s_ = sgv % 2
                        sgv += 1
                        for kc in range(32):
                            P.add("pe", lambda e, b=b, kc=kc, pb=pb, tb=tb: e.matmul(
                                bank[pb][:, 0:256], lhsT=hT[:, kc, tb * 128:(tb + 1) * 128], rhs=wbuf[b][:, kc, :],
                                start=(kc == 0), stop=(kc == 31)),
                                reads=[("wbuf", b), ("hT", tb)], writes=[("bank", pb)])
                        evac(stgv[s_][:], bank[pb][:, 0:256], [("bank", pb)], [("stgv", s_)],
                             scale=(flag[:, 0:1] if tb < 8 else None))
                        P.add("sp", lambda e, s_=s_, tb=tb, c0=c0: e.dma_start(out=Vd[tb * 128:(tb + 1) * 128, c0:c0 + 256],
                                                                             in_=stgv[s_][:]),
                              reads=[("stgv", s_)], writes=[("Vd", tb, c0)], dma=("stgv", s_))
            A.top = regionT
            if "VG" in dbg:
                dump("VG", [D, VGW], F32,
                     [((lambda o, r=r: o[r * 128:(r + 1) * 128, :]), (lambda r=r: VG[r * 128:(r + 1) * 128, :])) for r in range(32)],
                     [("VG", r) for r in range(0, D, 128)])
            if "QT" in dbg:
                dump("QT", [16, 128, NTOK], BF16, [((lambda o, h=h: o[h]), (lambda h=h: QT[h])) for h in range(16)],
                     [("QT", h) for h in range(16)])
            if "KT" in dbg:
                dump("KT", [16, 128, 2 * NTOK], BF16, [((lambda o, h=h: o[h]), (lambda h=h: KT[h])) for h in range(16)],
                     [("KT", h) for h in range(16)])
            if "Vd" in dbg:
                dump("Vd", [2 * NTOK, 2048], BF16,
                     [((lambda o, r=r: o[r * 128:(r + 1) * 128, :]), (lambda r=r: Vd[r * 128:(r + 1) * 128, :])) for r in range(16)],
                     [("Vd", tb, c0) for tb in range(16) for c0 in range(0, 2048, 256)])

        if stage >= 3:
            A.top = hT_off
            oldk = [("hT", tb) for tb in range(16)] + [("stg", 0), ("stg", 1), ("stgk", 0), ("stgk", 1), ("stgv", 0),
                                                      ("stgv", 1), "xt", "xs"]
            mixT = A.alloc([128, 32, NTOK], BF16, "mixT")
            bgluT = A.alloc([128, 32], F32, "bgluT")
            cwT = A.alloc([128, 16 * 31], F32, "cwT")
            cvec = A.alloc([128, 48], F32, "cvec")
            cst = A.alloc([128, 6, 128], F32, "cst")
            cstb = A.alloc([128, 6, 128], BF16, "cstb")
            P3top = A.top
            cv_all = A.alloc([128, 16, NTOK], F32, "cv_all")
            vt = [A.alloc([128, VGW], F32, "vt%d" % i) for i in range(2)]
            gt = [A.alloc([128, VGW], F32, "gt%d" % i) for i in range(2)]
            ut = A.alloc([128, VGW], F32, "ut")
            sq = A.alloc([128, NTOK], F32, "sq")
            mixk = [("mix", c) for c in range(32)]
            newk = mixk + ["bgluT", "cwT", "cvec", "cst", "cstb", "ut", "sq", ("vt", 0), ("vt", 1), ("gt", 0), ("gt", 1)] + \
                [("cv", g) for g in range(16)]
            P.add("dve", lambda e: e.engine_nop(), writes=oldk + newk)
            P.add("sp", lambda e: e.dma_start(out=bgluT[:], in_=bgluT_d), writes=["bgluT"], dma="c0")
            P.add("sp", lambda e: e.dma_start(out=cwT[:], in_=cwT_d), writes=["cwT"], dma="c1")
            P.add("sp", lambda e: e.dma_start(out=cvec[:], in_=cvec_d), writes=["cvec"], dma="c2")
            P.add("sp", lambda e: e.dma_start(out=cst[:], in_=consts_d.rearrange("c p n -> p c n")), writes=["cst"], dma="c3")
            P.add("dve", lambda e: e.tensor_copy(out=cstb[:], in_=cst[:]), reads=["cst"], writes=["cstb"])
            ones_f = cst[:, 0, :]
            ntri_f = cst[:, 1, :]
            maskT_f = cst[:, 2, :]
            nones_f = cst[:, 3, :]
            triS_b = cstb[:, 4, :]
            ones_b = cstb[:, 0, :]
            for g in range(16):
                i = g % 2
                P.add("sp", lambda e, g=g, i=i: e.dma_start(out=vt[i][:], in_=VG[g * 128:(g + 1) * 128, :]),
                      reads=[("VG", g * 128)], writes=[("vt", i)], dma=("vt", i))
                P.add("sp", lambda e, g=g, i=i: e.dma_start(out=gt[i][:], in_=VG[2048 + g * 128:2048 + (g + 1) * 128, :]),
                      reads=[("VG", 2048 + g * 128)], writes=[("gt", i)], dma=("gt", i))
                P.add("act", lambda e, g=g, i=i: e.activation(out=gt[i][:], in_=gt[i][:], func=AF.Sigmoid,
                                                              bias=bgluT[:, 16 + g:17 + g]),
                      reads=[("gt", i), "bgluT"], writes=[("gt", i)])
                P.add("dve", lambda e, g=g, i=i: e.scalar_tensor_tensor(out=ut[:], in0=vt[i][:], scalar=bgluT[:, g:g + 1],
                                                                        in1=gt[i][:], op0=ALU.add, op1=ALU.mult),
                      reads=[("vt", i), ("gt", i), "bgluT"], writes=["ut"])
                P.add("dve", lambda e: e.tensor_scalar(out=ut[:, 0:HALO], in0=ut[:, 0:HALO], scalar1=flag[:, 0:1], scalar2=None,
                                                       op0=ALU.mult), reads=["ut", "flag"], writes=["ut"])
                o0 = HALO - 30
                P.add("dve", lambda e, g=g, o0=o0: e.tensor_scalar(out=cv_all[:, g, :], in0=ut[:, o0:o0 + NTOK],
                                                                   scalar1=cwT[:, g * 31:g * 31 + 1], scalar2=cvec[:, g:g + 1],
                                                                   op0=ALU.mult, op1=ALU.add),
                      reads=["ut", "cwT", "cvec"], writes=[("cv", g)])
                for j in range(1, 31):
                    P.add("dve", lambda e, g=g, j=j, o0=o0: e.scalar_tensor_tensor(
                        out=cv_all[:, g, :], in0=ut[:, o0 + j:o0 + j + NTOK], scalar=cwT[:, g * 31 + j:g * 31 + j + 1],
                        in1=cv_all[:, g, :], op0=ALU.mult, op1=ALU.add),
                        reads=["ut", "cwT", ("cv", g)], writes=[("cv", g)])
                P.add("act", lambda e, g=g: e.activation(out=sq[:], in_=cv_all[:, g, :], func=AF.Square),
                      reads=[("cv", g)], writes=["sq"])
                for hf in range(2):
                    P.add("pe", lambda e, g=g, hf=hf: e.matmul(bank[hf][:, 0:512], lhsT=ones_f,
                                                               rhs=cv_all[:, g, hf * 512:(hf + 1) * 512],
                                                               start=(g == 0), stop=(g == 15)),
                          reads=[("cv", g), "cst"], writes=[("bank", hf)])
                    P.add("pe", lambda e, g=g, hf=hf: e.matmul(bank[2 + hf][:, 0:512], lhsT=ones_f,
                                                               rhs=sq[:, hf * 512:(hf + 1) * 512],
                                                               start=(g == 0), stop=(g == 15)),
                          reads=["sq", "cst"], writes=[("bank", 2 + hf)])
            mean = vt[0]
            var = vt[1]
            nmr = gt[0]
            tmp = gt[1]
            for hf in range(2):
                sl = slice(hf * 512, (hf + 1) * 512)
                P.add("act", lambda e, hf=hf, sl=sl: e.activation(out=mean[:, sl], in_=bank[hf][:, 0:512], func=AF.Copy,
                                                                  scale=1.0 / 2048),
                      reads=[("bank", hf)], writes=[("vt", 0)])
                P.add("dve", lambda e, hf=hf, sl=sl: e.tensor_single_scalar(out=var[:, sl], in_=bank[2 + hf][:, 0:512],
                                                                            scalar=1.0 / 2048, op=ALU.mult),
                      reads=[("bank", 2 + hf)], writes=[("vt", 1)])
            P.add("dve", lambda e: e.tensor_tensor(out=tmp[:, 0:NTOK], in0=mean[:, 0:NTOK], in1=mean[:, 0:NTOK], op=ALU.mult),
                  reads=[("vt", 0)], writes=[("gt", 1)])
            P.add("dve", lambda e: e.tensor_tensor(out=var[:, 0:NTOK], in0=var[:, 0:NTOK], in1=tmp[:, 0:NTOK], op=ALU.subtract),
                  reads=[("vt", 1), ("gt", 1)], writes=[("vt", 1)])
            P.add("act", lambda e: e.activation(out=var[:, 0:NTOK], in_=var[:, 0:NTOK], func=AF.Sqrt, bias=1e-5),
                  reads=[("vt", 1)], writes=[("vt", 1)])
            P.add("dve", lambda e: e.reciprocal(out=var[:, 0:NTOK], in_=var[:, 0:NTOK]), reads=[("vt", 1)], writes=[("vt", 1)])
            P.add("dve", lambda e: e.scalar_tensor_tensor(out=nmr[:, 0:NTOK], in0=mean[:, 0:NTOK], scalar=-1.0,
                                                          in1=var[:, 0:NTOK], op0=ALU.mult, op1=ALU.mult),
                  reads=[("vt", 0), ("vt", 1)], writes=[("gt", 0)])
            for g in range(16):
                P.add("dve", lambda e, g=g: e.tensor_tensor(out=cv_all[:, g, :], in0=cv_all[:, g, :], in1=var[:, 0:NTOK],
                                                            op=ALU.mult), reads=[("cv", g), ("vt", 1)], writes=[("cv", g)])
                P.add("dve", lambda e, g=g: e.tensor_tensor(out=cv_all[:, g, :], in0=cv_all[:, g, :], in1=nmr[:, 0:NTOK],
                                                            op=ALU.add), reads=[("cv", g), ("gt", 0)], writes=[("cv", g)])
                P.add("act", lambda e, g=g: e.activation(out=mixT[:, g, :], in_=cv_all[:, g, :], func=AF.Silu,
                                                         scale=cvec[:, 16 + g:17 + g], bias=cvec[:, 32 + g:33 + g]),
                      reads=[("cv", g), "cvec"], writes=[("mix", g)])
            A.top = P3top

        if stage >= 4:
            qt = [A.alloc([128, NTOK], BF16, "qt%d" % i) for i in range(2)]
            kt = [A.alloc([128, 2 * NTOK], BF16, "kt%d" % i) for i in range(2)]
            vv = [A.alloc([128, 16, 128], BF16, "vv%d" % i) for i in range(2)]
            Rt = A.alloc([128, 128], F32, "Rt")
            wk = [[A.alloc([128, 128], F32, "wk%d_%d" % (i, j)) for j in range(4)] for i in range(4)]
            ab = [A.alloc([128, 128], BF16, "ab%d" % i) for i in range(4)]
            oldk = [("cv", g) for g in range(16)] + ["ut", "sq", ("vt", 0), ("vt", 1), ("gt", 0), ("gt", 1)]
            newk = [("qt", 0), ("qt", 1), ("kt", 0), ("kt", 1), ("vv", 0), ("vv", 1), "Rt", "ab0", "ab1", "ab2", "ab3"] + \
                [("wk", i, j) for i in range(4) for j in range(4)]
            P.add("dve", lambda e: e.engine_nop(), writes=oldk + newk)
            pc = 0
            for h in range(16):
                i = h % 2
                P.add("sp", lambda e, h=h, i=i: e.dma_start(out=qt[i][:], in_=QT[h]), reads=[("QT", h)], writes=[("qt", i)],
                      dma=("qt", i))
                P.add("sp", lambda e, h=h, i=i: e.dma_start(out=kt[i][:], in_=KT[h]), reads=[("KT", h)], writes=[("kt", i)],
                      dma=("kt", i))
                P.add("sp", lambda e, h=h, i=i: e.dma_start(
                    out=vv[i][:], in_=Vd[:, h * 128:(h + 1) * 128].rearrange("(blk p) d -> p blk d", p=128)),
                    reads=[("Vd", tb, (h // 2) * 256) for tb in range(16)], writes=[("vv", i)], dma=("vv", i))
                for qb in range(8):
                    ob = 6 + qb % 2
                    nkb = 9 + qb
                    for n_, gkb in enumerate(range(8 + qb, -1, -1)):
                        first = n_ == 0
                        last = n_ == nkb - 1
                        w = pc % 4
                        pc += 1
                        zb = ("z", w)
                        tb_ = ("tri", w)
                        nb = ("ones", w)
                        zA = bank[0 + w // 2][:, (w % 2) * 128:(w % 2) * 128 + 128]
                        tA = bank[2 + w // 2][:, (w % 2) * 128:(w % 2) * 128 + 128]
                        nA = bank[4 + w // 2][:, (w % 2) * 128:(w % 2) * 128 + 128]
                        ex, sp_, lg_, aa = wk[w]
                        P.add("pe", lambda e, i=i, gkb=gkb, qb=qb, zA=zA: e.matmul(
                            zA, lhsT=kt[i][:, gkb * 128:(gkb + 1) * 128], rhs=qt[i][:, qb * 128:(qb + 1) * 128],
                            start=True, stop=True), reads=[("kt", i), ("qt", i)], writes=[zb])
                        P.add("act", lambda e, zA=zA, ex=ex: e.activation(out=ex[:], in_=zA, func=AF.Exp),
                              reads=[zb], writes=[("wk", w, 0)])
                        P.add("act", lambda e, ex=ex, sp_=sp_: e.activation(out=sp_[:], in_=ex[:], func=AF.Ln, bias=1.0),
                              reads=[("wk", w, 0)], writes=[("wk", w, 1)])
                        if first:
                            P.add("dve", lambda e, ex=ex, sp_=sp_: e.tensor_tensor(out=ex[:], in0=sp_[:], in1=maskT_f, op=ALU.mult),
                                  reads=[("wk", w, 1), "cst"], writes=[("wk", w, 0)])
                            spm = ex
                            spk = ("wk", w, 0)
                        else:
                            spm = sp_
                            spk = ("wk", w, 1)
                        P.add("pe", lambda e, tA=tA, spm=spm: e.matmul(tA, lhsT=ntri_f, rhs=spm[:],
                                                                         start=True, stop=True),
                              reads=[spk, "cst"], writes=[tb_])
                        P.add("pe", lambda e, nA=nA, spm=spm: e.matmul(nA, lhsT=nones_f, rhs=spm[:],
                                                                       start=True, stop=True),
                              reads=[spk, "cst"], writes=[nb])
                        P.add("dve", lambda e, zA=zA, sp_=sp_, lg_=lg_: e.tensor_tensor(out=lg_[:], in0=zA,
                                                                                        in1=sp_[:], op=ALU.subtract),
                              reads=[zb, ("wk", w, 1)], writes=[("wk", w, 2)])
                        P.add("dve", lambda e, tA=tA, lg_=lg_: e.tensor_tensor(out=lg_[:], in0=lg_[:], in1=tA,
                                                                                 op=ALU.add),
                              reads=[tb_, ("wk", w, 2)], writes=[("wk", w, 2)])
                        if not first:
                            P.add("dve", lambda e, lg_=lg_: e.tensor_tensor(out=lg_[:], in0=lg_[:], in1=Rt[:], op=ALU.add),
                                  reads=["Rt", ("wk", w, 2)], writes=[("wk", w, 2)])
                        if first:
                            P.add("act", lambda e, lg_=lg_, aa=aa: e.activation(out=aa[:], in_=lg_[:], func=AF.Exp),
                                  reads=[("wk", w, 2)], writes=[("wk", w, 3)])
                            P.add("dve", lambda e, aa=aa, w=w: e.tensor_tensor(out=ab[w][:], in0=aa[:], in1=maskT_f, op=ALU.mult),
                                  reads=[("wk", w, 3), "cst"], writes=["ab%d" % w])
                        else:
                            P.add("act", lambda e, lg_=lg_, w=w: e.activation(out=ab[w][:], in_=lg_[:], func=AF.Exp),
                                  reads=[("wk", w, 2)], writes=["ab%d" % w])
                        P.add("pe", lambda e, i=i, gkb=gkb, ob=ob, w=w, first=first, last=last: e.matmul(
                            bank[ob][:, 0:128], lhsT=vv[i][:, gkb, :], rhs=ab[w][:], start=first, stop=last),
                            reads=[("vv", i), "ab%d" % w], writes=[("bank", ob)])
                        if not last:
                            if first:
                                P.add("dve", lambda e, nA=nA: e.tensor_copy(out=Rt[:], in_=nA),
                                      reads=[nb], writes=["Rt"])
                            else:
                                P.add("dve", lambda e, nA=nA: e.tensor_tensor(out=Rt[:], in0=Rt[:], in1=nA,
                                                                              op=ALU.add),
                                      reads=[nb, "Rt"], writes=["Rt"])
                    P.add("act", lambda e, h=h, qb=qb, ob=ob: e.copy(out=mixT[:, 16 + h, qb * 128:(qb + 1) * 128],
                                                                    in_=bank[ob][:, 0:128]),
                          reads=[("bank", ob)], writes=[("mix", 16 + h)])
            A.top = P3top
            if "mixT" in dbg:
                dump("mixT", [128, 32, NTOK], BF16,
                     [((lambda o, c=c: o[:, c, :]), (lambda c=c: mixT[:, c, :])) for c in range(32)], mixk)

        if stage >= 5:
            mst = [A.alloc([128, 256], F32, "mst%d" % i) for i in range(2)]
            junk = A.alloc([128, 256], F32, "junk")
            ssq = A.alloc([128, 8, 16], F32, "ssq")
            oldk = [("qt", 0), ("qt", 1), ("kt", 0), ("kt", 1), ("vv", 0), ("vv", 1), "Rt", "ab0", "ab1", "ab2", "ab3"] + \
                [("wk", i, j) for i in range(4) for j in range(4)]
            P.add("dve", lambda e: e.engine_nop(), writes=oldk + [("mst", 0), ("mst", 1), "junk", "ssq"])
            wv_out = w_out.rearrange("(kc p) n -> p kc n", p=128)
            pbc = 0
            ms = 0
            for ci in range(16):
                b = ci % 2
                P.add("pool", lambda e, b=b, ci=ci: e.dma_start(out=wbuf[b][:], in_=wv_out[:, :, ci * 256:(ci + 1) * 256]),
                      writes=[("wbuf", b)], dma=("wbuf", b))
                for tb in range(8):
                    pb = pbc % 8
                    pbc += 1
                    s_ = ms % 2
                    ms += 1
                    for kc in range(32):
                        P.add("pe", lambda e, b=b, kc=kc, pb=pb, tb=tb: e.matmul(
                            bank[pb][:, 0:256], lhsT=mixT[:, kc, tb * 128:(tb + 1) * 128], rhs=wbuf[b][:, kc, :],
                            start=(kc == 0), stop=(kc == 31)), reads=[("wbuf", b), ("mix", kc)], writes=[("bank", pb)])
                    P.add("act", lambda e, pb=pb, s_=s_: e.copy(out=mst[s_][:], in_=bank[pb][:, 0:256]),
                          reads=[("bank", pb)], writes=[("mst", s_)])
                    P.add("act", lambda e, s_=s_, tb=tb, ci=ci: e.activation(out=junk[:], in_=mst[s_][:], func=AF.Square,
                                                                             accum_out=ssq[:, tb, ci:ci + 1]),
                          reads=[("mst", s_)], writes=["junk", "ssq"])
                    P.add("sp", lambda e, s_=s_, tb=tb, ci=ci: e.dma_start(out=Md[tb * 128:(tb + 1) * 128, ci * 256:(ci + 1) * 256],
                                                                         in_=mst[s_][:]),
                          reads=[("mst", s_)], writes=[("Md", tb)], dma=("mst", s_))
            A.top = hT_off
            h2_all = A.alloc([128, 8, D], BF16, "h2_all")
            A.top = P3top
            wr = A.alloc([128, 32, 32], BF16, "wr")
            brt = A.alloc([128, 32], F32, "brt")
            lgt = A.alloc([128, 32], F32, "lgt")
            mx8 = A.alloc([128, 8], F32, "mx8")
            mkf = A.alloc([128, 32], F32, "mkf")
            ext = A.alloc([128, 32], F32, "ext")
            G_all = A.alloc([128, 8, 32], F32, "G_all")
            mk_all = A.alloc([128, 8, 32], BF16, "mk_all")
            mkf_all = A.alloc([128, 8, 32], F32, "mkf_all")
            pos_all = A.alloc([128, 8, 32], F32, "pos_all")
            P5keep = A.top
            ggt = A.alloc([128, D], F32, "ggt")
            a2t = A.alloc([128, D], F32, "a2t")
            b2t = A.alloc([128, D], F32, "b2t")
            xm = A.alloc([128, D], F32, "xm")
            xx = A.alloc([128, D], F32, "xx")
            h2T = A.alloc([128, 32, 128], BF16, "h2T")
            P5top = A.top
            h2k = [("h2", tb) for tb in range(8)]
            P.add("dve", lambda e: e.engine_nop(),
                  writes=mixk + ["bgluT", "cwT", "cvec", ("mst", 0), ("mst", 1), "junk"] + h2k +
                  ["ggt", "a2t", "b2t", "xm", "xx", "h2T", "wr", "brt", "lgt", "mx8", "mkf", "ext", "G_all", "mk_all", "mkf_all",
                   "pos_all"])
            BCk = lambda j: [("BC", j, c0) for c0 in range(0, D, 256)]
            P.add("sp", lambda e: e.dma_start(out=ggt[:], in_=BC[0]), reads=BCk(0), writes=["ggt"], dma="c0")
            P.add("sp", lambda e: e.dma_start(out=a2t[:], in_=BC[2]), reads=BCk(2), writes=["a2t"], dma="c1")
            P.add("sp", lambda e: e.dma_start(out=b2t[:], in_=BC[1]), reads=BCk(1), writes=["b2t"], dma="c2")
            P.add("sp", lambda e: e.dma_start(out=brt[:], in_=brt_d), writes=["brt"], dma="c3")
            P.add("pool", lambda e: e.dma_start(out=wr[:], in_=w_router.rearrange("(kc p) n -> p kc n", p=128)),
                  writes=["wr"], dma="wr")
            ev = 0
            for tb in range(8):
                P.add("sp", lambda e, tb=tb: e.dma_start(out=xm[:], in_=Md[tb * 128:(tb + 1) * 128, :]), reads=[("Md", tb)],
                      writes=["xm"], dma="xm")
                P.add("sp", lambda e, tb=tb: e.dma_start(out=xx[:], in_=xo[tb * 128:(tb + 1) * 128, :]), writes=["xx"], dma="xx")
                P.add("dve", lambda e, tb=tb: e.reduce_sum(out=small[:, 8:9], in_=ssq[:, tb, :], axis=mybir.AxisListType.X),
                      reads=["ssq"], writes=["s8"])
                P.add("act", lambda e: e.activation(out=small[:, 9:10], in_=small[:, 8:9], func=AF.Sqrt, scale=1.0 / D, bias=1e-6),
                      reads=["s8"], writes=["s9"])
                P.add("dve", lambda e: e.reciprocal(out=small[:, 10:11], in_=small[:, 9:10]), reads=["s9"], writes=["s10"])
                P.add("dve", lambda e: e.scalar_tensor_tensor(out=xm[:], in0=xm[:], scalar=small[:, 10:11], in1=ggt[:],
                                                              op0=ALU.mult, op1=ALU.mult),
                      reads=["xm", "s10", "ggt"], writes=["xm"])
                P.add("dve", lambda e: e.tensor_tensor(out=xx[:], in0=xx[:], in1=xm[:], op=ALU.add), reads=["xx", "xm"],
                      writes=["xx"])
                P.add("sp", lambda e, tb=tb: e.dma_start(out=X1d[tb * 128:(tb + 1) * 128, :], in_=xx[:]), reads=["xx"],
                      writes=[("X1", tb)], dma="x1s")
                P.add("act", lambda e: e.activation(out=xm[:], in_=xx[:], func=AF.Square, accum_out=small[:, 11:12]),
                      reads=["xx"], writes=["xm", "s11"])
                P.add("act", lambda e: e.activation(out=small[:, 12:13], in_=small[:, 11:12], func=AF.Sqrt, scale=1.0 / D,
                                                    bias=1e-6), reads=["s11"], writes=["s12"])
                P.add("dve", lambda e: e.reciprocal(out=small[:, 13:14], in_=small[:, 12:13]), reads=["s12"], writes=["s13"])
                P.add("dve", lambda e: e.scalar_tensor_tensor(out=xm[:], in0=xx[:], scalar=small[:, 13:14], in1=a2t[:],
                                                              op0=ALU.mult, op1=ALU.mult),
                      reads=["xx", "s13", "a2t"], writes=["xm"])
                P.add("dve", lambda e, tb=tb: e.tensor_tensor(out=h2_all[:, tb, :], in0=xm[:], in1=b2t[:], op=ALU.add),
                      reads=["xm", "b2t"], writes=[("h2", tb)])
                for kc in range(32):
                    pb = kc % 4
                    pt = bank[pb][:, 0:64].bitcast(BF16)
                    P.add("pe", lambda e, kc=kc, pt=pt, tb=tb: e.transpose(out=pt, in_=h2_all[:, tb, kc * 128:(kc + 1) * 128],
                                                                          identity=ident[:]),
                          reads=[("h2", tb), "ident"], writes=[("bank", pb)])
                    if ev % 2 == 0:
                        P.add("act", lambda e, kc=kc, pt=pt: e.copy(out=h2T[:, kc, :], in_=pt), reads=[("bank", pb)],
                              writes=[("h2T", kc)])
                    else:
                        P.add("dve", lambda e, kc=kc, pt=pt: e.tensor_copy(out=h2T[:, kc, :], in_=pt), reads=[("bank", pb)],
                              writes=[("h2T", kc)])
                    ev += 1
                for kc in range(32):
                    P.add("pe", lambda e, kc=kc: e.matmul(bank[4][:, 0:32], lhsT=h2T[:, kc, :], rhs=wr[:, kc, :],
                                                          start=(kc == 0), stop=(kc == 31)),
                          reads=[("h2T", kc), "wr"], writes=[("bank", 4)])
                P.add("dve", lambda e: e.tensor_tensor(out=lgt[:], in0=bank[4][:, 0:32], in1=brt[:], op=ALU.add),
                      reads=[("bank", 4), "brt"], writes=["lgt"])
                P.add("dve", lambda e: e.max(out=mx8[:], in_=lgt[:]), reads=["lgt"], writes=["mx8"])
                P.add("dve", lambda e: e.tensor_single_scalar(out=small[:, 14:15], in_=mx8[:, 0:1], scalar=-1.0, op=ALU.mult),
                      reads=["mx8"], writes=["s14"])
                P.add("dve", lambda e: e.tensor_scalar(out=mkf[:], in0=lgt[:], scalar1=mx8[:, 3:4], scalar2=None, op0=ALU.is_ge),
                      reads=["lgt", "mx8"], writes=["mkf"])
                P.add("act", lambda e: e.activation(out=ext[:], in_=lgt[:], func=AF.Exp, bias=small[:, 14:15]),
                      reads=["lgt", "s14"], writes=["ext"])
                P.add("dve", lambda e: e.tensor_tensor(out=ext[:], in0=ext[:], in1=mkf[:], op=ALU.mult), reads=["ext", "mkf"],
                      writes=["ext"])
                P.add("dve", lambda e: e.reduce_sum(out=small[:, 15:16], in_=ext[:], axis=mybir.AxisListType.X), reads=["ext"],
                      writes=["s15"])
                P.add("dve", lambda e: e.reciprocal(out=small[:, 16:17], in_=small[:, 15:16]), reads=["s15"], writes=["s16"])
                P.add("dve", lambda e, tb=tb: e.tensor_scalar(out=G_all[:, tb, :], in0=ext[:], scalar1=small[:, 16:17],
                                                              scalar2=None, op0=ALU.mult),
                      reads=["ext", "s16"], writes=["G_all"])
                P.add("dve", lambda e, tb=tb: e.tensor_copy(out=mk_all[:, tb, :], in_=mkf[:]), reads=["mkf"], writes=[("mk", tb)])
                P.add("dve", lambda e, tb=tb: e.tensor_copy(out=mkf_all[:, tb, :], in_=mkf[:]), reads=["mkf"], writes=["mkf_all"])
                P.add("pe", lambda e, tb=tb: e.matmul(bank[5][:, 0:32], lhsT=triS_b, rhs=mk_all[:, tb, :], start=True,
                                                      stop=(tb == 0)), reads=[("mk", tb), "cstb"], writes=[("bank", 5)])
                for pb_ in range(tb):
                    P.add("pe", lambda e, pb_=pb_, tb=tb: e.matmul(bank[5][:, 0:32], lhsT=ones_b, rhs=mk_all[:, pb_, :],
                                                                   start=False, stop=(pb_ == tb - 1)),
                          reads=[("mk", pb_), "cstb"], writes=[("bank", 5)])
                P.add("dve", lambda e, tb=tb: e.tensor_copy(out=pos_all[:, tb, :], in_=bank[5][:, 0:32]), reads=[("bank", 5)],
                      writes=["pos_all"])
            if "X1" in dbg:
                dump("X1", [NTOK, D], F32,
                     [((lambda o, r=r: o[r * 128:(r + 1) * 128, :]), (lambda r=r: X1d[r * 128:(r + 1) * 128, :])) for r in range(8)],
                     [("X1", tb) for tb in range(8)])
            if "G" in dbg:
                dump("G", [128, 8, 32], F32, [((lambda o: o), (lambda: G_all[:]))], ["G_all"])
                dump("pos", [128, 8, 32], F32, [((lambda o: o), (lambda: pos_all[:]))], ["pos_all"])

        if stage >= 6:
            A.top = P5keep
            NSC = CAP // 128
            iot = A.alloc([128, CAP], F32, "iot")
            tidf = A.alloc([128, 8, 5], F32, "tidf")
            Rb = A.alloc([128, 8, 5], BF16, "Rb")
            ghi = A.alloc([128, 8], BF16, "ghi")
            ghf = A.alloc([128, 8], F32, "ghf")
            sel = A.alloc([128, 8, CAP], BF16, "sel")
            XeT = A.alloc([128, 32, CAP], BF16, "XeT")
            gsT = A.alloc([128, 2, CAP], BF16, "gsT")
            actT = A.alloc([128, 12, CAP], BF16, "actT")
            gm = [A.alloc([128, CAP], F32, "gm%d" % i) for i in range(2)]
            sg = A.alloc([128, CAP], F32, "sg")
            bgu = [A.alloc([128, 24], F32, "bgu%d" % i) for i in range(2)]
            bd = [A.alloc([1, 512], BF16, "bd%d" % i) for i in range(2)]
            onesr = A.alloc([1, 128], BF16, "onesr")
            wd = [A.alloc([128, 12, 256], BF16, "wd%d" % i) for i in range(2)]
            Yt = [A.alloc([128, 512], F32, "Yt%d" % i) for i in range(2 * NSC)]
            inf = [A.alloc([128, 8], F32, "inf%d" % i) for i in range(NSC)]
            idx = [A.alloc([128, 1], I32, "idx%d" % i) for i in range(NSC)]
            zt = Yt[0]
            wbuf3 = A.alloc([128, 32, 256], BF16, "wbuf3")
            wd3 = A.alloc([128, 12, 256], BF16, "wd3")
            wbm = [wbuf[0], wbuf[1], wbuf3]
            wdm = [wd[0], wd[1], wd3]
            oldk = ["ggt", "a2t", "b2t", "xm", "xx", "h2T"]
            newk = ["iot", "tidf", "Rb", "ghi", "ghf", "XeT", ("gs", 0), ("gs", 1), ("gm", 0), ("gm", 1), "sg",
                    ("bgu", 0), ("bgu", 1), ("bd", 0), ("bd", 1), "onesr", ("wd", 0), ("wd", 1), ("wd", 2), ("wbuf", 2)] + \
                [("sel", tb) for tb in range(8)] + [("XeT", dc) for dc in range(32)] + [("act", f) for f in range(12)] + \
                [("Yt", i) for i in range(2 * NSC)] + [("inf", i) for i in range(NSC)] + [("idx", i) for i in range(NSC)]
            P.add("dve", lambda e: e.engine_nop(), writes=oldk + newk)
            P.add("sp", lambda e: e.dma_start(out=iot[:], in_=iota_d), writes=["iot"], dma="c0")
            P.add("sp", lambda e: e.dma_start(out=tidf[:], in_=tid_d.rearrange("p (b c) -> p b c", c=5)), writes=["tidf"], dma="c1")
            P.add("dve", lambda e: e.tensor_copy(out=Rb[:], in_=tidf[:]), reads=["tidf"], writes=["Rb"])
            P.add("dve", lambda e: e.memset(onesr[:], 1.0), writes=["onesr"])
            P.add("dve", lambda e: e.memset(zt[:], 0.0), writes=[("Yt", 0)])
            Fk = [("Fd", c) for c in range(8)]
            for tb in range(NTOK // 128 + 1):
                rows = 128 if tb < 8 else 1
                for hf in range(8):
                    P.add("sp", lambda e, tb=tb, hf=hf, rows=rows: e.dma_start(
                        out=Fd[hf][tb * 128:tb * 128 + rows, :], in_=zt[0:rows, :]), reads=[("Yt", 0)],
                        writes=[Fk[hf]], dma="fz")
            wch = 0
            wdc = 0
            pbc = 0
            yrot = 0
            pending = []

            def flush_pending():
                while pending:
                    sci, M, yi, c0, fk = pending.pop(0)
                    P.add("pool", lambda e, sci=sci, M=M, yi=yi, c0=c0: e.indirect_dma_start(
                        out=Fd[c0 // 512][:, :], out_offset=bass.IndirectOffsetOnAxis(ap=idx[sci][0:M, 0:1], axis=0),
                        in_=Yt[yi][0:M, :], in_offset=None, compute_op=ALU.add),
                        reads=[("Yt", yi), ("idx", sci)], writes=[("Fd", fk)], dma=("scat", yi))

            slots = [(c * 128, 128) for c in range(NSC)]
            for ex_ in range(nexp):
                eb = ex_ % 2
                P.add("sp", lambda e, ex_=ex_, eb=eb: e.dma_start(out=bgu[eb][:], in_=bguT_d[ex_]), writes=[("bgu", eb)],
                      dma=("bgu", eb))
                P.add("dve", lambda e, ex_=ex_: e.tensor_copy(out=ghi[:], in_=G_all[:, :, ex_]), reads=["G_all"], writes=["ghi"])
                P.add("dve", lambda e: e.tensor_copy(out=ghf[:], in_=ghi[:]), reads=["ghi"], writes=["ghf"])
                P.add("dve", lambda e, ex_=ex_: e.tensor_tensor(out=ghf[:], in0=G_all[:, :, ex_], in1=ghf[:], op=ALU.subtract),
                      reads=["G_all", "ghf"], writes=["ghf"])
                P.add("dve", lambda e: e.tensor_copy(out=Rb[:, :, 3], in_=ghi[:]), reads=["ghi"], writes=["Rb"])
                P.add("dve", lambda e: e.tensor_copy(out=Rb[:, :, 4], in_=ghf[:]), reads=["ghf"], writes=["Rb"])
                for tb in range(8):
                    P.add("dve", lambda e, tb=tb, ex_=ex_: e.tensor_scalar(
                        out=sel[:, tb, :], in0=iot[:], scalar1=pos_all[:, tb, ex_:ex_ + 1], scalar2=mkf_all[:, tb, ex_:ex_ + 1],
                        op0=ALU.is_equal, op1=ALU.mult), reads=["iot", "pos_all", "mkf_all"], writes=[("sel", tb)])
                wv_gu = w_gu[ex_].rearrange("(kc p) n -> p kc n", p=128)
                gu_cols = []
                for pc_ in range(6):
                    gu_cols.append(pc_ * 256)
                    gu_cols.append(1536 + pc_ * 256)
                wv_dn = w_dn[ex_].rearrange("(fc p) n -> p fc n", p=128)

                def load_gu(j):
                    nonlocal wch
                    b = wch % 3
                    wch += 1
                    c0 = gu_cols[j]
                    P.add("pool", lambda e, b=b, c0=c0, wv_gu=wv_gu: e.dma_start(out=wbm[b][:], in_=wv_gu[:, :, c0:c0 + 256]),
                          writes=[("wbuf", b)], dma=("wbuf", b))
                    return b

                def load_dn(dc):
                    nonlocal wdc
                    b2 = wdc % 3
                    wdc += 1
                    P.add("pool", lambda e, b2=b2, dc=dc, wv_dn=wv_dn: e.dma_start(out=wdm[b2][:], in_=wv_dn[:, :, dc * 256:(dc + 1) * 256]),
                          writes=[("wd", b2)], dma=("wd", b2))
                    return b2

                gq = [load_gu(0), load_gu(1)]
                flush_pending()
                for dc in range(32):
                    pb = pbc % 6
                    pbc += 1
                    for tb in range(8):
                        P.add("pe", lambda e, dc=dc, tb=tb, pb=pb: e.matmul(
                            bank[pb][:, 0:CAP], lhsT=h2_all[:, tb, dc * 128:(dc + 1) * 128], rhs=sel[:, tb, :],
                            start=(tb == 0), stop=(tb == 7)), reads=[("h2", tb), ("sel", tb)], writes=[("bank", pb)])
                    if dc % 2 == 0:
                        P.add("act", lambda e, dc=dc, pb=pb: e.copy(out=XeT[:, dc, :], in_=bank[pb][:, 0:CAP]),
                              reads=[("bank", pb)], writes=[("XeT", dc)])
                    else:
                        P.add("dve", lambda e, dc=dc, pb=pb: e.tensor_copy(out=XeT[:, dc, :], in_=bank[pb][:, 0:CAP]),
                              reads=[("bank", pb)], writes=[("XeT", dc)])
                for sci, (s0, M) in enumerate(slots):
                    ib = 6 + sci % 2
                    for tb in range(8):
                        P.add("pe", lambda e, tb=tb, s0=s0, M=M, ib=ib: e.matmul(
                            bank[ib][0:M, 0:5], lhsT=sel[:, tb, s0:s0 + M], rhs=Rb[:, tb, :], start=(tb == 0), stop=(tb == 7)),
                            reads=[("sel", tb), "Rb"], writes=[("bank", ib)])
                    P.add("dve", lambda e, sci=sci, M=M, ib=ib: e.tensor_copy(out=inf[sci][0:M, 0:5], in_=bank[ib][0:M, 0:5]),
                          reads=[("bank", ib)], writes=[("inf", sci)])
                    P.add("dve", lambda e, sci=sci, M=M: e.scalar_tensor_tensor(
                        out=inf[sci][0:M, 5:6], in0=inf[sci][0:M, 0:1], scalar=128.0, in1=inf[sci][0:M, 1:2], op0=ALU.mult, op1=ALU.add),
                        reads=[("inf", sci)], writes=[("inf", sci)])
                    P.add("dve", lambda e, sci=sci, M=M: e.tensor_scalar(
                        out=inf[sci][0:M, 6:7], in0=inf[sci][0:M, 2:3], scalar1=-float(NTOK), scalar2=float(NTOK), op0=ALU.mult,
                        op1=ALU.add), reads=[("inf", sci)], writes=[("inf", sci)])
                    P.add("dve", lambda e, sci=sci, M=M: e.tensor_tensor(out=inf[sci][0:M, 5:6], in0=inf[sci][0:M, 5:6],
                                                                         in1=inf[sci][0:M, 6:7], op=ALU.add),
                          reads=[("inf", sci)], writes=[("inf", sci)])
                    P.add("dve", lambda e, sci=sci, M=M: e.tensor_copy(out=idx[sci][0:M, :], in_=inf[sci][0:M, 5:6]),
                          reads=[("inf", sci)], writes=[("idx", sci)])
                    P.add("dve", lambda e, sci=sci, M=M: e.tensor_tensor(out=inf[sci][0:M, 7:8], in0=inf[sci][0:M, 3:4],
                                                                         in1=inf[sci][0:M, 4:5], op=ALU.add),
                          reads=[("inf", sci)], writes=[("inf", sci)])
                dq = []
                for j in range(12):
                    b = gq.pop(0)
                    if j + 2 < 12:
                        gq.append(load_gu(j + 2))
                    else:
                        dq.append(load_dn(j + 2 - 12))
                    isg = j % 2 == 0
                    for sub in range(2):
                        fcb = (gu_cols[j] + sub * 128) // 128
                        f2 = (j // 2) * 2 + sub
                        pb = pbc % 6
                        pbc += 1
                        for kc in range(32):
                            P.add("pe", lambda e, b=b, sub=sub, kc=kc, pb=pb: e.matmul(
                                bank[pb][:, 0:CAP], lhsT=wbm[b][:, kc, sub * 128:(sub + 1) * 128], rhs=XeT[:, kc, :],
                                start=(kc == 0), stop=(kc == 31)), reads=[("wbuf", b), ("XeT", kc)], writes=[("bank", pb)])
                        w_ = sub
                        P.add("dve", lambda e, pb=pb, w_=w_, fcb=fcb, eb=eb: e.tensor_scalar(
                            out=gm[w_][:], in0=bank[pb][:, 0:CAP], scalar1=bgu[eb][:, fcb:fcb + 1], scalar2=7.0,
                            op0=ALU.add, op1=ALU.min), reads=[("bank", pb), ("bgu", eb)], writes=[("gm", w_)])
                        if isg:
                            P.add("act", lambda e, w_=w_: e.activation(out=sg[:], in_=gm[w_][:], func=AF.Sigmoid, scale=1.702),
                                  reads=[("gm", w_)], writes=["sg"])
                            P.add("dve", lambda e, w_=w_, sub=sub: e.tensor_tensor(out=gsT[:, sub, :], in0=gm[w_][:], in1=sg[:],
                                                                                  op=ALU.mult),
                                  reads=[("gm", w_), "sg"], writes=[("gs", sub)])
                        else:
                            P.add("dve", lambda e, w_=w_: e.tensor_scalar(out=gm[w_][:], in0=gm[w_][:], scalar1=-7.0, scalar2=1.0,
                                                                          op0=ALU.max, op1=ALU.add),
                                  reads=[("gm", w_)], writes=[("gm", w_)])
                            P.add("dve", lambda e, w_=w_, f2=f2, sub=sub: e.tensor_tensor(out=actT[:, f2, :], in0=gm[w_][:],
                                                                                         in1=gsT[:, sub, :], op=ALU.mult),
                                  reads=[("gm", w_), ("gs", sub)], writes=[("act", f2)])
                for dc in range(16):
                    b2 = dq.pop(0)
                    if dc + 2 < 16:
                        dq.append(load_dn(dc + 2))
                    flush_pending()
                    half = dc % 2
                    if half == 0:
                        bb = (dc // 2) % 2
                        P.add("pool", lambda e, ex_=ex_, bb=bb, dc=dc: e.dma_start(out=bd[bb][:], in_=b_dn[ex_:ex_ + 1, dc * 256:dc * 256 + 512]),
                              writes=[("bd", bb)], dma=("bd", bb))
                        yset = yrot % 2
                        yrot += 1
                    for sci, (s0, M) in enumerate(slots):
                        pb = pbc % 6
                        pbc += 1
                        yi = yset * NSC + sci
                        for fc in range(12):
                            P.add("pe", lambda e, b2=b2, fc=fc, pb=pb, s0=s0, M=M: e.matmul(
                                bank[pb][0:M, 0:256], lhsT=actT[:, fc, s0:s0 + M], rhs=wdm[b2][:, fc, :], start=(fc == 0), stop=False),
                                reads=[("wd", b2), ("act", fc)], writes=[("bank", pb)])
                        P.add("pe", lambda e, pb=pb, M=M, half=half, bb=bb: e.matmul(
                            bank[pb][0:M, 0:256], lhsT=onesr[0:1, 0:M], rhs=bd[bb][0:1, half * 256:(half + 1) * 256], start=False,
                            stop=True), reads=[("bd", bb), "onesr"], writes=[("bank", pb)])
                        P.add("act", lambda e, sci=sci, M=M, pb=pb, half=half, yi=yi: e.activation(
                            out=Yt[yi][0:M, half * 256:(half + 1) * 256], in_=bank[pb][0:M, 0:256], func=AF.Copy,
                            scale=inf[sci][0:M, 7:8]), reads=[("bank", pb), ("inf", sci)], writes=[("Yt", yi)])
                        if half == 1:
                            c0 = (dc // 2) * 512
                            pending.append((sci, M, yi, c0, dc // 2))
            flush_pending()
            if "F" in dbg:
                dump("F", [NTOK, D], F32,
                     [((lambda o, c=c: o[:, c * 512:(c + 1) * 512]), (lambda c=c: Fd[c][0:NTOK, :])) for c in range(8)],
                     Fk)

        if stage >= 7:
            A.top = hT_off
            g2t = A.alloc([128, D], F32, "g2t")
            fm_ = A.alloc([128, D], F32, "fm_")
            x1t = A.alloc([128, D], F32, "x1t")
            jk = A.alloc([128, D], BF16, "jk")
            P.add("dve", lambda e: e.engine_nop(), writes=[("h2", tb) for tb in range(8)] + ["g2t", "fm_", "x1t", "jk"])
            P.add("sp", lambda e: e.dma_start(out=g2t[:], in_=BC[3]), reads=[("BC", 3, c0) for c0 in range(0, D, 256)],
                  writes=["g2t"], dma="c0")
            for tb in range(8):
                for c in range(8):
                    P.add("sp", lambda e, tb=tb, c=c: e.dma_start(out=fm_[:, c * 512:(c + 1) * 512], in_=Fd[c][tb * 128:(tb + 1) * 128, :]),
                          reads=[("Fd", c)], writes=["fm_"], dma=("fm_", c))
                P.add("sp", lambda e, tb=tb: e.dma_start(out=x1t[:], in_=X1d[tb * 128:(tb + 1) * 128, :]), reads=[("X1", tb)],
                      writes=["x1t"], dma="x1t")
                P.add("act", lambda e: e.activation(out=jk[:], in_=fm_[:], func=AF.Square, accum_out=small[:, 20:21]),
                      reads=["fm_"], writes=["jk", "s20"])
                P.add("act", lambda e: e.activation(out=small[:, 21:22], in_=small[:, 20:21], func=AF.Sqrt, scale=1.0 / D,
                                                    bias=1e-6), reads=["s20"], writes=["s21"])
                P.add("dve", lambda e: e.reciprocal(out=small[:, 22:23], in_=small[:, 21:22]), reads=["s21"], writes=["s22"])
                P.add("dve", lambda e: e.scalar_tensor_tensor(out=fm_[:], in0=fm_[:], scalar=small[:, 22:23], in1=g2t[:],
                                                              op0=ALU.mult, op1=ALU.mult),
                      reads=["fm_", "s22", "g2t"], writes=["fm_"])
                P.add("dve", lambda e: e.tensor_tensor(out=x1t[:], in0=x1t[:], in1=fm_[:], op=ALU.add), reads=["x1t", "fm_"],
                      writes=["x1t"])
                P.add("sp", lambda e, tb=tb: e.dma_start(out=out_d[tb * 128:(tb + 1) * 128, :], in_=x1t[:]), reads=["x1t"],
                      writes=[("out", tb)], dma="outs")
                out_keys.append(("out", tb))

        if stage < 3:
            zt = A.alloc([128, D], F32, "zt")
            P.add("pool", lambda e: e.memset(zt[:], 0.0),
                  writes=["zt", "xt", "xs", ("stg", 0), ("stg", 1), ("stgk", 0), ("stgk", 1), ("stgv", 0), ("stgv", 1)])
            for tb in range(8):
                P.add("sp", lambda e, tb=tb: e.dma_start(out=out_d[tb * 128:(tb + 1) * 128, :], in_=zt[:]), reads=["zt"],
                      writes=[("out", tb)], dma=("out", tb % 2))
                out_keys.append(("out", tb))
        P.add("sp", lambda e: e.nop(), reads=out_keys)
        P.emit(nc)
    return nc, dbg_out


def _consts():
    i = np.arange(128)
    ones = np.ones((128, 128), np.float32)
    ntri = -(i[:, None] > i[None, :]).astype(np.float32)
    maskT = (i[None, :] > i[:, None]).astype(np.float32)
    triS = (i[:, None] < i[None, :]).astype(np.float32)
    return np.ascontiguousarray(np.stack([ones, ntri, maskT, -ones, triS, ones]))


def _tid():
    t = np.zeros((128, 8, 5), np.float32)
    t[:, :, 0] = np.arange(8, dtype=np.float32)[None, :]
    t[:, :, 1] = np.arange(128, dtype=np.float32)[:, None]
    t[:, :, 2] = 1.0
    return np.ascontiguousarray(t.reshape(128, 40))


def prep_inputs(inp):
    f = lambda a: np.ascontiguousarray(np.asarray(a, dtype=np.float32))
    x = f(inp["x"])
    c = f(inp["c"])
    b_ada = f(inp["b_ada"])[0]
    fm = lambda v: np.ascontiguousarray(v.reshape(-1, 128).T)
    bc = lambda v: np.broadcast_to(v[None, :], (128, v.shape[0]))
    vec_bc = np.ascontiguousarray(np.stack([
        bc(b_ada[2 * D:3 * D]), bc(b_ada[3 * D:4 * D]), bc(b_ada[4 * D:5 * D]), bc(b_ada[5 * D:6 * D]),
        bc(f(inp["g_post_mix"])[0]), bc(f(inp["g_pre_ffn"])[0]), bc(f(inp["g_post_ffn"])[0])]))
    shared = {
        "w_ada": f(inp["w_ada"])[0],
        "b_adaT": fm(b_ada[:2 * D]),
        "vec_bc": vec_bc,
        "gpreT": fm(f(inp["g_pre_mix"])[0]),
        "w_in": f(inp["w_in"])[0],
        "w_out": f(inp["w_out"])[0],
        "bgluT": fm(f(inp["b_glu"])[0]),
        "cwT": np.ascontiguousarray(f(inp["conv_w"])[0][:, 0, :].reshape(31, 16, 128).transpose(2, 1, 0).reshape(128, 16 * 31)),
        "cvec": np.ascontiguousarray(np.concatenate([fm(f(inp["conv_b"])[0]), fm(f(inp["conv_ln_g"])[0]),
                                                     fm(f(inp["conv_ln_b"])[0])], 1)),
        "consts": _consts(),
        "w_router": f(inp["w_router"])[0],
        "brt": np.ascontiguousarray(bc(f(inp["b_router"])[0])),
        "iota": np.ascontiguousarray(np.broadcast_to(np.arange(CAP, dtype=np.float32)[None, :], (128, CAP))),
        "tid": _tid(),
        "w_gu": f(inp["w_gate_up"])[0],
        "bguT": np.ascontiguousarray(f(inp["b_gate_up"])[0].reshape(32, 24, 128).transpose(0, 2, 1)),
        "w_dn": f(inp["w_down"])[0],
        "b_dn": f(inp["b_down"])[0],
    }
    zeros = np.zeros((NTOK, D), np.float32)
    maps = []
    for r in range(8):
        b, half = r // 2, r % 2
        m = dict(shared)
        m["xo"] = np.ascontiguousarray(x[b, half * NTOK:(half + 1) * NTOK])
        m["xp"] = np.ascontiguousarray(x[b, 0:NTOK]) if half == 1 else zeros
        m["flag"] = np.full((128, 1), float(half), np.float32)
        m["cT"] = fm(c[b])
        maps.append(m)
    return maps


def kernel(**inputs):
    nc, _ = build()
    maps = prep_inputs(inputs)
    res = run_bass_kernel_spmd(nc, maps, core_ids=list(range(8)))
    out = np.empty((4, 2048, D), np.float32)
    for r in range(8):
        b, half = r // 2, r % 2
        out[b, half * NTOK:(half + 1) * NTOK] = res.results[r]["out"]
    return out
```

```python
import numpy as np
import concourse.bass as bass
import concourse.mybir as mybir
from concourse.bass_utils import run_bass_kernel_spmd
from contextlib import ExitStack

F32 = mybir.dt.float32
BF16 = mybir.dt.bfloat16
I32 = mybir.dt.int32
AF = mybir.ActivationFunctionType
ALU = mybir.AluOpType

ENGS = ["pe", "act", "dve", "pool", "sp"]
TRUST_SAME = {"pe": True, "act": False, "dve": False, "pool": False, "sp": True}


class Op:
    __slots__ = ("eng", "fn", "deps", "signal", "sigval", "dma_key", "dma_val")

    def __init__(self, eng, fn):
        self.eng = eng
        self.fn = fn
        self.deps = ()
        self.signal = False
        self.sigval = 0
        self.dma_key = None
        self.dma_val = 0


class Prog:
    def __init__(self):
        self.ops = {e: [] for e in ENGS}
        self.last_w = {}
        self.readers = {}
        self.dma_cnt = {}

    def add(self, eng, fn, reads=(), writes=(), dma=None):
        op = Op(eng, fn)
        deps = set()
        for r in reads:
            lw = self.last_w.get(r)
            if lw is not None:
                deps.add(lw)
        for w in writes:
            lw = self.last_w.get(w)
            if lw is not None:
                deps.add(lw)
            rs = self.readers.get(w)
            if rs:
                deps.update(rs)
        op.deps = tuple(deps)
        for r in reads:
            self.readers.setdefault(r, []).append(op)
        for w in writes:
            self.last_w[w] = op
            self.readers[w] = []
        if dma is not None:
            op.dma_key = dma
            self.dma_cnt[dma] = self.dma_cnt.get(dma, 0) + 16
            op.dma_val = self.dma_cnt[dma]
        self.ops[eng].append(op)
        return op

    def emit(self, nc):
        for e in ENGS:
            for op in self.ops[e]:
                for d in op.deps:
                    if d.dma_key is None:
                        if d.eng == op.eng and TRUST_SAME[e]:
                            continue
                        d.signal = True
        for e in ENGS:
            c = 0
            for op in self.ops[e]:
                if op.signal and op.dma_key is None:
                    c += 1
                    op.sigval = c
        with ExitStack() as st:
            sem = {}
            for e in ENGS:
                sem[("eng", e)] = st.enter_context(nc.semaphore("s_" + e))
            for i, k in enumerate(self.dma_cnt):
                sem[("dma", k)] = st.enter_context(nc.semaphore("d%d" % i))
            block = st.enter_context(nc.Block())

            def run(e, eng):
                waited = {}
                for op in self.ops[e]:
                    need = {}
                    for d in op.deps:
                        if d.dma_key is not None:
                            k = ("dma", d.dma_key)
                            v = d.dma_val
                        else:
                            if d.eng == e and TRUST_SAME[e]:
                                continue
                            k = ("eng", d.eng)
                            v = d.sigval
                        if need.get(k, 0) < v:
                            need[k] = v
                    for k, v in need.items():
                        if waited.get(k, 0) < v:
                            eng.wait_ge(sem[k], v)
                            waited[k] = v
                    inst = op.fn(eng)
                    if op.dma_key is not None:
                        inst.then_inc(sem[("dma", op.dma_key)], 16)
                    elif op.signal:
                        inst.then_inc(sem[("eng", e)], 1)

            block.tensor(lambda eng: run("pe", eng))
            block.scalar(lambda eng: run("act", eng))
            block.vector(lambda eng: run("dve", eng))
            block.gpsimd(lambda eng: run("pool", eng))
            block.sync(lambda eng: run("sp", eng))


SB_BASE = 16512 + 2048
SB_END = 229344


class Arena:
    def __init__(self, nc):
        self.nc = nc
        self.top = SB_BASE
        self.n = 0

    def alloc(self, shape, dt, name=None):
        nbytes = int(np.prod(shape[1:])) * (4 if dt in (F32, I32) else 2)
        nbytes = (nbytes + 31) // 32 * 32
        off = self.top
        assert off + nbytes <= SB_END, ("SBUF overflow", name, off + nbytes - SB_END)
        self.top += nbytes
        self.n += 1
        return self.nc.alloc_sbuf_tensor_at("%s_%d" % (name or "t", self.n), list(shape), dt, offset=off)


NTOK = 1024
D = 4096
DH_SCALE = 128 ** -0.5
HALO = 128
VGW = NTOK + HALO
CAP = 384


def build(stage=99, dbg=(), nada=96, ntb=16, nexp=32):
    nc = bass.Bass("TRN2", target_bir_lowering=False)
    di = lambda name, shape, dt=F32: nc.dram_tensor(name, list(shape), dt, kind="ExternalInput").ap()
    ds = lambda name, shape, dt=F32: nc.dram_tensor(name, list(shape), dt).ap()
    xo = di("xo", [NTOK, D])
    xp = di("xp", [NTOK, D])
    flag_d = di("flag", [128, 1])
    cT_d = di("cT", [128, 32])
    w_ada = di("w_ada", [D, 6 * D])
    b_adaT_d = di("b_adaT", [128, 64])
    vec_bc = di("vec_bc", [7, 128, D])
    gpreT_d = di("gpreT", [128, 32])
    w_in = di("w_in", [D, 10240])
    out_d = nc.dram_tensor("out", [NTOK, D], F32, kind="ExternalOutput").ap()
    w_out = di("w_out", [D, D])
    bgluT_d = di("bgluT", [128, 32])
    cwT_d = di("cwT", [128, 16 * 31])
    cvec_d = di("cvec", [128, 48])
    consts_d = di("consts", [6, 128, 128])
    w_router = di("w_router", [D, 32])
    brt_d = di("brt", [128, 32])
    iota_d = di("iota", [128, CAP])
    tid_d = di("tid", [128, 8 * 5])
    w_gu = di("w_gu", [32, D, 3072]) if stage >= 6 else None
    bguT_d = di("bguT", [32, 128, 24])
    w_dn = di("w_dn", [32, 1536, D]) if stage >= 6 else None
    b_dn = di("b_dn", [32, D])
    Md = ds("Md", [NTOK, D])
    X1d = ds("X1d", [NTOK, D])
    Fd = [ds("Fd%d" % c, [NTOK + 1, 512]) for c in range(8)]

    BC = ds("BC", [4, 128, D])
    VG = ds("VG", [D, VGW])
    QT = ds("QT", [16, 128, NTOK], BF16)
    KT = ds("KT", [16, 128, 2 * NTOK], BF16)
    Vd = ds("Vd", [2 * NTOK, 2048], BF16)

    dbg_out = {}

    def dbg_tensor(name, shape, dt):
        dbg_out[name] = nc.dram_tensor("dbg_" + name, list(shape), dt, kind="ExternalOutput").ap()
        return dbg_out[name]

    P = Prog()
    A = Arena(nc)
    out_keys = []
    with ExitStack() as st:
        bank = [st.enter_context(nc.psum_tensor("bank%d" % i, [128, 512], F32)) for i in range(8)]

        ident = A.alloc([128, 128], BF16, "ident")
        flag = A.alloc([128, 1], F32, "flag")
        cT = A.alloc([128, 32], F32, "cT")
        sc = A.alloc([128, 32], BF16, "sc")
        b_adaT = A.alloc([128, 64], F32, "b_adaT")
        gpreT = A.alloc([128, 32], F32, "gpreT")
        modT = A.alloc([128, 64], F32, "modT")
        A1 = A.alloc([128, 32], F32, "A1")
        small = A.alloc([128, 64], F32, "small")
        wbuf = [A.alloc([128, 32, 256], BF16, "wbuf%d" % i) for i in range(2)]
        hT_off = A.top
        hT = A.alloc([128, 32, 2 * NTOK], BF16, "hT")
        regionT = A.top

        P.add("pool", lambda e: e.memset(ident[:], 1.0), writes=["ident"])
        P.add("pool", lambda e: e.affine_select(out=ident[:], in_=ident[:], pattern=[[-1, 128]],
                                                compare_op=ALU.is_equal, fill=0.0, base=0, channel_multiplier=1),
              reads=["ident"], writes=["ident"])
        P.add("sp", lambda e: e.dma_start(out=flag[:], in_=flag_d), writes=["flag"], dma="c0")
        P.add("sp", lambda e: e.dma_start(out=cT[:], in_=cT_d), writes=["cT"], dma="c1")
        P.add("sp", lambda e: e.dma_start(out=b_adaT[:], in_=b_adaT_d), writes=["b_adaT"], dma="c2")
        P.add("sp", lambda e: e.dma_start(out=gpreT[:], in_=gpreT_d), writes=["gpreT"], dma="c3")

        screp = A.alloc([128, 32, 128], BF16, "screp")
        bch = [A.alloc([128, 256], F32, "bch%d" % i) for i in range(2)]
        gch = [A.alloc([128, 256], F32, "gch%d" % i) for i in range(2)]
        och = [A.alloc([128, 256], F32, "och%d" % i) for i in range(2)]
        tch = [A.alloc([128, 256], F32, "tch%d" % i) for i in range(2)]
        P.add("act", lambda e: e.activation(out=sc[:], in_=cT[:], func=AF.Silu), reads=["cT"], writes=["sc"])
        P.add("dve", lambda e: e.tensor_copy(out=screp[:], in_=sc[:].unsqueeze(2).to_broadcast([128, 32, 128])),
              reads=["sc"], writes=["screp"])
        wv_ada = w_ada.rearrange("(kc p) n -> p kc n", p=128)
        NCH_ADA = nada
        for ci in range(NCH_ADA):
            b = ci % 2
            P.add("pool", lambda e, b=b, ci=ci: e.dma_start(out=wbuf[b][:], in_=wv_ada[:, :, ci * 256:(ci + 1) * 256]),
                  writes=[("wbuf", b)], dma=("wbuf", b))
            if ci < 32:
                for sub in range(2):
                    cc = ci * 2 + sub
                    for kd in range(32):
                        P.add("pe", lambda e, b=b, sub=sub, kd=kd, cc=cc: e.matmul(
                            bank[0][:, cc:cc + 1], lhsT=wbuf[b][:, kd, sub * 128:(sub + 1) * 128], rhs=sc[:, kd:kd + 1],
                            start=(kd == 0), stop=(kd == 31)),
                            reads=[("wbuf", b), "sc"], writes=["bank0"])
                if ci == 31:
                    P.add("dve", lambda e: e.tensor_tensor(out=modT[:], in0=bank[0][:, 0:64], in1=b_adaT[:], op=ALU.add),
                          reads=["bank0", "b_adaT"], writes=["modT"])
                    P.add("dve", lambda e: e.scalar_tensor_tensor(out=A1[:], in0=modT[:, 32:64], scalar=1.0, in1=gpreT[:],
                                                                  op0=ALU.add, op1=ALU.mult),
                          reads=["modT", "gpreT"], writes=["A1"])
            else:
                j = (ci - 32) // 16
                c0 = ((ci - 32) % 16) * 256
                pb = 1 + (ci % 2)
                for kd in range(32):
                    P.add("pe", lambda e, b=b, kd=kd, pb=pb: e.matmul(
                        bank[pb][:, 0:256], lhsT=screp[:, kd, :], rhs=wbuf[b][:, kd, :],
                        start=(kd == 0), stop=(kd == 31)),
                        reads=[("wbuf", b), "screp"], writes=[("bank", pb)])
                P.add("sp", lambda e, b=b, j=j, c0=c0: e.dma_start(out=bch[b][:], in_=vec_bc[j, :, c0:c0 + 256]),
                      writes=[("bch", b)], dma=("bch", b))
                if j != 1:
                    gi = {0: 4, 2: 5, 3: 6}[j]
                    P.add("sp", lambda e, b=b, gi=gi, c0=c0: e.dma_start(out=gch[b][:], in_=vec_bc[gi, :, c0:c0 + 256]),
                          writes=[("gch", b)], dma=("gch", b))
                if j == 1:
                    P.add("dve", lambda e, b=b, pb=pb: e.tensor_tensor(out=och[b][:], in0=bank[pb][:, 0:256], in1=bch[b][:],
                                                                      op=ALU.add),
                          reads=[("bank", pb), ("bch", b)], writes=[("och", b)])
                else:
                    addc = 1.0 if j == 2 else 0.0
                    P.add("dve", lambda e, b=b, pb=pb, addc=addc: e.scalar_tensor_tensor(
                        out=tch[b][:], in0=bank[pb][:, 0:256], scalar=addc, in1=bch[b][:], op0=ALU.add, op1=ALU.add),
                        reads=[("bank", pb), ("bch", b)], writes=[("tch", b)])
                    P.add("dve", lambda e, b=b: e.tensor_tensor(out=och[b][:], in0=tch[b][:], in1=gch[b][:], op=ALU.mult),
                          reads=[("tch", b), ("gch", b)], writes=[("och", b)])
                P.add("sp", lambda e, b=b, j=j, c0=c0: e.dma_start(out=BC[j, :, c0:c0 + 256], in_=och[b][:]),
                      reads=[("och", b)], writes=[("BC", j, c0)], dma=("och", b))
        A.top = regionT

        xt = A.alloc([128, D], F32, "xt")
        xs = A.alloc([128, D], BF16, "xs")
        P.add("dve", lambda e: e.engine_nop(),
              writes=[("bch", 0), ("bch", 1), ("gch", 0), ("gch", 1), ("och", 0), ("och", 1), ("tch", 0), ("tch", 1), "screp",
                      "xt", "xs"])
        ev = 0
        for tb in range(ntb):
            src = xp if tb < 8 else xo
            r0 = (tb % 8) * 128
            P.add("sp", lambda e, src=src, r0=r0: e.dma_start(out=xt[:], in_=src[r0:r0 + 128, :]), writes=["xt"], dma="xt")
            P.add("act", lambda e: e.activation(out=xs[:], in_=xt[:], func=AF.Square, accum_out=small[:, 0:1]),
                  reads=["xt"], writes=["xs", "ss"])
            P.add("act", lambda e: e.activation(out=small[:, 1:2], in_=small[:, 0:1], func=AF.Sqrt, scale=1.0 / D, bias=1e-6),
                  reads=["ss"], writes=["sq"])
            P.add("dve", lambda e: e.reciprocal(out=small[:, 2:3], in_=small[:, 1:2]), reads=["sq"], writes=["rstd"])
            P.add("act", lambda e: e.activation(out=xs[:], in_=xt[:], func=AF.Copy, scale=small[:, 2:3]),
                  reads=["xt", "rstd"], writes=["xs"])
            for kc in range(32):
                pb = 3 + kc % 4
                slot = 0
                pt = bank[pb][:, 0:64].bitcast(BF16)
                P.add("pe", lambda e, kc=kc, pt=pt: e.transpose(out=pt, in_=xs[:, kc * 128:(kc + 1) * 128], identity=ident[:]),
                      reads=["xs", "ident"], writes=[("pt", pb, slot)])
                dst = hT[:, kc, tb * 128:(tb + 1) * 128]
                if ev % 2 == 0:
                    P.add("act", lambda e, kc=kc, pt=pt, dst=dst: e.activation(
                        out=dst, in_=pt, func=AF.Identity, scale=A1[:, kc:kc + 1], bias=modT[:, kc:kc + 1]),
                        reads=[("pt", pb, slot), "A1", "modT"], writes=[("hT", tb)])
                else:
                    P.add("dve", lambda e, kc=kc, pt=pt, dst=dst: e.tensor_scalar(
                        out=dst, in0=pt, scalar1=A1[:, kc:kc + 1], scalar2=modT[:, kc:kc + 1], op0=ALU.mult, op1=ALU.add),
                        reads=[("pt", pb, slot), "A1", "modT"], writes=[("hT", tb)])
                ev += 1
        A.top = regionT

        def dump(nm, shp, dt, parts, keys):
            o = dbg_tensor(nm, shp, dt)
            for i, (dst_fn, src_fn) in enumerate(parts):
                P.add("sp", lambda e, o=o, dst_fn=dst_fn, src_fn=src_fn: e.dma_start(out=dst_fn(o), in_=src_fn()),
                      reads=keys, writes=[("dbg_" + nm, i)], dma=("dbg", i % 2))
                out_keys.append(("dbg_" + nm, i))

        if "modT" in dbg:
            dump("modT", [128, 64], F32, [((lambda o: o), (lambda: modT[:]))], ["modT"])
        if "hT" in dbg:
            dump("hT", [128, 32, 2 * NTOK], BF16,
                 [((lambda o, kc=kc: o[:, kc, :]), (lambda kc=kc: hT[:, kc, :])) for kc in range(32)],
                 [("hT", tb) for tb in range(16)])
        if "BC" in dbg:
            dump("BC", [4, 128, D], F32,
                 [((lambda o, j=j: o[j]), (lambda j=j: BC[j])) for j in range(4)],
                 [("BC", j, c0) for j in range(4) for c0 in range(0, D, 256)])

        if stage >= 2:
            stg = [A.alloc([128, VGW], F32, "stg%d" % i) for i in range(2)]
            stgk = [A.alloc([128, 2 * NTOK], BF16, "stgk%d" % i) for i in range(2)]
            stgv = [A.alloc([128, 256], BF16, "stgv%d" % i) for i in range(2)]
            P.add("dve", lambda e: e.engine_nop(),
                  writes=["xt", "xs", ("stg", 0), ("stg", 1), ("stgk", 0), ("stgk", 1), ("stgv", 0), ("stgv", 1)])
            wv_in = w_in.rearrange("(kc p) n -> p kc n", p=128)
            allh = [("hT", tb) for tb in range(16)]
            pbc = 0
            sg = 0
            sgk = 0
            sgv = 0
            ev = 0

            def evac(dst, srcp, rk, wk, scale=None):
                nonlocal ev
                if ev % 2 == 0:
                    if scale is None:
                        P.add("act", lambda e: e.copy(out=dst, in_=srcp), reads=rk, writes=wk)
                    elif isinstance(scale, float):
                        P.add("act", lambda e: e.activation(out=dst, in_=srcp, func=AF.Copy, scale=scale), reads=rk, writes=wk)
                    else:
                        P.add("act", lambda e: e.activation(out=dst, in_=srcp, func=AF.Copy, scale=scale),
                              reads=rk + ["flag"], writes=wk)
                else:
                    if scale is None:
                        P.add("dve", lambda e: e.tensor_copy(out=dst, in_=srcp), reads=rk, writes=wk)
                    elif isinstance(scale, float):
                        P.add("dve", lambda e: e.tensor_single_scalar(out=dst, in_=srcp, scalar=scale, op=ALU.mult),
                              reads=rk, writes=wk)
                    else:
                        P.add("dve", lambda e: e.tensor_scalar(out=dst, in0=srcp, scalar1=scale, scalar2=None, op0=ALU.mult),
                              reads=rk + ["flag"], writes=wk)
                ev += 1

            for ci in range(40):
                b = ci % 2
                P.add("pool", lambda e, b=b, ci=ci: e.dma_start(out=wbuf[b][:], in_=wv_in[:, :, ci * 256:(ci + 1) * 256]),
                      writes=[("wbuf", b)], dma=("wbuf", b))
                kind = ci // 8
                if kind <= 1:
                    for sub in range(2):
                        s_ = sg % 2
                        sg += 1
                        row0 = ci * 256 + sub * 128
                        for (t0, n, o0) in ((NTOK - HALO, HALO, 0), (NTOK, 512, HALO), (NTOK + 512, 512, HALO + 512)):
                            pb = pbc % 8
                            pbc += 1
                            for kc in range(32):
                                P.add("pe", lambda e, b=b, sub=sub, kc=kc, pb=pb, t0=t0, n=n: e.matmul(
                                    bank[pb][:, 0:n], lhsT=wbuf[b][:, kc, sub * 128:(sub + 1) * 128], rhs=hT[:, kc, t0:t0 + n],
                                    start=(kc == 0), stop=(kc == 31)),
                                    reads=[("wbuf", b)] + allh, writes=[("bank", pb)])
                            evac(stg[s_][:, o0:o0 + n], bank[pb][:, 0:n], [("bank", pb)], [("stg", s_)])
                        P.add("sp", lambda e, s_=s_, row0=row0: e.dma_start(out=VG[row0:row0 + 128, :], in_=stg[s_][:]),
                              reads=[("stg", s_)], writes=[("VG", row0)], dma=("stg", s_))
                elif kind <= 3:
                    isq = kind == 2
                    for sub in range(2):
                        head = (ci % 8) * 2 + sub
                        s_ = sgk % 2
                        sgk += 1
                        groups = ((NTOK, 0), (NTOK + 512, 512)) if isq else ((0, 0), (512, 512), (1024, 1024), (1536, 1536))
                        for (t0, o0) in groups:
                            pb = pbc % 8
                            pbc += 1
                            for kc in range(32):
                                P.add("pe", lambda e, b=b, sub=sub, kc=kc, pb=pb, t0=t0: e.matmul(
                                    bank[pb][:, 0:512], lhsT=wbuf[b][:, kc, sub * 128:(sub + 1) * 128], rhs=hT[:, kc, t0:t0 + 512],
                                    start=(kc == 0), stop=(kc == 31)),
                                    reads=[("wbuf", b)] + allh, writes=[("bank", pb)])
                            evac(stgk[s_][:, o0:o0 + 512], bank[pb][:, 0:512], [("bank", pb)], [("stgk", s_)],
                                 scale=(DH_SCALE if isq else None))
                        if isq:
                            P.add("sp", lambda e, s_=s_, head=head: e.dma_start(out=QT[head], in_=stgk[s_][:, 0:NTOK]),
                                  reads=[("stgk", s_)], writes=[("QT", head)], dma=("stgk", s_))
                        else:
                            P.add("sp", lambda e, s_=s_, head=head: e.dma_start(out=KT[head], in_=stgk[s_][:]),
                                  reads=[("stgk", s_)], writes=[("KT", head)], dma=("stgk", s_))
                else:
                    c0 = (ci - 32) * 256
                    for tb in range(16):
                        pb = pbc % 8
                        pbc += 1
                        s_ = sgv % 2
                        sgv += 1
                        for kc in range(32):
                            P.add("pe", lambda e, b=b, kc=kc, pb=pb, tb=tb: e.matmul(
                                bank[pb][:, 0:256], lhsT=hT[:, kc, tb * 128:(tb + 1) * 128], rhs=wbuf[b][:, kc, :],
                                start=(kc == 0), stop=(kc == 31)),
                                reads=[("wbuf", b), ("hT", tb)], writes=[("bank", pb)])
                        evac(stgv[s_][:], bank[pb][:, 0:256], [("bank", pb)], [("stgv", s_)],
                             scale=(flag[:, 0:1] if tb < 8 else None))
                        P.add("sp", lambda e, s_=s_, tb=tb, c0=c0: e.dma_start(out=Vd[tb * 128:(tb + 1) * 128, c0:c0 + 256],
                                                                             in_=stgv[s_][:]),
                              reads=[("stgv", s_)], writes=[("Vd", tb, c0)], dma=("stgv", s_))
            A.top = regionT
            if "VG" in dbg:
                dump("VG", [D, VGW], F32,
                     [((lambda o, r=r: o[r * 128:(r + 1) * 128, :]), (lambda r=r: VG[r * 128:(r + 1) * 128, :])) for r in range(32)],
                     [("VG", r) for r in range(0, D, 128)])
            if "QT" in dbg:
                dump("QT", [16, 128, NTOK], BF16, [((lambda o, h=h: o[h]), (lambda h=h: QT[h])) for h in range(16)],
                     [("QT", h) for h in range(16)])
            if "KT" in dbg:
                dump("KT", [16, 128, 2 * NTOK], BF16, [((lambda o, h=h: o[h]), (lambda h=h: KT[h])) for h in range(16)],
                     [("KT", h) for h in range(16)])
            if "Vd" in dbg:
                dump("Vd", [2 * NTOK, 2048], BF16,
                     [((lambda o, r=r: o[r * 128:(r + 1) * 128, :]), (lambda r=r: Vd[r * 128:(r + 1) * 128, :])) for r in range(16)],
                     [("Vd", tb, c0) for tb in range(16) for c0 in range(0, 2048, 256)])

        if stage >= 3:
            A.top = hT_off
            oldk = [("hT", tb) for tb in range(16)] + [("stg", 0), ("stg", 1), ("stgk", 0), ("stgk", 1), ("stgv", 0),
                                                      ("stgv", 1), "xt", "xs"]
            mixT = A.alloc([128, 32, NTOK], BF16, "mixT")
            bgluT = A.alloc([128, 32], F32, "bgluT")
            cwT = A.alloc([128, 16 * 31], F32, "cwT")
            cvec = A.alloc([128, 48], F32, "cvec")
            cst = A.alloc([128, 6, 128], F32, "cst")
            cstb = A.alloc([128, 6, 128], BF16, "cstb")
            P3top = A.top
            cv_all = A.alloc([128, 16, NTOK], F32, "cv_all")
            vt = [A.alloc([128, VGW], F32, "vt%d" % i) for i in range(2)]
            gt = [A.alloc([128, VGW], F32, "gt%d" % i) for i in range(2)]
            ut = A.alloc([128, VGW], F32, "ut")
            sq = A.alloc([128, NTOK], F32, "sq")
            mixk = [("mix", c) for c in range(32)]
            newk = mixk + ["bgluT", "cwT", "cvec", "cst", "cstb", "ut", "sq", ("vt", 0), ("vt", 1), ("gt", 0), ("gt", 1)] + \
                [("cv", g) for g in range(16)]
            P.add("dve", lambda e: e.engine_nop(), writes=oldk + newk)
            P.add("sp", lambda e: e.dma_start(out=bgluT[:], in_=bgluT_d), writes=["bgluT"], dma="c0")
            P.add("sp", lambda e: e.dma_start(out=cwT[:], in_=cwT_d), writes=["cwT"], dma="c1")
            P.add("sp", lambda e: e.dma_start(out=cvec[:], in_=cvec_d), writes=["cvec"], dma="c2")
            P.add("sp", lambda e: e.dma_start(out=cst[:], in_=consts_d.rearrange("c p n -> p c n")), writes=["cst"], dma="c3")
            P.add("dve", lambda e: e.tensor_copy(out=cstb[:], in_=cst[:]), reads=["cst"], writes=["cstb"])
            ones_f = cst[:, 0, :]
            ntri_f = cst[:, 1, :]
            maskT_f = cst[:, 2, :]
            nones_f = cst[:, 3, :]
            triS_b = cstb[:, 4, :]
            ones_b = cstb[:, 0, :]
            for g in range(16):
                i = g % 2
                P.add("sp", lambda e, g=g, i=i: e.dma_start(out=vt[i][:], in_=VG[g * 128:(g + 1) * 128, :]),
                      reads=[("VG", g * 128)], writes=[("vt", i)], dma=("vt", i))
                P.add("sp", lambda e, g=g, i=i: e.dma_start(out=gt[i][:], in_=VG[2048 + g * 128:2048 + (g + 1) * 128, :]),
                      reads=[("VG", 2048 + g * 128)], writes=[("gt", i)], dma=("gt", i))
                P.add("act", lambda e, g=g, i=i: e.activation(out=gt[i][:], in_=gt[i][:], func=AF.Sigmoid,
                                                              bias=bgluT[:, 16 + g:17 + g]),
                      reads=[("gt", i), "bgluT"], writes=[("gt", i)])
                P.add("dve", lambda e, g=g, i=i: e.scalar_tensor_tensor(out=ut[:], in0=vt[i][:], scalar=bgluT[:, g:g + 1],
                                                                        in1=gt[i][:], op0=ALU.add, op1=ALU.mult),
                      reads=[("vt", i), ("gt", i), "bgluT"], writes=["ut"])
                P.add("dve", lambda e: e.tensor_scalar(out=ut[:, 0:HALO], in0=ut[:, 0:HALO], scalar1=flag[:, 0:1], scalar2=None,
                                                       op0=ALU.mult), reads=["ut", "flag"], writes=["ut"])
                o0 = HALO - 30
                P.add("dve", lambda e, g=g, o0=o0: e.tensor_scalar(out=cv_all[:, g, :], in0=ut[:, o0:o0 + NTOK],
                                                                   scalar1=cwT[:, g * 31:g * 31 + 1], scalar2=cvec[:, g:g + 1],
                                                                   op0=ALU.mult, op1=ALU.add),
                      reads=["ut", "cwT", "cvec"], writes=[("cv", g)])
                for j in range(1, 31):
                    P.add("dve", lambda e, g=g, j=j, o0=o0: e.scalar_tensor_tensor(
                        out=cv_all[:, g, :], in0=ut[:, o0 + j:o0 + j + NTOK], scalar=cwT[:, g * 31 + j:g * 31 + j + 1],
                        in1=cv_all[:, g, :], op0=ALU.mult, op1=ALU.add),
                        reads=["ut", "cwT", ("cv", g)], writes=[("cv", g)])
                P.add("act", lambda e, g=g: e.activation(out=sq[:], in_=cv_all[:, g, :], func=AF.Square),
                      reads=[("cv", g)], writes=["sq"])
                for hf in range(2):
                    P.add("pe", lambda e, g=g, hf=hf: e.matmul(bank[hf][:, 0:512], lhsT=ones_f,
                                                               rhs=cv_all[:, g, hf * 512:(hf + 1) * 512],
                                                               start=(g == 0), stop=(g == 15)),
                          reads=[("cv", g), "cst"], writes=[("bank", hf)])
                    P.add("pe", lambda e, g=g, hf=hf: e.matmul(bank[2 + hf][:, 0:512], lhsT=ones_f,
                                                               rhs=sq[:, hf * 512:(hf + 1) * 512],
                                                               start=(g == 0), stop=(g == 15)),
                          reads=["sq", "cst"], writes=[("bank", 2 + hf)])
            mean = vt[0]
            var = vt[1]
            nmr = gt[0]
            tmp = gt[1]
            for hf in range(2):
                sl = slice(hf * 512, (hf + 1) * 512)
                P.add("act", lambda e, hf=hf, sl=sl: e.activation(out=mean[:, sl], in_=bank[hf][:, 0:512], func=AF.Copy,
                                                                  scale=1.0 / 2048),
                      reads=[("bank", hf)], writes=[("vt", 0)])
                P.add("dve", lambda e, hf=hf, sl=sl: e.tensor_single_scalar(out=var[:, sl], in_=bank[2 + hf][:, 0:512],
                                                                            scalar=1.0 / 2048, op=ALU.mult),
                      reads=[("bank", 2 + hf)], writes=[("vt", 1)])
            P.add("dve", lambda e: e.tensor_tensor(out=tmp[:, 0:NTOK], in0=mean[:, 0:NTOK], in1=mean[:, 0:NTOK], op=ALU.mult),
                  reads=[("vt", 0)], writes=[("gt", 1)])
            P.add("dve", lambda e: e.tensor_tensor(out=var[:, 0:NTOK], in0=var[:, 0:NTOK], in1=tmp[:, 0:NTOK], op=ALU.subtract),
                  reads=[("vt", 1), ("gt", 1)], writes=[("vt", 1)])
            P.add("act", lambda e: e.activation(out=var[:, 0:NTOK], in_=var[:, 0:NTOK], func=AF.Sqrt, bias=1e-5),
                  reads=[("vt", 1)], writes=[("vt", 1)])
            P.add("dve", lambda e: e.reciprocal(out=var[:, 0:NTOK], in_=var[:, 0:NTOK]), reads=[("vt", 1)], writes=[("vt", 1)])
            P.add("dve", lambda e: e.scalar_tensor_tensor(out=nmr[:, 0:NTOK], in0=mean[:, 0:NTOK], scalar=-1.0,
                                                          in1=var[:, 0:NTOK], op0=ALU.mult, op1=ALU.mult),
                  reads=[("vt", 0), ("vt", 1)], writes=[("gt", 0)])
            for g in range(16):
                P.add("dve", lambda e, g=g: e.tensor_tensor(out=cv_all[:, g, :], in0=cv_all[:, g, :], in1=var[:, 0:NTOK],
                                                            op=ALU.mult), reads=[("cv", g), ("vt", 1)], writes=[("cv", g)])
                P.add("dve", lambda e, g=g: e.tensor_tensor(out=cv_all[:, g, :], in0=cv_all[:, g, :], in1=nmr[:, 0:NTOK],
                                                            op=ALU.add), reads=[("cv", g), ("gt", 0)], writes=[("cv", g)])
                P.add("act", lambda e, g=g: e.activation(out=mixT[:, g, :], in_=cv_all[:, g, :], func=AF.Silu,
                                                         scale=cvec[:, 16 + g:17 + g], bias=cvec[:, 32 + g:33 + g]),
                      reads=[("cv", g), "cvec"], writes=[("mix", g)])
            A.top = P3top

        if stage >= 4:
            qt = [A.alloc([128, NTOK], BF16, "qt%d" % i) for i in range(2)]
            kt = [A.alloc([128, 2 * NTOK], BF16, "kt%d" % i) for i in range(2)]
            vv = [A.alloc([128, 16, 128], BF16, "vv%d" % i) for i in range(2)]
            Rt = A.alloc([128, 128], F32, "Rt")
            wk = [[A.alloc([128, 128], F32, "wk%d_%d" % (i, j)) for j in range(4)] for i in range(4)]
            ab = [A.alloc([128, 128], BF16, "ab%d" % i) for i in range(4)]
            oldk = [("cv", g) for g in range(16)] + ["ut", "sq", ("vt", 0), ("vt", 1), ("gt", 0), ("gt", 1)]
            newk = [("qt", 0), ("qt", 1), ("kt", 0), ("kt", 1), ("vv", 0), ("vv", 1), "Rt", "ab0", "ab1", "ab2", "ab3"] + \
                [("wk", i, j) for i in range(4) for j in range(4)]
            P.add("dve", lambda e: e.engine_nop(), writes=oldk + newk)
            pc = 0
            for h in range(16):
                i = h % 2
                P.add("sp", lambda e, h=h, i=i: e.dma_start(out=qt[i][:], in_=QT[h]), reads=[("QT", h)], writes=[("qt", i)],
                      dma=("qt", i))
                P.add("sp", lambda e, h=h, i=i: e.dma_start(out=kt[i][:], in_=KT[h]), reads=[("KT", h)], writes=[("kt", i)],
                      dma=("kt", i))
                P.add("sp", lambda e, h=h, i=i: e.dma_start(
                    out=vv[i][:], in_=Vd[:, h * 128:(h + 1) * 128].rearrange("(blk p) d -> p blk d", p=128)),
                    reads=[("Vd", tb, (h // 2) * 256) for tb in range(16)], writes=[("vv", i)], dma=("vv", i))
                pairs = []
                for qb in range(8):
                    nkb = 9 + qb
                    for n_, gkb in enumerate(range(8 + qb, -1, -1)):
                        pairs.append((qb, gkb, n_ == 0, n_ == nkb - 1))

                def emitA(p_, i=i, h=h):
                    qb, gkb, first, last, w = p_
                    zb = ("z", w)
                    tb_ = ("tri", w)
                    nb = ("ones", w)
                    zA = bank[0 + w // 2][:, (w % 2) * 128:(w % 2) * 128 + 128]
                    tA = bank[2 + w // 2][:, (w % 2) * 128:(w % 2) * 128 + 128]
                    nA = bank[4 + w // 2][:, (w % 2) * 128:(w % 2) * 128 + 128]
                    ex, sp_, lg_, aa = wk[w]
                    P.add("pe", lambda e: e.matmul(zA, lhsT=kt[i][:, gkb * 128:(gkb + 1) * 128],
                                                   rhs=qt[i][:, qb * 128:(qb + 1) * 128], start=True, stop=True),
                          reads=[("kt", i), ("qt", i)], writes=[zb])
                    P.add("act", lambda e: e.activation(out=ex[:], in_=zA, func=AF.Exp), reads=[zb], writes=[("wk", w, 0)])
                    P.add("act", lambda e: e.activation(out=sp_[:], in_=ex[:], func=AF.Ln, bias=1.0),
                          reads=[("wk", w, 0)], writes=[("wk", w, 1)])
                    if first:
                        P.add("dve", lambda e: e.tensor_tensor(out=ex[:], in0=sp_[:], in1=maskT_f, op=ALU.mult),
                              reads=[("wk", w, 1), "cst"], writes=[("wk", w, 0)])
                        spm, spk = ex, ("wk", w, 0)
                    else:
                        spm, spk = sp_, ("wk", w, 1)
                    P.add("pe", lambda e: e.matmul(tA, lhsT=ntri_f, rhs=spm[:], start=True, stop=True),
                          reads=[spk, "cst"], writes=[tb_])
                    P.add("pe", lambda e: e.matmul(nA, lhsT=nones_f, rhs=spm[:], start=True, stop=True),
                          reads=[spk, "cst"], writes=[nb])
                    P.add("dve", lambda e: e.tensor_tensor(out=lg_[:], in0=zA, in1=sp_[:], op=ALU.subtract),
                          reads=[zb, ("wk", w, 1)], writes=[("wk", w, 2)])
                    P.add("dve", lambda e: e.tensor_tensor(out=lg_[:], in0=lg_[:], in1=tA, op=ALU.add),
                          reads=[tb_, ("wk", w, 2)], writes=[("wk", w, 2)])

                def emitB(p_, i=i, h=h):
                    qb, gkb, first, last, w = p_
                    nb = ("ones", w)
                    nA = bank[4 + w // 2][:, (w % 2) * 128:(w % 2) * 128 + 128]
                    ob = 6 + qb % 2
                    ex, sp_, lg_, aa = wk[w]
                    if not first:
                        P.add("dve", lambda e: e.tensor_tensor(out=lg_[:], in0=lg_[:], in1=Rt[:], op=ALU.add),
                              reads=["Rt", ("wk", w, 2)], writes=[("wk", w, 2)])
                        P.add("act", lambda e: e.activation(out=ab[w][:], in_=lg_[:], func=AF.Exp),
                              reads=[("wk", w, 2)], writes=["ab%d" % w])
                    else:
                        P.add("act", lambda e: e.activation(out=aa[:], in_=lg_[:], func=AF.Exp),
                              reads=[("wk", w, 2)], writes=[("wk", w, 3)])
                        P.add("dve", lambda e: e.tensor_tensor(out=ab[w][:], in0=aa[:], in1=maskT_f, op=ALU.mult),
                              reads=[("wk", w, 3), "cst"], writes=["ab%d" % w])
                    P.add("pe", lambda e: e.matmul(bank[ob][:, 0:128], lhsT=vv[i][:, gkb, :], rhs=ab[w][:], start=first, stop=last),
                          reads=[("vv", i), "ab%d" % w], writes=[("bank", ob)])
                    if not last:
                        if first:
                            P.add("dve", lambda e: e.tensor_copy(out=Rt[:], in_=nA), reads=[nb], writes=["Rt"])
                        else:
                            P.add("dve", lambda e: e.tensor_tensor(out=Rt[:], in0=Rt[:], in1=nA, op=ALU.add),
                                  reads=[nb, "Rt"], writes=["Rt"])
                    else:
                        P.add("act", lambda e: e.copy(out=mixT[:, 16 + h, qb * 128:(qb + 1) * 128], in_=bank[ob][:, 0:128]),
                              reads=[("bank", ob)], writes=[("mix", 16 + h)])

                plist = []
                for p_ in pairs:
                    plist.append(p_ + (pc % 4,))
                    pc += 1
                SK = 2
                for n_ in range(min(SK, len(plist))):
                    emitA(plist[n_])
                for n_ in range(len(plist)):
                    if n_ + SK < len(plist):
                        emitA(plist[n_ + SK])
                    emitB(plist[n_])
            A.top = P3top
            if "mixT" in dbg:
                dump("mixT", [128, 32, NTOK], BF16,
                     [((lambda o, c=c: o[:, c, :]), (lambda c=c: mixT[:, c, :])) for c in range(32)], mixk)

        if stage >= 5:
            mst = [A.alloc([128, 256], F32, "mst%d" % i) for i in range(2)]
            junk = A.alloc([128, 256], F32, "junk")
            ssq = A.alloc([128, 8, 16], F32, "ssq")
            oldk = [("qt", 0), ("qt", 1), ("kt", 0), ("kt", 1), ("vv", 0), ("vv", 1), "Rt", "ab0", "ab1", "ab2", "ab3"] + \
                [("wk", i, j) for i in range(4) for j in range(4)]
            P.add("dve", lambda e: e.engine_nop(), writes=oldk + [("mst", 0), ("mst", 1), "junk", "ssq"])
            wv_out = w_out.rearrange("(kc p) n -> p kc n", p=128)
            pbc = 0
            ms = 0
            for ci in range(16):
                b = ci % 2
                P.add("pool", lambda e, b=b, ci=ci: e.dma_start(out=wbuf[b][:], in_=wv_out[:, :, ci * 256:(ci + 1) * 256]),
                      writes=[("wbuf", b)], dma=("wbuf", b))
                for tb in range(8):
                    pb = pbc % 8
                    pbc += 1
                    s_ = ms % 2
                    ms += 1
                    for kc in range(32):
                        P.add("pe", lambda e, b=b, kc=kc, pb=pb, tb=tb: e.matmul(
                            bank[pb][:, 0:256], lhsT=mixT[:, kc, tb * 128:(tb + 1) * 128], rhs=wbuf[b][:, kc, :],
                            start=(kc == 0), stop=(kc == 31)), reads=[("wbuf", b), ("mix", kc)], writes=[("bank", pb)])
                    P.add("act", lambda e, pb=pb, s_=s_: e.copy(out=mst[s_][:], in_=bank[pb][:, 0:256]),
                          reads=[("bank", pb)], writes=[("mst", s_)])
                    P.add("act", lambda e, s_=s_, tb=tb, ci=ci: e.activation(out=junk[:], in_=mst[s_][:], func=AF.Square,
                                                                             accum_out=ssq[:, tb, ci:ci + 1]),
                          reads=[("mst", s_)], writes=["junk", "ssq"])
                    P.add("sp", lambda e, s_=s_, tb=tb, ci=ci: e.dma_start(out=Md[tb * 128:(tb + 1) * 128, ci * 256:(ci + 1) * 256],
                                                                         in_=mst[s_][:]),
                          reads=[("mst", s_)], writes=[("Md", tb)], dma=("mst", s_))
            A.top = hT_off
            h2_all = A.alloc([128, 8, D], BF16, "h2_all")
            A.top = P3top
            wr = A.alloc([128, 32, 32], BF16, "wr")
            brt = A.alloc([128, 32], F32, "brt")
            lgt = A.alloc([128, 32], F32, "lgt")
            mx8 = A.alloc([128, 8], F32, "mx8")
            mkf = A.alloc([128, 32], F32, "mkf")
            ext = A.alloc([128, 32], F32, "ext")
            G_all = A.alloc([128, 8, 32], F32, "G_all")
            mk_all = A.alloc([128, 8, 32], BF16, "mk_all")
            mkf_all = A.alloc([128, 8, 32], F32, "mkf_all")
            pos_all = A.alloc([128, 8, 32], F32, "pos_all")
            P5keep = A.top
            ggt = A.alloc([128, D], F32, "ggt")
            a2t = A.alloc([128, D], F32, "a2t")
            b2t = A.alloc([128, D], F32, "b2t")
            xm = A.alloc([128, D], F32, "xm")
            xx = A.alloc([128, D], F32, "xx")
            h2T = A.alloc([128, 32, 128], BF16, "h2T")
            P5top = A.top
            h2k = [("h2", tb) for tb in range(8)]
            P.add("dve", lambda e: e.engine_nop(),
                  writes=mixk + ["bgluT", "cwT", "cvec", ("mst", 0), ("mst", 1), "junk"] + h2k +
                  ["ggt", "a2t", "b2t", "xm", "xx", "h2T", "wr", "brt", "lgt", "mx8", "mkf", "ext", "G_all", "mk_all", "mkf_all",
                   "pos_all"])
            BCk = lambda j: [("BC", j, c0) for c0 in range(0, D, 256)]
            P.add("sp", lambda e: e.dma_start(out=ggt[:], in_=BC[0]), reads=BCk(0), writes=["ggt"], dma="c0")
            P.add("sp", lambda e: e.dma_start(out=a2t[:], in_=BC[2]), reads=BCk(2), writes=["a2t"], dma="c1")
            P.add("sp", lambda e: e.dma_start(out=b2t[:], in_=BC[1]), reads=BCk(1), writes=["b2t"], dma="c2")
            P.add("sp", lambda e: e.dma_start(out=brt[:], in_=brt_d), writes=["brt"], dma="c3")
            P.add("pool", lambda e: e.dma_start(out=wr[:], in_=w_router.rearrange("(kc p) n -> p kc n", p=128)),
                  writes=["wr"], dma="wr")
            ev = 0
            for tb in range(8):
                P.add("sp", lambda e, tb=tb: e.dma_start(out=xm[:], in_=Md[tb * 128:(tb + 1) * 128, :]), reads=[("Md", tb)],
                      writes=["xm"], dma="xm")
                P.add("sp", lambda e, tb=tb: e.dma_start(out=xx[:], in_=xo[tb * 128:(tb + 1) * 128, :]), writes=["xx"], dma="xx")
                P.add("dve", lambda e, tb=tb: e.reduce_sum(out=small[:, 8:9], in_=ssq[:, tb, :], axis=mybir.AxisListType.X),
                      reads=["ssq"], writes=["s8"])
                P.add("act", lambda e: e.activation(out=small[:, 9:10], in_=small[:, 8:9], func=AF.Sqrt, scale=1.0 / D, bias=1e-6),
                      reads=["s8"], writes=["s9"])
                P.add("dve", lambda e: e.reciprocal(out=small[:, 10:11], in_=small[:, 9:10]), reads=["s9"], writes=["s10"])
                P.add("dve", lambda e: e.scalar_tensor_tensor(out=xm[:], in0=xm[:], scalar=small[:, 10:11], in1=ggt[:],
                                                              op0=ALU.mult, op1=ALU.mult),
                      reads=["xm", "s10", "ggt"], writes=["xm"])
                P.add("dve", lambda e: e.tensor_tensor(out=xx[:], in0=xx[:], in1=xm[:], op=ALU.add), reads=["xx", "xm"],
                      writes=["xx"])
                P.add("sp", lambda e, tb=tb: e.dma_start(out=X1d[tb * 128:(tb + 1) * 128, :], in_=xx[:]), reads=["xx"],
                      writes=[("X1", tb)], dma="x1s")
                P.add("act", lambda e: e.activation(out=xm[:], in_=xx[:], func=AF.Square, accum_out=small[:, 11:12]),
                      reads=["xx"], writes=["xm", "s11"])
                P.add("act", lambda e: e.activation(out=small[:, 12:13], in_=small[:, 11:12], func=AF.Sqrt, scale=1.0 / D,
                                                    bias=1e-6), reads=["s11"], writes=["s12"])
                P.add("dve", lambda e: e.reciprocal(out=small[:, 13:14], in_=small[:, 12:13]), reads=["s12"], writes=["s13"])
                P.add("dve", lambda e: e.scalar_tensor_tensor(out=xm[:], in0=xx[:], scalar=small[:, 13:14], in1=a2t[:],
                                                              op0=ALU.mult, op1=ALU.mult),
                      reads=["xx", "s13", "a2t"], writes=["xm"])
                P.add("dve", lambda e, tb=tb: e.tensor_tensor(out=h2_all[:, tb, :], in0=xm[:], in1=b2t[:], op=ALU.add),
                      reads=["xm", "b2t"], writes=[("h2", tb)])
                for kc in range(32):
                    pb = kc % 4
                    pt = bank[pb][:, 0:64].bitcast(BF16)
                    P.add("pe", lambda e, kc=kc, pt=pt, tb=tb: e.transpose(out=pt, in_=h2_all[:, tb, kc * 128:(kc + 1) * 128],
                                                                          identity=ident[:]),
                          reads=[("h2", tb), "ident"], writes=[("bank", pb)])
                    if ev % 2 == 0:
                        P.add("act", lambda e, kc=kc, pt=pt: e.copy(out=h2T[:, kc, :], in_=pt), reads=[("bank", pb)],
                              writes=[("h2T", kc)])
                    else:
                        P.add("dve", lambda e, kc=kc, pt=pt: e.tensor_copy(out=h2T[:, kc, :], in_=pt), reads=[("bank", pb)],
                              writes=[("h2T", kc)])
                    ev += 1
                for kc in range(32):
                    P.add("pe", lambda e, kc=kc: e.matmul(bank[4][:, 0:32], lhsT=h2T[:, kc, :], rhs=wr[:, kc, :],
                                                          start=(kc == 0), stop=(kc == 31)),
                          reads=[("h2T", kc), "wr"], writes=[("bank", 4)])
                P.add("dve", lambda e: e.tensor_tensor(out=lgt[:], in0=bank[4][:, 0:32], in1=brt[:], op=ALU.add),
                      reads=[("bank", 4), "brt"], writes=["lgt"])
                P.add("dve", lambda e: e.max(out=mx8[:], in_=lgt[:]), reads=["lgt"], writes=["mx8"])
                P.add("dve", lambda e: e.tensor_single_scalar(out=small[:, 14:15], in_=mx8[:, 0:1], scalar=-1.0, op=ALU.mult),
                      reads=["mx8"], writes=["s14"])
                P.add("dve", lambda e: e.tensor_scalar(out=mkf[:], in0=lgt[:], scalar1=mx8[:, 3:4], scalar2=None, op0=ALU.is_ge),
                      reads=["lgt", "mx8"], writes=["mkf"])
                P.add("act", lambda e: e.activation(out=ext[:], in_=lgt[:], func=AF.Exp, bias=small[:, 14:15]),
                      reads=["lgt", "s14"], writes=["ext"])
                P.add("dve", lambda e: e.tensor_tensor(out=ext[:], in0=ext[:], in1=mkf[:], op=ALU.mult), reads=["ext", "mkf"],
                      writes=["ext"])
                P.add("dve", lambda e: e.reduce_sum(out=small[:, 15:16], in_=ext[:], axis=mybir.AxisListType.X), reads=["ext"],
                      writes=["s15"])
                P.add("dve", lambda e: e.reciprocal(out=small[:, 16:17], in_=small[:, 15:16]), reads=["s15"], writes=["s16"])
                P.add("dve", lambda e, tb=tb: e.tensor_scalar(out=G_all[:, tb, :], in0=ext[:], scalar1=small[:, 16:17],
                                                              scalar2=None, op0=ALU.mult),
                      reads=["ext", "s16"], writes=["G_all"])
                P.add("dve", lambda e, tb=tb: e.tensor_copy(out=mk_all[:, tb, :], in_=mkf[:]), reads=["mkf"], writes=[("mk", tb)])
                P.add("dve", lambda e, tb=tb: e.tensor_copy(out=mkf_all[:, tb, :], in_=mkf[:]), reads=["mkf"], writes=["mkf_all"])
                P.add("pe", lambda e, tb=tb: e.matmul(bank[5][:, 0:32], lhsT=triS_b, rhs=mk_all[:, tb, :], start=True,
                                                      stop=(tb == 0)), reads=[("mk", tb), "cstb"], writes=[("bank", 5)])
                for pb_ in range(tb):
                    P.add("pe", lambda e, pb_=pb_, tb=tb: e.matmul(bank[5][:, 0:32], lhsT=ones_b, rhs=mk_all[:, pb_, :],
                                                                   start=False, stop=(pb_ == tb - 1)),
                          reads=[("mk", pb_), "cstb"], writes=[("bank", 5)])
                P.add("dve", lambda e, tb=tb: e.tensor_copy(out=pos_all[:, tb, :], in_=bank[5][:, 0:32]), reads=[("bank", 5)],
                      writes=["pos_all"])
            if "X1" in dbg:
                dump("X1", [NTOK, D], F32,
                     [((lambda o, r=r: o[r * 128:(r + 1) * 128, :]), (lambda r=r: X1d[r * 128:(r + 1) * 128, :])) for r in range(8)],
                     [("X1", tb) for tb in range(8)])
            if "G" in dbg:
                dump("G", [128, 8, 32], F32, [((lambda o: o), (lambda: G_all[:]))], ["G_all"])
                dump("pos", [128, 8, 32], F32, [((lambda o: o), (lambda: pos_all[:]))], ["pos_all"])

        if stage >= 6:
            A.top = P5keep
            NSC = CAP // 128
            iot = A.alloc([128, CAP], F32, "iot")
            tidf = A.alloc([128, 8, 5], F32, "tidf")
            Rb = A.alloc([128, 8, 5], BF16, "Rb")
            ghi = A.alloc([128, 8], BF16, "ghi")
            ghf = A.alloc([128, 8], F32, "ghf")
            sel = A.alloc([128, 8, CAP], BF16, "sel")
            XeT = A.alloc([128, 32, CAP], BF16, "XeT")
            gsT = A.alloc([128, 2, CAP], BF16, "gsT")
            actT = A.alloc([128, 12, CAP], BF16, "actT")
            gm = [A.alloc([128, CAP], F32, "gm%d" % i) for i in range(2)]
            sg = A.alloc([128, CAP], F32, "sg")
            bgu = [A.alloc([128, 24], F32, "bgu%d" % i) for i in range(2)]
            bd = [A.alloc([1, 512], BF16, "bd%d" % i) for i in range(2)]
            onesr = A.alloc([1, 128], BF16, "onesr")
            wd = [A.alloc([128, 12, 256], BF16, "wd%d" % i) for i in range(2)]
            Yt = [A.alloc([128, 512], F32, "Yt%d" % i) for i in range(2 * NSC)]
            inf = [A.alloc([128, 8], F32, "inf%d" % i) for i in range(NSC)]
            idx = [A.alloc([128, 1], I32, "idx%d" % i) for i in range(NSC)]
            zt = Yt[0]
            wbuf3 = A.alloc([128, 32, 256], BF16, "wbuf3")
            wd3 = A.alloc([128, 12, 256], BF16, "wd3")
            wbm = [wbuf[0], wbuf[1], wbuf3]
            wdm = [wd[0], wd[1], wd3]
            oldk = ["ggt", "a2t", "b2t", "xm", "xx", "h2T"]
            newk = ["iot", "tidf", "Rb", "ghi", "ghf", "XeT", ("gs", 0), ("gs", 1), ("gm", 0), ("gm", 1), "sg",
                    ("bgu", 0), ("bgu", 1), ("bd", 0), ("bd", 1), "onesr", ("wd", 0), ("wd", 1), ("wd", 2), ("wbuf", 2)] + \
                [("sel", tb) for tb in range(8)] + [("XeT", dc) for dc in range(32)] + [("act", f) for f in range(12)] + \
                [("Yt", i) for i in range(2 * NSC)] + [("inf", i) for i in range(NSC)] + [("idx", i) for i in range(NSC)]
            P.add("dve", lambda e: e.engine_nop(), writes=oldk + newk)
            P.add("sp", lambda e: e.dma_start(out=iot[:], in_=iota_d), writes=["iot"], dma="c0")
            P.add("sp", lambda e: e.dma_start(out=tidf[:], in_=tid_d.rearrange("p (b c) -> p b c", c=5)), writes=["tidf"], dma="c1")
            P.add("dve", lambda e: e.tensor_copy(out=Rb[:], in_=tidf[:]), reads=["tidf"], writes=["Rb"])
            P.add("dve", lambda e: e.memset(onesr[:], 1.0), writes=["onesr"])
            P.add("dve", lambda e: e.memset(zt[:], 0.0), writes=[("Yt", 0)])
            Fk = [("Fd", c) for c in range(8)]
            for tb in range(NTOK // 128 + 1):
                rows = 128 if tb < 8 else 1
                for hf in range(8):
                    P.add("sp", lambda e, tb=tb, hf=hf, rows=rows: e.dma_start(
                        out=Fd[hf][tb * 128:tb * 128 + rows, :], in_=zt[0:rows, :]), reads=[("Yt", 0)],
                        writes=[Fk[hf]], dma="fz")
            wch = 0
            wdc = 0
            pbc = 0
            yrot = 0
            pending = []

            def flush_pending():
                while pending:
                    sci, M, yi, c0, fk = pending.pop(0)
                    P.add("pool", lambda e, sci=sci, M=M, yi=yi, c0=c0: e.indirect_dma_start(
                        out=Fd[c0 // 512][:, :], out_offset=bass.IndirectOffsetOnAxis(ap=idx[sci][0:M, 0:1], axis=0),
                        in_=Yt[yi][0:M, :], in_offset=None, compute_op=ALU.add),
                        reads=[("Yt", yi), ("idx", sci)], writes=[("Fd", fk)], dma=("scat", yi))

            slots = [(c * 128, 128) for c in range(NSC)]
            for ex_ in range(nexp):
                eb = ex_ % 2
                P.add("sp", lambda e, ex_=ex_, eb=eb: e.dma_start(out=bgu[eb][:], in_=bguT_d[ex_]), writes=[("bgu", eb)],
                      dma=("bgu", eb))
                P.add("dve", lambda e, ex_=ex_: e.tensor_copy(out=ghi[:], in_=G_all[:, :, ex_]), reads=["G_all"], writes=["ghi"])
                P.add("dve", lambda e: e.tensor_copy(out=ghf[:], in_=ghi[:]), reads=["ghi"], writes=["ghf"])
                P.add("dve", lambda e, ex_=ex_: e.tensor_tensor(out=ghf[:], in0=G_all[:, :, ex_], in1=ghf[:], op=ALU.subtract),
                      reads=["G_all", "ghf"], writes=["ghf"])
                P.add("dve", lambda e: e.tensor_copy(out=Rb[:, :, 3], in_=ghi[:]), reads=["ghi"], writes=["Rb"])
                P.add("dve", lambda e: e.tensor_copy(out=Rb[:, :, 4], in_=ghf[:]), reads=["ghf"], writes=["Rb"])
                for tb in range(8):
                    P.add("dve", lambda e, tb=tb, ex_=ex_: e.tensor_scalar(
                        out=sel[:, tb, :], in0=iot[:], scalar1=pos_all[:, tb, ex_:ex_ + 1], scalar2=mkf_all[:, tb, ex_:ex_ + 1],
                        op0=ALU.is_equal, op1=ALU.mult), reads=["iot", "pos_all", "mkf_all"], writes=[("sel", tb)])
                wv_gu = w_gu[ex_].rearrange("(kc p) n -> p kc n", p=128)
                gu_cols = []
                for pc_ in range(6):
                    gu_cols.append(pc_ * 256)
                    gu_cols.append(1536 + pc_ * 256)
                wv_dn = w_dn[ex_].rearrange("(fc p) n -> p fc n", p=128)

                def load_gu(j):
                    nonlocal wch
                    b = wch % 3
                    wch += 1
                    c0 = gu_cols[j]
                    P.add("pool", lambda e, b=b, c0=c0, wv_gu=wv_gu: e.dma_start(out=wbm[b][:], in_=wv_gu[:, :, c0:c0 + 256]),
                          writes=[("wbuf", b)], dma=("wbuf", b))
                    return b

                def load_dn(dc):
                    nonlocal wdc
                    b2 = wdc % 3
                    wdc += 1
                    P.add("pool", lambda e, b2=b2, dc=dc, wv_dn=wv_dn: e.dma_start(out=wdm[b2][:], in_=wv_dn[:, :, dc * 256:(dc + 1) * 256]),
                          writes=[("wd", b2)], dma=("wd", b2))
                    return b2

                gq = [load_gu(0), load_gu(1)]
                flush_pending()
                for dc in range(32):
                    pb = pbc % 6
                    pbc += 1
                    for tb in range(8):
                        P.add("pe", lambda e, dc=dc, tb=tb, pb=pb: e.matmul(
                            bank[pb][:, 0:CAP], lhsT=h2_all[:, tb, dc * 128:(dc + 1) * 128], rhs=sel[:, tb, :],
                            start=(tb == 0), stop=(tb == 7)), reads=[("h2", tb), ("sel", tb)], writes=[("bank", pb)])
                    if dc % 2 == 0:
                        P.add("act", lambda e, dc=dc, pb=pb: e.copy(out=XeT[:, dc, :], in_=bank[pb][:, 0:CAP]),
                              reads=[("bank", pb)], writes=[("XeT", dc)])
                    else:
                        P.add("dve", lambda e, dc=dc, pb=pb: e.tensor_copy(out=XeT[:, dc, :], in_=bank[pb][:, 0:CAP]),
                              reads=[("bank", pb)], writes=[("XeT", dc)])
                for sci, (s0, M) in enumerate(slots):
                    ib = 6 + sci % 2
                    for tb in range(8):
                        P.add("pe", lambda e, tb=tb, s0=s0, M=M, ib=ib: e.matmul(
                            bank[ib][0:M, 0:5], lhsT=sel[:, tb, s0:s0 + M], rhs=Rb[:, tb, :], start=(tb == 0), stop=(tb == 7)),
                            reads=[("sel", tb), "Rb"], writes=[("bank", ib)])
                    P.add("dve", lambda e, sci=sci, M=M, ib=ib: e.tensor_copy(out=inf[sci][0:M, 0:5], in_=bank[ib][0:M, 0:5]),
                          reads=[("bank", ib)], writes=[("inf", sci)])
                    P.add("dve", lambda e, sci=sci, M=M: e.scalar_tensor_tensor(
                        out=inf[sci][0:M, 5:6], in0=inf[sci][0:M, 0:1], scalar=128.0, in1=inf[sci][0:M, 1:2], op0=ALU.mult, op1=ALU.add),
                        reads=[("inf", sci)], writes=[("inf", sci)])
                    P.add("dve", lambda e, sci=sci, M=M: e.tensor_scalar(
                        out=inf[sci][0:M, 6:7], in0=inf[sci][0:M, 2:3], scalar1=-float(NTOK), scalar2=float(NTOK), op0=ALU.mult,
                        op1=ALU.add), reads=[("inf", sci)], writes=[("inf", sci)])
                    P.add("dve", lambda e, sci=sci, M=M: e.tensor_tensor(out=inf[sci][0:M, 5:6], in0=inf[sci][0:M, 5:6],
                                                                         in1=inf[sci][0:M, 6:7], op=ALU.add),
                          reads=[("inf", sci)], writes=[("inf", sci)])
                    P.add("dve", lambda e, sci=sci, M=M: e.tensor_copy(out=idx[sci][0:M, :], in_=inf[sci][0:M, 5:6]),
                          reads=[("inf", sci)], writes=[("idx", sci)])
                    P.add("dve", lambda e, sci=sci, M=M: e.tensor_tensor(out=inf[sci][0:M, 7:8], in0=inf[sci][0:M, 3:4],
                                                                         in1=inf[sci][0:M, 4:5], op=ALU.add),
                          reads=[("inf", sci)], writes=[("inf", sci)])
                dq = []
                for j in range(12):
                    b = gq.pop(0)
                    if j + 2 < 12:
                        gq.append(load_gu(j + 2))
                    else:
                        dq.append(load_dn(j + 2 - 12))
                    isg = j % 2 == 0
                    for sub in range(2):
                        fcb = (gu_cols[j] + sub * 128) // 128
                        f2 = (j // 2) * 2 + sub
                        pb = pbc % 6
                        pbc += 1
                        for kc in range(32):
                            P.add("pe", lambda e, b=b, sub=sub, kc=kc, pb=pb: e.matmul(
                                bank[pb][:, 0:CAP], lhsT=wbm[b][:, kc, sub * 128:(sub + 1) * 128], rhs=XeT[:, kc, :],
                                start=(kc == 0), stop=(kc == 31)), reads=[("wbuf", b), ("XeT", kc)], writes=[("bank", pb)])
                        w_ = sub
                        P.add("dve", lambda e, pb=pb, w_=w_, fcb=fcb, eb=eb: e.tensor_scalar(
                            out=gm[w_][:], in0=bank[pb][:, 0:CAP], scalar1=bgu[eb][:, fcb:fcb + 1], scalar2=7.0,
                            op0=ALU.add, op1=ALU.min), reads=[("bank", pb), ("bgu", eb)], writes=[("gm", w_)])
                        if isg:
                            P.add("act", lambda e, w_=w_: e.activation(out=sg[:], in_=gm[w_][:], func=AF.Sigmoid, scale=1.702),
                                  reads=[("gm", w_)], writes=["sg"])
                            P.add("dve", lambda e, w_=w_, sub=sub: e.tensor_tensor(out=gsT[:, sub, :], in0=gm[w_][:], in1=sg[:],
                                                                                  op=ALU.mult),
                                  reads=[("gm", w_), "sg"], writes=[("gs", sub)])
                        else:
                            P.add("dve", lambda e, w_=w_: e.tensor_scalar(out=gm[w_][:], in0=gm[w_][:], scalar1=-7.0, scalar2=1.0,
                                                                          op0=ALU.max, op1=ALU.add),
                                  reads=[("gm", w_)], writes=[("gm", w_)])
                            P.add("dve", lambda e, w_=w_, f2=f2, sub=sub: e.tensor_tensor(out=actT[:, f2, :], in0=gm[w_][:],
                                                                                         in1=gsT[:, sub, :], op=ALU.mult),
                                  reads=[("gm", w_), ("gs", sub)], writes=[("act", f2)])
                for dc in range(16):
                    b2 = dq.pop(0)
                    if dc + 2 < 16:
                        dq.append(load_dn(dc + 2))
                    flush_pending()
                    half = dc % 2
                    if half == 0:
                        bb = (dc // 2) % 2
                        P.add("pool", lambda e, ex_=ex_, bb=bb, dc=dc: e.dma_start(out=bd[bb][:], in_=b_dn[ex_:ex_ + 1, dc * 256:dc * 256 + 512]),
                              writes=[("bd", bb)], dma=("bd", bb))
                        yset = yrot % 2
                        yrot += 1
                    for sci, (s0, M) in enumerate(slots):
                        pb = pbc % 6
                        pbc += 1
                        yi = yset * NSC + sci
                        for fc in range(12):
                            P.add("pe", lambda e, b2=b2, fc=fc, pb=pb, s0=s0, M=M: e.matmul(
                                bank[pb][0:M, 0:256], lhsT=actT[:, fc, s0:s0 + M], rhs=wdm[b2][:, fc, :], start=(fc == 0), stop=False),
                                reads=[("wd", b2), ("act", fc)], writes=[("bank", pb)])
                        P.add("pe", lambda e, pb=pb, M=M, half=half, bb=bb: e.matmul(
                            bank[pb][0:M, 0:256], lhsT=onesr[0:1, 0:M], rhs=bd[bb][0:1, half * 256:(half + 1) * 256], start=False,
                            stop=True), reads=[("bd", bb), "onesr"], writes=[("bank", pb)])
                        P.add("act", lambda e, sci=sci, M=M, pb=pb, half=half, yi=yi: e.activation(
                            out=Yt[yi][0:M, half * 256:(half + 1) * 256], in_=bank[pb][0:M, 0:256], func=AF.Copy,
                            scale=inf[sci][0:M, 7:8]), reads=[("bank", pb), ("inf", sci)], writes=[("Yt", yi)])
                        if half == 1:
                            c0 = (dc // 2) * 512
                            pending.append((sci, M, yi, c0, dc // 2))
            flush_pending()
            if "F" in dbg:
                dump("F", [NTOK, D], F32,
                     [((lambda o, c=c: o[:, c * 512:(c + 1) * 512]), (lambda c=c: Fd[c][0:NTOK, :])) for c in range(8)],
                     Fk)

        if stage >= 7:
            A.top = hT_off
            g2t = A.alloc([128, D], F32, "g2t")
            fm_ = A.alloc([128, D], F32, "fm_")
            x1t = A.alloc([128, D], F32, "x1t")
            jk = A.alloc([128, D], BF16, "jk")
            P.add("dve", lambda e: e.engine_nop(), writes=[("h2", tb) for tb in range(8)] + ["g2t", "fm_", "x1t", "jk"])
            P.add("sp", lambda e: e.dma_start(out=g2t[:], in_=BC[3]), reads=[("BC", 3, c0) for c0 in range(0, D, 256)],
                  writes=["g2t"], dma="c0")
            for tb in range(8):
                for c in range(8):
                    P.add("sp", lambda e, tb=tb, c=c: e.dma_start(out=fm_[:, c * 512:(c + 1) * 512], in_=Fd[c][tb * 128:(tb + 1) * 128, :]),
                          reads=[("Fd", c)], writes=["fm_"], dma=("fm_", c))
                P.add("sp", lambda e, tb=tb: e.dma_start(out=x1t[:], in_=X1d[tb * 128:(tb + 1) * 128, :]), reads=[("X1", tb)],
                      writes=["x1t"], dma="x1t")
                P.add("act", lambda e: e.activation(out=jk[:], in_=fm_[:], func=AF.Square, accum_out=small[:, 20:21]),
                      reads=["fm_"], writes=["jk", "s20"])
                P.add("act", lambda e: e.activation(out=small[:, 21:22], in_=small[:, 20:21], func=AF.Sqrt, scale=1.0 / D,
                                                    bias=1e-6), reads=["s20"], writes=["s21"])
                P.add("dve", lambda e: e.reciprocal(out=small[:, 22:23], in_=small[:, 21:22]), reads=["s21"], writes=["s22"])
                P.add("dve", lambda e: e.scalar_tensor_tensor(out=fm_[:], in0=fm_[:], scalar=small[:, 22:23], in1=g2t[:],
                                                              op0=ALU.mult, op1=ALU.mult),
                      reads=["fm_", "s22", "g2t"], writes=["fm_"])
                P.add("dve", lambda e: e.tensor_tensor(out=x1t[:], in0=x1t[:], in1=fm_[:], op=ALU.add), reads=["x1t", "fm_"],
                      writes=["x1t"])
                P.add("sp", lambda e, tb=tb: e.dma_start(out=out_d[tb * 128:(tb + 1) * 128, :], in_=x1t[:]), reads=["x1t"],
                      writes=[("out", tb)], dma="outs")
                out_keys.append(("out", tb))

        if stage < 3:
            zt = A.alloc([128, D], F32, "zt")
            P.add("pool", lambda e: e.memset(zt[:], 0.0),
                  writes=["zt", "xt", "xs", ("stg", 0), ("stg", 1), ("stgk", 0), ("stgk", 1), ("stgv", 0), ("stgv", 1)])
            for tb in range(8):
                P.add("sp", lambda e, tb=tb: e.dma_start(out=out_d[tb * 128:(tb + 1) * 128, :], in_=zt[:]), reads=["zt"],
                      writes=[("out", tb)], dma=("out", tb % 2))
                out_keys.append(("out", tb))
        P.add("sp", lambda e: e.nop(), reads=out_keys)
        P.emit(nc)
    return nc, dbg_out


def _consts():
    i = np.arange(128)
    ones = np.ones((128, 128), np.float32)
    ntri = -(i[:, None] > i[None, :]).astype(np.float32)
    maskT = (i[None, :] > i[:, None]).astype(np.float32)
    triS = (i[:, None] < i[None, :]).astype(np.float32)
    return np.ascontiguousarray(np.stack([ones, ntri, maskT, -ones, triS, ones]))


def _tid():
    t = np.zeros((128, 8, 5), np.float32)
    t[:, :, 0] = np.arange(8, dtype=np.float32)[None, :]
    t[:, :, 1] = np.arange(128, dtype=np.float32)[:, None]
    t[:, :, 2] = 1.0
    return np.ascontiguousarray(t.reshape(128, 40))


def prep_inputs(inp):
    f = lambda a: np.ascontiguousarray(np.asarray(a, dtype=np.float32))
    x = f(inp["x"])
    c = f(inp["c"])
    b_ada = f(inp["b_ada"])[0]
    fm = lambda v: np.ascontiguousarray(v.reshape(-1, 128).T)
    bc = lambda v: np.broadcast_to(v[None, :], (128, v.shape[0]))
    vec_bc = np.ascontiguousarray(np.stack([
        bc(b_ada[2 * D:3 * D]), bc(b_ada[3 * D:4 * D]), bc(b_ada[4 * D:5 * D]), bc(b_ada[5 * D:6 * D]),
        bc(f(inp["g_post_mix"])[0]), bc(f(inp["g_pre_ffn"])[0]), bc(f(inp["g_post_ffn"])[0])]))
    shared = {
        "w_ada": f(inp["w_ada"])[0],
        "b_adaT": fm(b_ada[:2 * D]),
        "vec_bc": vec_bc,
        "gpreT": fm(f(inp["g_pre_mix"])[0]),
        "w_in": f(inp["w_in"])[0],
        "w_out": f(inp["w_out"])[0],
        "bgluT": fm(f(inp["b_glu"])[0]),
        "cwT": np.ascontiguousarray(f(inp["conv_w"])[0][:, 0, :].reshape(31, 16, 128).transpose(2, 1, 0).reshape(128, 16 * 31)),
        "cvec": np.ascontiguousarray(np.concatenate([fm(f(inp["conv_b"])[0]), fm(f(inp["conv_ln_g"])[0]),
                                                     fm(f(inp["conv_ln_b"])[0])], 1)),
        "consts": _consts(),
        "w_router": f(inp["w_router"])[0],
        "brt": np.ascontiguousarray(bc(f(inp["b_router"])[0])),
        "iota": np.ascontiguousarray(np.broadcast_to(np.arange(CAP, dtype=np.float32)[None, :], (128, CAP))),
        "tid": _tid(),
        "w_gu": f(inp["w_gate_up"])[0],
        "bguT": np.ascontiguousarray(f(inp["b_gate_up"])[0].reshape(32, 24, 128).transpose(0, 2, 1)),
        "w_dn": f(inp["w_down"])[0],
        "b_dn": f(inp["b_down"])[0],
    }
    zeros = np.zeros((NTOK, D), np.float32)
    maps = []
    for r in range(8):
        b, half = r // 2, r % 2
        m = dict(shared)
        m["xo"] = np.ascontiguousarray(x[b, half * NTOK:(half + 1) * NTOK])
        m["xp"] = np.ascontiguousarray(x[b, 0:NTOK]) if half == 1 else zeros
        m["flag"] = np.full((128, 1), float(half), np.float32)
        m["cT"] = fm(c[b])
        maps.append(m)
    return maps


def kernel(**inputs):
    nc, _ = build()
    maps = prep_inputs(inputs)
    res = run_bass_kernel_spmd(nc, maps, core_ids=list(range(8)))
    out = np.empty((4, 2048, D), np.float32)
    for r in range(8):
        b, half = r // 2, r % 2
        out[b, half * NTOK:(half + 1) * NTOK] = res.results[r]["out"]
    return out
```

```python
import numpy as np
import concourse.bass as bass
import concourse.mybir as mybir
from concourse.bass_utils import run_bass_kernel_spmd
from contextlib import ExitStack

F32 = mybir.dt.float32
BF16 = mybir.dt.bfloat16
I32 = mybir.dt.int32
AF = mybir.ActivationFunctionType
ALU = mybir.AluOpType

ENGS = ["pe", "act", "dve", "pool", "sp"]
TRUST_SAME = {"pe": True, "act": False, "dve": False, "pool": False, "sp": True}


class Op:
    __slots__ = ("eng", "fn", "deps", "signal", "sigval", "dma_key", "dma_val")

    def __init__(self, eng, fn):
        self.eng = eng
        self.fn = fn
        self.deps = ()
        self.signal = False
        self.sigval = 0
        self.dma_key = None
        self.dma_val = 0


class Prog:
    def __init__(self):
        self.ops = {e: [] for e in ENGS}
        self.last_w = {}
        self.readers = {}
        self.dma_cnt = {}

    def add(self, eng, fn, reads=(), writes=(), dma=None):
        op = Op(eng, fn)
        deps = set()
        for r in reads:
            lw = self.last_w.get(r)
            if lw is not None:
                deps.add(lw)
        for w in writes:
            lw = self.last_w.get(w)
            if lw is not None:
                deps.add(lw)
            rs = self.readers.get(w)
            if rs:
                deps.update(rs)
        op.deps = tuple(deps)
        for r in reads:
            self.readers.setdefault(r, []).append(op)
        for w in writes:
            self.last_w[w] = op
            self.readers[w] = []
        if dma is not None:
            op.dma_key = dma
            self.dma_cnt[dma] = self.dma_cnt.get(dma, 0) + 16
            op.dma_val = self.dma_cnt[dma]
        self.ops[eng].append(op)
        return op

    def emit(self, nc):
        for e in ENGS:
            for op in self.ops[e]:
                for d in op.deps:
                    if d.dma_key is None:
                        if d.eng == op.eng and TRUST_SAME[e]:
                            continue
                        d.signal = True
        for e in ENGS:
            c = 0
            for op in self.ops[e]:
                if op.signal and op.dma_key is None:
                    c += 1
                    op.sigval = c
        with ExitStack() as st:
            sem = {}
            for e in ENGS:
                sem[("eng", e)] = st.enter_context(nc.semaphore("s_" + e))
            for i, k in enumerate(self.dma_cnt):
                sem[("dma", k)] = st.enter_context(nc.semaphore("d%d" % i))
            block = st.enter_context(nc.Block())

            def run(e, eng):
                waited = {}
                for op in self.ops[e]:
                    need = {}
                    for d in op.deps:
                        if d.dma_key is not None:
                            k = ("dma", d.dma_key)
                            v = d.dma_val
                        else:
                            if d.eng == e and TRUST_SAME[e]:
                                continue
                            k = ("eng", d.eng)
                            v = d.sigval
                        if need.get(k, 0) < v:
                            need[k] = v
                    for k, v in need.items():
                        if waited.get(k, 0) < v:
                            eng.wait_ge(sem[k], v)
                            waited[k] = v
                    inst = op.fn(eng)
                    if op.dma_key is not None:
                        inst.then_inc(sem[("dma", op.dma_key)], 16)
                    elif op.signal:
                        inst.then_inc(sem[("eng", e)], 1)

            block.tensor(lambda eng: run("pe", eng))
            block.scalar(lambda eng: run("act", eng))
            block.vector(lambda eng: run("dve", eng))
            block.gpsimd(lambda eng: run("pool", eng))
            block.sync(lambda eng: run("sp", eng))


SB_BASE = 16512 + 2048
SB_END = 229344


class Arena:
    def __init__(self, nc):
        self.nc = nc
        self.top = SB_BASE
        self.n = 0

    def alloc(self, shape, dt, name=None):
        nbytes = int(np.prod(shape[1:])) * (4 if dt in (F32, I32) else 2)
        nbytes = (nbytes + 31) // 32 * 32
        off = self.top
        assert off + nbytes <= SB_END, ("SBUF overflow", name, off + nbytes - SB_END)
        self.top += nbytes
        self.n += 1
        return self.nc.alloc_sbuf_tensor_at("%s_%d" % (name or "t", self.n), list(shape), dt, offset=off)


NTOK = 1024
D = 4096
DH_SCALE = 128 ** -0.5
HALO = 128
VGW = NTOK + HALO
CAP = 384


def build(stage=99, dbg=(), nada=96, ntb=16, nexp=32):
    nc = bass.Bass("TRN2", target_bir_lowering=False)
    di = lambda name, shape, dt=F32: nc.dram_tensor(name, list(shape), dt, kind="ExternalInput").ap()
    ds = lambda name, shape, dt=F32: nc.dram_tensor(name, list(shape), dt).ap()
    xo = di("xo", [NTOK, D])
    xp = di("xp", [NTOK, D])
    flag_d = di("flag", [128, 1])
    cT_d = di("cT", [128, 32])
    w_ada = di("w_ada", [D, 6 * D])
    b_adaT_d = di("b_adaT", [128, 64])
    vec_bc = di("vec_bc", [7, 128, D])
    gpreT_d = di("gpreT", [128, 32])
    w_in = di("w_in", [D, 10240])
    out_d = nc.dram_tensor("out", [NTOK, D], F32, kind="ExternalOutput").ap()
    w_out = di("w_out", [D, D])
    bgluT_d = di("bgluT", [128, 32])
    cwT_d = di("cwT", [128, 16 * 31])
    cvec_d = di("cvec", [128, 48])
    consts_d = di("consts", [6, 128, 128])
    w_router = di("w_router", [D, 32])
    brt_d = di("brt", [128, 32])
    iota_d = di("iota", [128, CAP])
    tid_d = di("tid", [128, 8 * 5])
    w_gu = di("w_gu", [32, D, 3072]) if stage >= 6 else None
    bguT_d = di("bguT", [32, 128, 24])
    w_dn = di("w_dn", [32, 1536, D]) if stage >= 6 else None
    b_dn = di("b_dn", [32, D])
    Md = ds("Md", [NTOK, D])
    X1d = ds("X1d", [NTOK, D])
    Fd = [ds("Fd%d" % c, [NTOK + 1, 512]) for c in range(8)]

    BC = ds("BC", [4, 128, D])
    VG = ds("VG", [D, VGW])
    QT = ds("QT", [16, 128, NTOK], BF16)
    KT = ds("KT", [16, 128, 2 * NTOK], BF16)
    Vd = ds("Vd", [2 * NTOK, 2048], BF16)

    dbg_out = {}

    def dbg_tensor(name, shape, dt):
        dbg_out[name] = nc.dram_tensor("dbg_" + name, list(shape), dt, kind="ExternalOutput").ap()
        return dbg_out[name]

    P = Prog()
    A = Arena(nc)
    out_keys = []
    with ExitStack() as st:
        bank = [st.enter_context(nc.psum_tensor("bank%d" % i, [128, 512], F32)) for i in range(8)]

        ident = A.alloc([128, 128], BF16, "ident")
        flag = A.alloc([128, 1], F32, "flag")
        cT = A.alloc([128, 32], F32, "cT")
        sc = A.alloc([128, 32], BF16, "sc")
        b_adaT = A.alloc([128, 64], F32, "b_adaT")
        gpreT = A.alloc([128, 32], F32, "gpreT")
        modT = A.alloc([128, 64], F32, "modT")
        A1 = A.alloc([128, 32], F32, "A1")
        small = A.alloc([128, 64], F32, "small")
        wbuf = [A.alloc([128, 32, 256], BF16, "wbuf%d" % i) for i in range(2)]
        hT_off = A.top
        hT = A.alloc([128, 32, 2 * NTOK], BF16, "hT")
        regionT = A.top

        P.add("pool", lambda e: e.memset(ident[:], 1.0), writes=["ident"])
        P.add("pool", lambda e: e.affine_select(out=ident[:], in_=ident[:], pattern=[[-1, 128]],
                                                compare_op=ALU.is_equal, fill=0.0, base=0, channel_multiplier=1),
              reads=["ident"], writes=["ident"])
        P.add("sp", lambda e: e.dma_start(out=flag[:], in_=flag_d), writes=["flag"], dma="c0")
        P.add("sp", lambda e: e.dma_start(out=cT[:], in_=cT_d), writes=["cT"], dma="c1")
        P.add("sp", lambda e: e.dma_start(out=b_adaT[:], in_=b_adaT_d), writes=["b_adaT"], dma="c2")
        P.add("sp", lambda e: e.dma_start(out=gpreT[:], in_=gpreT_d), writes=["gpreT"], dma="c3")

        screp = A.alloc([128, 32, 128], BF16, "screp")
        bch = [A.alloc([128, 256], F32, "bch%d" % i) for i in range(2)]
        gch = [A.alloc([128, 256], F32, "gch%d" % i) for i in range(2)]
        och = [A.alloc([128, 256], F32, "och%d" % i) for i in range(2)]
        tch = [A.alloc([128, 256], F32, "tch%d" % i) for i in range(2)]
        P.add("act", lambda e: e.activation(out=sc[:], in_=cT[:], func=AF.Silu), reads=["cT"], writes=["sc"])
        P.add("dve", lambda e: e.tensor_copy(out=screp[:], in_=sc[:].unsqueeze(2).to_broadcast([128, 32, 128])),
              reads=["sc"], writes=["screp"])
        wv_ada = w_ada.rearrange("(kc p) n -> p kc n", p=128)
        NCH_ADA = nada
        for ci in range(NCH_ADA):
            b = ci % 2
            P.add("pool", lambda e, b=b, ci=ci: e.dma_start(out=wbuf[b][:], in_=wv_ada[:, :, ci * 256:(ci + 1) * 256]),
                  writes=[("wbuf", b)], dma=("wbuf", b))
            if ci < 32:
                for sub in range(2):
                    cc = ci * 2 + sub
                    for kd in range(32):
                        P.add("pe", lambda e, b=b, sub=sub, kd=kd, cc=cc: e.matmul(
                            bank[0][:, cc:cc + 1], lhsT=wbuf[b][:, kd, sub * 128:(sub + 1) * 128], rhs=sc[:, kd:kd + 1],
                            start=(kd == 0), stop=(kd == 31)),
                            reads=[("wbuf", b), "sc"], writes=["bank0"])
                if ci == 31:
                    P.add("dve", lambda e: e.tensor_tensor(out=modT[:], in0=bank[0][:, 0:64], in1=b_adaT[:], op=ALU.add),
                          reads=["bank0", "b_adaT"], writes=["modT"])
                    P.add("dve", lambda e: e.scalar_tensor_tensor(out=A1[:], in0=modT[:, 32:64], scalar=1.0, in1=gpreT[:],
                                                                  op0=ALU.add, op1=ALU.mult),
                          reads=["modT", "gpreT"], writes=["A1"])
            else:
                j = (ci - 32) // 16
                c0 = ((ci - 32) % 16) * 256
                pb = 1 + (ci % 2)
                for kd in range(32):
                    P.add("pe", lambda e, b=b, kd=kd, pb=pb: e.matmul(
                        bank[pb][:, 0:256], lhsT=screp[:, kd, :], rhs=wbuf[b][:, kd, :],
                        start=(kd == 0), stop=(kd == 31)),
                        reads=[("wbuf", b), "screp"], writes=[("bank", pb)])
                P.add("sp", lambda e, b=b, j=j, c0=c0: e.dma_start(out=bch[b][:], in_=vec_bc[j, :, c0:c0 + 256]),
                      writes=[("bch", b)], dma=("bch", b))
                if j != 1:
                    gi = {0: 4, 2: 5, 3: 6}[j]
                    P.add("sp", lambda e, b=b, gi=gi, c0=c0: e.dma_start(out=gch[b][:], in_=vec_bc[gi, :, c0:c0 + 256]),
                          writes=[("gch", b)], dma=("gch", b))
                if j == 1:
                    P.add("dve", lambda e, b=b, pb=pb: e.tensor_tensor(out=och[b][:], in0=bank[pb][:, 0:256], in1=bch[b][:],
                                                                      op=ALU.add),
                          reads=[("bank", pb), ("bch", b)], writes=[("och", b)])
                else:
                    addc = 1.0 if j == 2 else 0.0
                    P.add("dve", lambda e, b=b, pb=pb, addc=addc: e.scalar_tensor_tensor(
                        out=tch[b][:], in0=bank[pb][:, 0:256], scalar=addc, in1=bch[b][:], op0=ALU.add, op1=ALU.add),
                        reads=[("bank", pb), ("bch", b)], writes=[("tch", b)])
                    P.add("dve", lambda e, b=b: e.tensor_tensor(out=och[b][:], in0=tch[b][:], in1=gch[b][:], op=ALU.mult),
                          reads=[("tch", b), ("gch", b)], writes=[("och", b)])
                P.add("sp", lambda e, b=b, j=j, c0=c0: e.dma_start(out=BC[j, :, c0:c0 + 256], in_=och[b][:]),
                      reads=[("och", b)], writes=[("BC", j, c0)], dma=("och", b))
        A.top = regionT

        xt = A.alloc([128, D], F32, "xt")
        xs = A.alloc([128, D], BF16, "xs")
        P.add("dve", lambda e: e.engine_nop(),
              writes=[("bch", 0), ("bch", 1), ("gch", 0), ("gch", 1), ("och", 0), ("och", 1), ("tch", 0), ("tch", 1), "screp",
                      "xt", "xs"])
        ev = 0
        for tb in range(ntb):
            src = xp if tb < 8 else xo
            r0 = (tb % 8) * 128
            P.add("sp", lambda e, src=src, r0=r0: e.dma_start(out=xt[:], in_=src[r0:r0 + 128, :]), writes=["xt"], dma="xt")
            P.add("act", lambda e: e.activation(out=xs[:], in_=xt[:], func=AF.Square, accum_out=small[:, 0:1]),
                  reads=["xt"], writes=["xs", "ss"])
            P.add("act", lambda e: e.activation(out=small[:, 1:2], in_=small[:, 0:1], func=AF.Sqrt, scale=1.0 / D, bias=1e-6),
                  reads=["ss"], writes=["sq"])
            P.add("dve", lambda e: e.reciprocal(out=small[:, 2:3], in_=small[:, 1:2]), reads=["sq"], writes=["rstd"])
            P.add("act", lambda e: e.activation(out=xs[:], in_=xt[:], func=AF.Copy, scale=small[:, 2:3]),
                  reads=["xt", "rstd"], writes=["xs"])
            for kc in range(32):
                pb = 3 + kc % 4
                slot = 0
                pt = bank[pb][:, 0:64].bitcast(BF16)
                P.add("pe", lambda e, kc=kc, pt=pt: e.transpose(out=pt, in_=xs[:, kc * 128:(kc + 1) * 128], identity=ident[:]),
                      reads=["xs", "ident"], writes=[("pt", pb, slot)])
                dst = hT[:, kc, tb * 128:(tb + 1) * 128]
                if ev % 2 == 0:
                    P.add("act", lambda e, kc=kc, pt=pt, dst=dst: e.activation(
                        out=dst, in_=pt, func=AF.Identity, scale=A1[:, kc:kc + 1], bias=modT[:, kc:kc + 1]),
                        reads=[("pt", pb, slot), "A1", "modT"], writes=[("hT", tb)])
                else:
                    P.add("dve", lambda e, kc=kc, pt=pt, dst=dst: e.tensor_scalar(
                        out=dst, in0=pt, scalar1=A1[:, kc:kc + 1], scalar2=modT[:, kc:kc + 1], op0=ALU.mult, op1=ALU.add),
                        reads=[("pt", pb, slot), "A1", "modT"], writes=[("hT", tb)])
                ev += 1
        A.top = regionT

        def dump(nm, shp, dt, parts, keys):
            o = dbg_tensor(nm, shp, dt)
            for i, (dst_fn, src_fn) in enumerate(parts):
                P.add("sp", lambda e, o=o, dst_fn=dst_fn, src_fn=src_fn: e.dma_start(out=dst_fn(o), in_=src_fn()),
                      reads=keys, writes=[("dbg_" + nm, i)], dma=("dbg", i % 2))
                out_keys.append(("dbg_" + nm, i))

        if "modT" in dbg:
            dump("modT", [128, 64], F32, [((lambda o: o), (lambda: modT[:]))], ["modT"])
        if "hT" in dbg:
            dump("hT", [128, 32, 2 * NTOK], BF16,
                 [((lambda o, kc=kc: o[:, kc, :]), (lambda kc=kc: hT[:, kc, :])) for kc in range(32)],
                 [("hT", tb) for tb in range(16)])
        if "BC" in dbg:
            dump("BC", [4, 128, D], F32,
                 [((lambda o, j=j: o[j]), (lambda j=j: BC[j])) for j in range(4)],
                 [("BC", j, c0) for j in range(4) for c0 in range(0, D, 256)])

        if stage >= 2:
            stg = [A.alloc([128, VGW], F32, "stg%d" % i) for i in range(2)]
            stgk = [A.alloc([128, 2 * NTOK], BF16, "stgk%d" % i) for i in range(2)]
            stgv = [A.alloc([128, 256], BF16, "stgv%d" % i) for i in range(2)]
            P.add("dve", lambda e: e.engine_nop(),
                  writes=["xt", "xs", ("stg", 0), ("stg", 1), ("stgk", 0), ("stgk", 1), ("stgv", 0), ("stgv", 1)])
            wv_in = w_in.rearrange("(kc p) n -> p kc n", p=128)
            allh = [("hT", tb) for tb in range(16)]
            pbc = 0
            sg = 0
            sgk = 0
            sgv = 0
            ev = 0

            def evac(dst, srcp, rk, wk, scale=None):
                nonlocal ev
                if ev % 2 == 0:
                    if scale is None:
                        P.add("act", lambda e: e.copy(out=dst, in_=srcp), reads=rk, writes=wk)
                    elif isinstance(scale, float):
                        P.add("act", lambda e: e.activation(out=dst, in_=srcp, func=AF.Copy, scale=scale), reads=rk, writes=wk)
                    else:
                        P.add("act", lambda e: e.activation(out=dst, in_=srcp, func=AF.Copy, scale=scale),
                              reads=rk + ["flag"], writes=wk)
                else:
                    if scale is None:
                        P.add("dve", lambda e: e.tensor_copy(out=dst, in_=srcp), reads=rk, writes=wk)
                    elif isinstance(scale, float):
                        P.add("dve", lambda e: e.tensor_single_scalar(out=dst, in_=srcp, scalar=scale, op=ALU.mult),
                              reads=rk, writes=wk)
                    else:
                        P.add("dve", lambda e: e.tensor_scalar(out=dst, in0=srcp, scalar1=scale, scalar2=None, op0=ALU.mult),
                              reads=rk + ["flag"], writes=wk)
                ev += 1

            for ci in range(40):
                b = ci % 2
                P.add("pool", lambda e, b=b, ci=ci: e.dma_start(out=wbuf[b][:], in_=wv_in[:, :, ci * 256:(ci + 1) * 256]),
                      writes=[("wbuf", b)], dma=("wbuf", b))
                kind = ci // 8
                if kind <= 1:
                    for sub in range(2):
                        s_ = sg % 2
                        sg += 1
                        row0 = ci * 256 + sub * 128
                        for (t0, n, o0) in ((NTOK - HALO, HALO, 0), (NTOK, 512, HALO), (NTOK + 512, 512, HALO + 512)):
                            pb = pbc % 8
                            pbc += 1
                            for kc in range(32):
                                P.add("pe", lambda e, b=b, sub=sub, kc=kc, pb=pb, t0=t0, n=n: e.matmul(
                                    bank[pb][:, 0:n], lhsT=wbuf[b][:, kc, sub * 128:(sub + 1) * 128], rhs=hT[:, kc, t0:t0 + n],
                                    start=(kc == 0), stop=(kc == 31)),
                                    reads=[("wbuf", b)] + allh, writes=[("bank", pb)])
                            evac(stg[s_][:, o0:o0 + n], bank[pb][:, 0:n], [("bank", pb)], [("stg", s_)])
                        P.add("sp", lambda e, s_=s_, row0=row0: e.dma_start(out=VG[row0:row0 + 128, :], in_=stg[s_][:]),
                              reads=[("stg", s_)], writes=[("VG", row0)], dma=("stg", s_))
                elif kind <= 3:
                    isq = kind == 2
                    for sub in range(2):
                        head = (ci % 8) * 2 + sub
                        s_ = sgk % 2
                        sgk += 1
                        groups = ((NTOK, 0), (NTOK + 512, 512)) if isq else ((0, 0), (512, 512), (1024, 1024), (1536, 1536))
                        for (t0, o0) in groups:
                            pb = pbc % 8
                            pbc += 1
                            for kc in range(32):
                                P.add("pe", lambda e, b=b, sub=sub, kc=kc, pb=pb, t0=t0: e.matmul(
                                    bank[pb][:, 0:512], lhsT=wbuf[b][:, kc, sub * 128:(sub + 1) * 128], rhs=hT[:, kc, t0:t0 + 512],
                                    start=(kc == 0), stop=(kc == 31)),
                                    reads=[("wbuf", b)] + allh, writes=[("bank", pb)])
                            evac(stgk[s_][:, o0:o0 + 512], bank[pb][:, 0:512], [("bank", pb)], [("stgk", s_)],
                                 scale=(DH_SCALE if isq else None))
                        if isq:
                            P.add("sp", lambda e, s_=s_, head=head: e.dma_start(out=QT[head], in_=stgk[s_][:, 0:NTOK]),
                                  reads=[("stgk", s_)], writes=[("QT", head)], dma=("stgk", s_))
                        else:
                            P.add("sp", lambda e, s_=s_, head=head: e.dma_start(out=KT[head], in_=stgk[s_][:]),
                                  reads=[("stgk", s_)], writes=[("KT", head)], dma=("stgk", s_))
                else:
                    c0 = (ci - 32) * 256
                    for tb in range(16):
                        pb = pbc % 8
                        pbc += 1
                        s_ = sgv % 2
                        sgv += 1
                        for kc in range(32):
                            P.add("pe", lambda e, b=b, kc=kc, pb=pb, tb=tb: e.matmul(
                                bank[pb][:, 0:256], lhsT=hT[:, kc, tb * 128:(tb + 1) * 128], rhs=wbuf[b][:, kc, :],
                                start=(kc == 0), stop=(kc == 31)),
                                reads=[("wbuf", b), ("hT", tb)], writes=[("bank", pb)])
                        evac(stgv[s_][:], bank[pb][:, 0:256], [("bank", pb)], [("stgv", s_)],
                             scale=(flag[:, 0:1] if tb < 8 else None))
                        P.add("sp", lambda e, s_=s_, tb=tb, c0=c0: e.dma_start(out=Vd[tb * 128:(tb + 1) * 128, c0:c0 + 256],
                                                                             in_=stgv[s_][:]),
                              reads=[("stgv", s_)], writes=[("Vd", tb, c0)], dma=("stgv", s_))
            A.top = regionT
            if "VG" in dbg:
                dump("VG", [D, VGW], F32,
                     [((lambda o, r=r: o[r * 128:(r + 1) * 128, :]), (lambda r=r: VG[r * 128:(r + 1) * 128, :])) for r in range(32)],
                     [("VG", r) for r in range(0, D, 128)])
            if "QT" in dbg:
                dump("QT", [16, 128, NTOK], BF16, [((lambda o, h=h: o[h]), (lambda h=h: QT[h])) for h in range(16)],
                     [("QT", h) for h in range(16)])
            if "KT" in dbg:
                dump("KT", [16, 128, 2 * NTOK], BF16, [((lambda o, h=h: o[h]), (lambda h=h: KT[h])) for h in range(16)],
                     [("KT", h) for h in range(16)])
            if "Vd" in dbg:
                dump("Vd", [2 * NTOK, 2048], BF16,
                     [((lambda o, r=r: o[r * 128:(r + 1) * 128, :]), (lambda r=r: Vd[r * 128:(r + 1) * 128, :])) for r in range(16)],
                     [("Vd", tb, c0) for tb in range(16) for c0 in range(0, 2048, 256)])

        if stage >= 3:
            A.top = hT_off
            oldk = [("hT", tb) for tb in range(16)] + [("stg", 0), ("stg", 1), ("stgk", 0), ("stgk", 1), ("stgv", 0),
                                                      ("stgv", 1), "xt", "xs"]
            mixT = A.alloc([128, 32, NTOK], BF16, "mixT")
            bgluT = A.alloc([128, 32], F32, "bgluT")
            cwT = A.alloc([128, 16 * 31], F32, "cwT")
            cvec = A.alloc([128, 48], F32, "cvec")
            cst = A.alloc([128, 6, 128], F32, "cst")
            cstb = A.alloc([128, 6, 128], BF16, "cstb")
            P3top = A.top
            cv_all = A.alloc([128, 16, NTOK], F32, "cv_all")
            vt = [A.alloc([128, VGW], F32, "vt%d" % i) for i in range(2)]
            gt = [A.alloc([128, VGW], F32, "gt%d" % i) for i in range(2)]
            ut = A.alloc([128, VGW], F32, "ut")
            sq = A.alloc([128, NTOK], F32, "sq")
            mixk = [("mix", c) for c in range(32)]
            newk = mixk + ["bgluT", "cwT", "cvec", "cst", "cstb", "ut", "sq", ("vt", 0), ("vt", 1), ("gt", 0), ("gt", 1)] + \
                [("cv", g) for g in range(16)]
            P.add("dve", lambda e: e.engine_nop(), writes=oldk + newk)
            P.add("sp", lambda e: e.dma_start(out=bgluT[:], in_=bgluT_d), writes=["bgluT"], dma="c0")
            P.add("sp", lambda e: e.dma_start(out=cwT[:], in_=cwT_d), writes=["cwT"], dma="c1")
            P.add("sp", lambda e: e.dma_start(out=cvec[:], in_=cvec_d), writes=["cvec"], dma="c2")
            P.add("sp", lambda e: e.dma_start(out=cst[:], in_=consts_d.rearrange("c p n -> p c n")), writes=["cst"], dma="c3")
            P.add("dve", lambda e: e.tensor_copy(out=cstb[:], in_=cst[:]), reads=["cst"], writes=["cstb"])
            ones_f = cst[:, 0, :]
            ntri_f = cst[:, 1, :]
            maskT_f = cst[:, 2, :]
            nones_f = cst[:, 3, :]
            triS_b = cstb[:, 4, :]
            ones_b = cstb[:, 0, :]
            for g in range(16):
                i = g % 2
                P.add("sp", lambda e, g=g, i=i: e.dma_start(out=vt[i][:], in_=VG[g * 128:(g + 1) * 128, :]),
                      reads=[("VG", g * 128)], writes=[("vt", i)], dma=("vt", i))
                P.add("sp", lambda e, g=g, i=i: e.dma_start(out=gt[i][:], in_=VG[2048 + g * 128:2048 + (g + 1) * 128, :]),
                      reads=[("VG", 2048 + g * 128)], writes=[("gt", i)], dma=("gt", i))
                P.add("act", lambda e, g=g, i=i: e.activation(out=gt[i][:], in_=gt[i][:], func=AF.Sigmoid,
                                                              bias=bgluT[:, 16 + g:17 + g]),
                      reads=[("gt", i), "bgluT"], writes=[("gt", i)])
                P.add("dve", lambda e, g=g, i=i: e.scalar_tensor_tensor(out=ut[:], in0=vt[i][:], scalar=bgluT[:, g:g + 1],
                                                                        in1=gt[i][:], op0=ALU.add, op1=ALU.mult),
                      reads=[("vt", i), ("gt", i), "bgluT"], writes=["ut"])
                P.add("dve", lambda e: e.tensor_scalar(out=ut[:, 0:HALO], in0=ut[:, 0:HALO], scalar1=flag[:, 0:1], scalar2=None,
                                                       op0=ALU.mult), reads=["ut", "flag"], writes=["ut"])
                o0 = HALO - 30
                P.add("dve", lambda e, g=g, o0=o0: e.tensor_scalar(out=cv_all[:, g, :], in0=ut[:, o0:o0 + NTOK],
                                                                   scalar1=cwT[:, g * 31:g * 31 + 1], scalar2=cvec[:, g:g + 1],
                                                                   op0=ALU.mult, op1=ALU.add),
                      reads=["ut", "cwT", "cvec"], writes=[("cv", g)])
                for j in range(1, 31):
                    P.add("dve", lambda e, g=g, j=j, o0=o0: e.scalar_tensor_tensor(
                        out=cv_all[:, g, :], in0=ut[:, o0 + j:o0 + j + NTOK], scalar=cwT[:, g * 31 + j:g * 31 + j + 1],
                        in1=cv_all[:, g, :], op0=ALU.mult, op1=ALU.add),
                        reads=["ut", "cwT", ("cv", g)], writes=[("cv", g)])
                P.add("act", lambda e, g=g: e.activation(out=sq[:], in_=cv_all[:, g, :], func=AF.Square),
                      reads=[("cv", g)], writes=["sq"])
                for hf in range(2):
                    P.add("pe", lambda e, g=g, hf=hf: e.matmul(bank[hf][:, 0:512], lhsT=ones_f,
                                                               rhs=cv_all[:, g, hf * 512:(hf + 1) * 512],
                                                               start=(g == 0), stop=(g == 15)),
                          reads=[("cv", g), "cst"], writes=[("bank", hf)])
                    P.add("pe", lambda e, g=g, hf=hf: e.matmul(bank[2 + hf][:, 0:512], lhsT=ones_f,
                                                               rhs=sq[:, hf * 512:(hf + 1) * 512],
                                                               start=(g == 0), stop=(g == 15)),
                          reads=["sq", "cst"], writes=[("bank", 2 + hf)])
            mean = vt[0]
            var = vt[1]
            nmr = gt[0]
            tmp = gt[1]
            for hf in range(2):
                sl = slice(hf * 512, (hf + 1) * 512)
                P.add("act", lambda e, hf=hf, sl=sl: e.activation(out=mean[:, sl], in_=bank[hf][:, 0:512], func=AF.Copy,
                                                                  scale=1.0 / 2048),
                      reads=[("bank", hf)], writes=[("vt", 0)])
                P.add("dve", lambda e, hf=hf, sl=sl: e.tensor_single_scalar(out=var[:, sl], in_=bank[2 + hf][:, 0:512],
                                                                            scalar=1.0 / 2048, op=ALU.mult),
                      reads=[("bank", 2 + hf)], writes=[("vt", 1)])
            P.add("dve", lambda e: e.tensor_tensor(out=tmp[:, 0:NTOK], in0=mean[:, 0:NTOK], in1=mean[:, 0:NTOK], op=ALU.mult),
                  reads=[("vt", 0)], writes=[("gt", 1)])
            P.add("dve", lambda e: e.tensor_tensor(out=var[:, 0:NTOK], in0=var[:, 0:NTOK], in1=tmp[:, 0:NTOK], op=ALU.subtract),
                  reads=[("vt", 1), ("gt", 1)], writes=[("vt", 1)])
            P.add("act", lambda e: e.activation(out=var[:, 0:NTOK], in_=var[:, 0:NTOK], func=AF.Sqrt, bias=1e-5),
                  reads=[("vt", 1)], writes=[("vt", 1)])
            P.add("dve", lambda e: e.reciprocal(out=var[:, 0:NTOK], in_=var[:, 0:NTOK]), reads=[("vt", 1)], writes=[("vt", 1)])
            P.add("dve", lambda e: e.scalar_tensor_tensor(out=nmr[:, 0:NTOK], in0=mean[:, 0:NTOK], scalar=-1.0,
                                                          in1=var[:, 0:NTOK], op0=ALU.mult, op1=ALU.mult),
                  reads=[("vt", 0), ("vt", 1)], writes=[("gt", 0)])
            for g in range(16):
                P.add("dve", lambda e, g=g: e.tensor_tensor(out=cv_all[:, g, :], in0=cv_all[:, g, :], in1=var[:, 0:NTOK],
                                                            op=ALU.mult), reads=[("cv", g), ("vt", 1)], writes=[("cv", g)])
                P.add("dve", lambda e, g=g: e.tensor_tensor(out=cv_all[:, g, :], in0=cv_all[:, g, :], in1=nmr[:, 0:NTOK],
                                                            op=ALU.add), reads=[("cv", g), ("gt", 0)], writes=[("cv", g)])
                P.add("act", lambda e, g=g: e.activation(out=mixT[:, g, :], in_=cv_all[:, g, :], func=AF.Silu,
                                                         scale=cvec[:, 16 + g:17 + g], bias=cvec[:, 32 + g:33 + g]),
                      reads=[("cv", g), "cvec"], writes=[("mix", g)])
            A.top = P3top

        if stage >= 4:
            qt = [A.alloc([128, NTOK], BF16, "qt%d" % i) for i in range(2)]
            kt = [A.alloc([128, 2 * NTOK], BF16, "kt%d" % i) for i in range(2)]
            vv = [A.alloc([128, 16, 128], BF16, "vv%d" % i) for i in range(2)]
            Rt = A.alloc([128, 128], F32, "Rt")
            wk = [[A.alloc([128, 128], F32, "wk%d_%d" % (i, j)) for j in range(4)] for i in range(4)]
            ab = [A.alloc([128, 128], BF16, "ab%d" % i) for i in range(4)]
            oldk = [("cv", g) for g in range(16)] + ["ut", "sq", ("vt", 0), ("vt", 1), ("gt", 0), ("gt", 1)]
            newk = [("qt", 0), ("qt", 1), ("kt", 0), ("kt", 1), ("vv", 0), ("vv", 1), "Rt", "ab0", "ab1", "ab2", "ab3"] + \
                [("wk", i, j) for i in range(4) for j in range(4)]
            P.add("dve", lambda e: e.engine_nop(), writes=oldk + newk)
            pc = 0
            for h in range(16):
                i = h % 2
                P.add("sp", lambda e, h=h, i=i: e.dma_start(out=qt[i][:], in_=QT[h]), reads=[("QT", h)], writes=[("qt", i)],
                      dma=("qt", i))
                P.add("sp", lambda e, h=h, i=i: e.dma_start(out=kt[i][:], in_=KT[h]), reads=[("KT", h)], writes=[("kt", i)],
                      dma=("kt", i))
                P.add("sp", lambda e, h=h, i=i: e.dma_start(
                    out=vv[i][:], in_=Vd[:, h * 128:(h + 1) * 128].rearrange("(blk p) d -> p blk d", p=128)),
                    reads=[("Vd", tb, (h // 2) * 256) for tb in range(16)], writes=[("vv", i)], dma=("vv", i))
                pairs = []
                for qb in range(8):
                    nkb = 9 + qb
                    for n_, gkb in enumerate(range(8 + qb, -1, -1)):
                        pairs.append((qb, gkb, n_ == 0, n_ == nkb - 1))

                def emitA(p_, i=i, h=h):
                    qb, gkb, first, last, w = p_
                    zb = ("z", w)
                    tb_ = ("tri", w)
                    nb = ("ones", w)
                    zA = bank[0 + w // 2][:, (w % 2) * 128:(w % 2) * 128 + 128]
                    tA = bank[2 + w // 2][:, (w % 2) * 128:(w % 2) * 128 + 128]
                    nA = bank[4 + w // 2][:, (w % 2) * 128:(w % 2) * 128 + 128]
                    ex, sp_, lg_, aa = wk[w]
                    P.add("pe", lambda e: e.matmul(zA, lhsT=kt[i][:, gkb * 128:(gkb + 1) * 128],
                                                   rhs=qt[i][:, qb * 128:(qb + 1) * 128], start=True, stop=True),
                          reads=[("kt", i), ("qt", i)], writes=[zb])
                    P.add("act", lambda e: e.activation(out=ex[:], in_=zA, func=AF.Exp), reads=[zb], writes=[("wk", w, 0)])
                    P.add("act", lambda e: e.activation(out=sp_[:], in_=ex[:], func=AF.Ln, bias=1.0),
                          reads=[("wk", w, 0)], writes=[("wk", w, 1)])
                    if first:
                        P.add("dve", lambda e: e.tensor_tensor(out=ex[:], in0=sp_[:], in1=maskT_f, op=ALU.mult),
                              reads=[("wk", w, 1), "cst"], writes=[("wk", w, 0)])
                        spm, spk = ex, ("wk", w, 0)
                    else:
                        spm, spk = sp_, ("wk", w, 1)
                    P.add("pe", lambda e: e.matmul(tA, lhsT=ntri_f, rhs=spm[:], start=True, stop=True),
                          reads=[spk, "cst"], writes=[tb_])
                    P.add("pe", lambda e: e.matmul(nA, lhsT=nones_f, rhs=spm[:], start=True, stop=True),
                          reads=[spk, "cst"], writes=[nb])
                    P.add("dve", lambda e: e.tensor_tensor(out=lg_[:], in0=zA, in1=sp_[:], op=ALU.subtract),
                          reads=[zb, ("wk", w, 1)], writes=[("wk", w, 2)])
                    P.add("dve", lambda e: e.tensor_tensor(out=lg_[:], in0=lg_[:], in1=tA, op=ALU.add),
                          reads=[tb_, ("wk", w, 2)], writes=[("wk", w, 2)])

                def emitB(p_, i=i, h=h):
                    qb, gkb, first, last, w = p_
                    nb = ("ones", w)
                    nA = bank[4 + w // 2][:, (w % 2) * 128:(w % 2) * 128 + 128]
                    ob = 6 + qb % 2
                    ex, sp_, lg_, aa = wk[w]
                    if not first:
                        P.add("dve", lambda e: e.tensor_tensor(out=lg_[:], in0=lg_[:], in1=Rt[:], op=ALU.add),
                              reads=["Rt", ("wk", w, 2)], writes=[("wk", w, 2)])
                        P.add("act", lambda e: e.activation(out=ab[w][:], in_=lg_[:], func=AF.Exp),
                              reads=[("wk", w, 2)], writes=["ab%d" % w])
                    else:
                        P.add("act", lambda e: e.activation(out=aa[:], in_=lg_[:], func=AF.Exp),
                              reads=[("wk", w, 2)], writes=[("wk", w, 3)])
                        P.add("dve", lambda e: e.tensor_tensor(out=ab[w][:], in0=aa[:], in1=maskT_f, op=ALU.mult),
                              reads=[("wk", w, 3), "cst"], writes=["ab%d" % w])
                    P.add("pe", lambda e: e.matmul(bank[ob][:, 0:128], lhsT=vv[i][:, gkb, :], rhs=ab[w][:], start=first, stop=last),
                          reads=[("vv", i), "ab%d" % w], writes=[("bank", ob)])
                    if not last:
                        if first:
                            P.add("dve", lambda e: e.tensor_copy(out=Rt[:], in_=nA), reads=[nb], writes=["Rt"])
                        else:
                            P.add("dve", lambda e: e.tensor_tensor(out=Rt[:], in0=Rt[:], in1=nA, op=ALU.add),
                                  reads=[nb, "Rt"], writes=["Rt"])
                    else:
                        P.add("act", lambda e: e.copy(out=mixT[:, 16 + h, qb * 128:(qb + 1) * 128], in_=bank[ob][:, 0:128]),
                              reads=[("bank", ob)], writes=[("mix", 16 + h)])

                plist = []
                for p_ in pairs:
                    plist.append(p_ + (pc % 4,))
                    pc += 1
                SK = 3
                for n_ in range(min(SK, len(plist))):
                    emitA(plist[n_])
                for n_ in range(len(plist)):
                    if n_ + SK < len(plist):
                        emitA(plist[n_ + SK])
                    emitB(plist[n_])
            A.top = P3top
            if "mixT" in dbg:
                dump("mixT", [128, 32, NTOK], BF16,
                     [((lambda o, c=c: o[:, c, :]), (lambda c=c: mixT[:, c, :])) for c in range(32)], mixk)

        if stage >= 5:
            mst = [A.alloc([128, 256], F32, "mst%d" % i) for i in range(2)]
            junk = A.alloc([128, 256], F32, "junk")
            ssq = A.alloc([128, 8, 16], F32, "ssq")
            oldk = [("qt", 0), ("qt", 1), ("kt", 0), ("kt", 1), ("vv", 0), ("vv", 1), "Rt", "ab0", "ab1", "ab2", "ab3"] + \
                [("wk", i, j) for i in range(4) for j in range(4)]
            P.add("dve", lambda e: e.engine_nop(), writes=oldk + [("mst", 0), ("mst", 1), "junk", "ssq"])
            wv_out = w_out.rearrange("(kc p) n -> p kc n", p=128)
            pbc = 0
            ms = 0
            for ci in range(16):
                b = ci % 2
                P.add("pool", lambda e, b=b, ci=ci: e.dma_start(out=wbuf[b][:], in_=wv_out[:, :, ci * 256:(ci + 1) * 256]),
                      writes=[("wbuf", b)], dma=("wbuf", b))
                for tb in range(8):
                    pb = pbc % 8
                    pbc += 1
                    s_ = ms % 2
                    ms += 1
                    for kc in range(32):
                        P.add("pe", lambda e, b=b, kc=kc, pb=pb, tb=tb: e.matmul(
                            bank[pb][:, 0:256], lhsT=mixT[:, kc, tb * 128:(tb + 1) * 128], rhs=wbuf[b][:, kc, :],
                            start=(kc == 0), stop=(kc == 31)), reads=[("wbuf", b), ("mix", kc)], writes=[("bank", pb)])
                    P.add("act", lambda e, pb=pb, s_=s_: e.copy(out=mst[s_][:], in_=bank[pb][:, 0:256]),
                          reads=[("bank", pb)], writes=[("mst", s_)])
                    P.add("act", lambda e, s_=s_, tb=tb, ci=ci: e.activation(out=junk[:], in_=mst[s_][:], func=AF.Square,
                                                                             accum_out=ssq[:, tb, ci:ci + 1]),
                          reads=[("mst", s_)], writes=["junk", "ssq"])
                    P.add("sp", lambda e, s_=s_, tb=tb, ci=ci: e.dma_start(out=Md[tb * 128:(tb + 1) * 128, ci * 256:(ci + 1) * 256],
                                                                         in_=mst[s_][:]),
                          reads=[("mst", s_)], writes=[("Md", tb)], dma=("mst", s_))
            A.top = hT_off
            h2_all = A.alloc([128, 8, D], BF16, "h2_all")
            A.top = P3top
            wr = A.alloc([128, 32, 32], BF16, "wr")
            brt = A.alloc([128, 32], F32, "brt")
            lgt = A.alloc([128, 32], F32, "lgt")
            mx8 = A.alloc([128, 8], F32, "mx8")
            mkf = A.alloc([128, 32], F32, "mkf")
            ext = A.alloc([128, 32], F32, "ext")
            G_all = A.alloc([128, 8, 32], F32, "G_all")
            mk_all = A.alloc([128, 8, 32], BF16, "mk_all")
            mkf_all = A.alloc([128, 8, 32], F32, "mkf_all")
            pos_all = A.alloc([128, 8, 32], F32, "pos_all")
            P5keep = A.top
            ggt = A.alloc([128, D], F32, "ggt")
            a2t = A.alloc([128, D], F32, "a2t")
            b2t = A.alloc([128, D], F32, "b2t")
            xm = A.alloc([128, D], F32, "xm")
            xx = A.alloc([128, D], F32, "xx")
            h2T = A.alloc([128, 32, 128], BF16, "h2T")
            P5top = A.top
            h2k = [("h2", tb) for tb in range(8)]
            P.add("dve", lambda e: e.engine_nop(),
                  writes=mixk + ["bgluT", "cwT", "cvec", ("mst", 0), ("mst", 1), "junk"] + h2k +
                  ["ggt", "a2t", "b2t", "xm", "xx", "h2T", "wr", "brt", "lgt", "mx8", "mkf", "ext", "G_all", "mk_all", "mkf_all",
                   "pos_all"])
            BCk = lambda j: [("BC", j, c0) for c0 in range(0, D, 256)]
            P.add("sp", lambda e: e.dma_start(out=ggt[:], in_=BC[0]), reads=BCk(0), writes=["ggt"], dma="c0")
            P.add("sp", lambda e: e.dma_start(out=a2t[:], in_=BC[2]), reads=BCk(2), writes=["a2t"], dma="c1")
            P.add("sp", lambda e: e.dma_start(out=b2t[:], in_=BC[1]), reads=BCk(1), writes=["b2t"], dma="c2")
            P.add("sp", lambda e: e.dma_start(out=brt[:], in_=brt_d), writes=["brt"], dma="c3")
            P.add("pool", lambda e: e.dma_start(out=wr[:], in_=w_router.rearrange("(kc p) n -> p kc n", p=128)),
                  writes=["wr"], dma="wr")
            ev = 0
            for tb in range(8):
                P.add("sp", lambda e, tb=tb: e.dma_start(out=xm[:], in_=Md[tb * 128:(tb + 1) * 128, :]), reads=[("Md", tb)],
                      writes=["xm"], dma="xm")
                P.add("sp", lambda e, tb=tb: e.dma_start(out=xx[:], in_=xo[tb * 128:(tb + 1) * 128, :]), writes=["xx"], dma="xx")
                P.add("dve", lambda e, tb=tb: e.reduce_sum(out=small[:, 8:9], in_=ssq[:, tb, :], axis=mybir.AxisListType.X),
                      reads=["ssq"], writes=["s8"])
                P.add("act", lambda e: e.activation(out=small[:, 9:10], in_=small[:, 8:9], func=AF.Sqrt, scale=1.0 / D, bias=1e-6),
                      reads=["s8"], writes=["s9"])
                P.add("dve", lambda e: e.reciprocal(out=small[:, 10:11], in_=small[:, 9:10]), reads=["s9"], writes=["s10"])
                P.add("dve", lambda e: e.scalar_tensor_tensor(out=xm[:], in0=xm[:], scalar=small[:, 10:11], in1=ggt[:],
                                                              op0=ALU.mult, op1=ALU.mult),
                      reads=["xm", "s10", "ggt"], writes=["xm"])
                P.add("dve", lambda e: e.tensor_tensor(out=xx[:], in0=xx[:], in1=xm[:], op=ALU.add), reads=["xx", "xm"],
                      writes=["xx"])
                P.add("sp", lambda e, tb=tb: e.dma_start(out=X1d[tb * 128:(tb + 1) * 128, :], in_=xx[:]), reads=["xx"],
                      writes=[("X1", tb)], dma="x1s")
                P.add("act", lambda e: e.activation(out=xm[:], in_=xx[:], func=AF.Square, accum_out=small[:, 11:12]),
                      reads=["xx"], writes=["xm", "s11"])
                P.add("act", lambda e: e.activation(out=small[:, 12:13], in_=small[:, 11:12], func=AF.Sqrt, scale=1.0 / D,
                                                    bias=1e-6), reads=["s11"], writes=["s12"])
                P.add("dve", lambda e: e.reciprocal(out=small[:, 13:14], in_=small[:, 12:13]), reads=["s12"], writes=["s13"])
                P.add("dve", lambda e: e.scalar_tensor_tensor(out=xm[:], in0=xx[:], scalar=small[:, 13:14], in1=a2t[:],
                                                              op0=ALU.mult, op1=ALU.mult),
                      reads=["xx", "s13", "a2t"], writes=["xm"])
                P.add("dve", lambda e, tb=tb: e.tensor_tensor(out=h2_all[:, tb, :], in0=xm[:], in1=b2t[:], op=ALU.add),
                      reads=["xm", "b2t"], writes=[("h2", tb)])
                for kc in range(32):
                    pb = kc % 4
                    pt = bank[pb][:, 0:64].bitcast(BF16)
                    P.add("pe", lambda e, kc=kc, pt=pt, tb=tb: e.transpose(out=pt, in_=h2_all[:, tb, kc * 128:(kc + 1) * 128],
                                                                          identity=ident[:]),
                          reads=[("h2", tb), "ident"], writes=[("bank", pb)])
                    if ev % 2 == 0:
                        P.add("act", lambda e, kc=kc, pt=pt: e.copy(out=h2T[:, kc, :], in_=pt), reads=[("bank", pb)],
                              writes=[("h2T", kc)])
                    else:
                        P.add("dve", lambda e, kc=kc, pt=pt: e.tensor_copy(out=h2T[:, kc, :], in_=pt), reads=[("bank", pb)],
                              writes=[("h2T", kc)])
                    ev += 1
                for kc in range(32):
                    P.add("pe", lambda e, kc=kc: e.matmul(bank[4][:, 0:32], lhsT=h2T[:, kc, :], rhs=wr[:, kc, :],
                                                          start=(kc == 0), stop=(kc == 31)),
                          reads=[("h2T", kc), "wr"], writes=[("bank", 4)])
                P.add("dve", lambda e: e.tensor_tensor(out=lgt[:], in0=bank[4][:, 0:32], in1=brt[:], op=ALU.add),
                      reads=[("bank", 4), "brt"], writes=["lgt"])
                P.add("dve", lambda e: e.max(out=mx8[:], in_=lgt[:]), reads=["lgt"], writes=["mx8"])
                P.add("dve", lambda e: e.tensor_single_scalar(out=small[:, 14:15], in_=mx8[:, 0:1], scalar=-1.0, op=ALU.mult),
                      reads=["mx8"], writes=["s14"])
                P.add("dve", lambda e: e.tensor_scalar(out=mkf[:], in0=lgt[:], scalar1=mx8[:, 3:4], scalar2=None, op0=ALU.is_ge),
                      reads=["lgt", "mx8"], writes=["mkf"])
                P.add("act", lambda e: e.activation(out=ext[:], in_=lgt[:], func=AF.Exp, bias=small[:, 14:15]),
                      reads=["lgt", "s14"], writes=["ext"])
                P.add("dve", lambda e: e.tensor_tensor(out=ext[:], in0=ext[:], in1=mkf[:], op=ALU.mult), reads=["ext", "mkf"],
                      writes=["ext"])
                P.add("dve", lambda e: e.reduce_sum(out=small[:, 15:16], in_=ext[:], axis=mybir.AxisListType.X), reads=["ext"],
                      writes=["s15"])
                P.add("dve", lambda e: e.reciprocal(out=small[:, 16:17], in_=small[:, 15:16]), reads=["s15"], writes=["s16"])
                P.add("dve", lambda e, tb=tb: e.tensor_scalar(out=G_all[:, tb, :], in0=ext[:], scalar1=small[:, 16:17],
                                                              scalar2=None, op0=ALU.mult),
                      reads=["ext", "s16"], writes=["G_all"])
                P.add("dve", lambda e, tb=tb: e.tensor_copy(out=mk_all[:, tb, :], in_=mkf[:]), reads=["mkf"], writes=[("mk", tb)])
                P.add("dve", lambda e, tb=tb: e.tensor_copy(out=mkf_all[:, tb, :], in_=mkf[:]), reads=["mkf"], writes=["mkf_all"])
                P.add("pe", lambda e, tb=tb: e.matmul(bank[5][:, 0:32], lhsT=triS_b, rhs=mk_all[:, tb, :], start=True,
                                                      stop=(tb == 0)), reads=[("mk", tb), "cstb"], writes=[("bank", 5)])
                for pb_ in range(tb):
                    P.add("pe", lambda e, pb_=pb_, tb=tb: e.matmul(bank[5][:, 0:32], lhsT=ones_b, rhs=mk_all[:, pb_, :],
                                                                   start=False, stop=(pb_ == tb - 1)),
                          reads=[("mk", pb_), "cstb"], writes=[("bank", 5)])
                P.add("dve", lambda e, tb=tb: e.tensor_copy(out=pos_all[:, tb, :], in_=bank[5][:, 0:32]), reads=[("bank", 5)],
                      writes=["pos_all"])
            if "X1" in dbg:
                dump("X1", [NTOK, D], F32,
                     [((lambda o, r=r: o[r * 128:(r + 1) * 128, :]), (lambda r=r: X1d[r * 128:(r + 1) * 128, :])) for r in range(8)],
                     [("X1", tb) for tb in range(8)])
            if "G" in dbg:
                dump("G", [128, 8, 32], F32, [((lambda o: o), (lambda: G_all[:]))], ["G_all"])
                dump("pos", [128, 8, 32], F32, [((lambda o: o), (lambda: pos_all[:]))], ["pos_all"])

        if stage >= 6:
            A.top = P5keep
            NSC = CAP // 128
            iot = A.alloc([128, CAP], F32, "iot")
            tidf = A.alloc([128, 8, 5], F32, "tidf")
            Rb = A.alloc([128, 8, 5], BF16, "Rb")
            ghi = A.alloc([128, 8], BF16, "ghi")
            ghf = A.alloc([128, 8], F32, "ghf")
            sel = A.alloc([128, 8, CAP], BF16, "sel")
            XeT = A.alloc([128, 32, CAP], BF16, "XeT")
            gsT = A.alloc([128, 2, CAP], BF16, "gsT")
            actT = A.alloc([128, 12, CAP], BF16, "actT")
            gm = [A.alloc([128, CAP], F32, "gm%d" % i) for i in range(2)]
            sg = A.alloc([128, CAP], F32, "sg")
            bgu = [A.alloc([128, 24], F32, "bgu%d" % i) for i in range(2)]
            bd = [A.alloc([1, 512], BF16, "bd%d" % i) for i in range(2)]
            onesr = A.alloc([1, 128], BF16, "onesr")
            wd = [A.alloc([128, 12, 256], BF16, "wd%d" % i) for i in range(2)]
            Yt = [A.alloc([128, 512], F32, "Yt%d" % i) for i in range(2 * NSC)]
            inf = [A.alloc([128, 8], F32, "inf%d" % i) for i in range(NSC)]
            idx = [A.alloc([128, 1], I32, "idx%d" % i) for i in range(NSC)]
            zt = Yt[0]
            wbuf3 = A.alloc([128, 32, 256], BF16, "wbuf3")
            wd3 = A.alloc([128, 12, 256], BF16, "wd3")
            wbm = [wbuf[0], wbuf[1], wbuf3]
            wdm = [wd[0], wd[1], wd3]
            oldk = ["ggt", "a2t", "b2t", "xm", "xx", "h2T"]
            newk = ["iot", "tidf", "Rb", "ghi", "ghf", "XeT", ("gs", 0), ("gs", 1), ("gm", 0), ("gm", 1), "sg",
                    ("bgu", 0), ("bgu", 1), ("bd", 0), ("bd", 1), "onesr", ("wd", 0), ("wd", 1), ("wd", 2), ("wbuf", 2)] + \
                [("sel", tb) for tb in range(8)] + [("XeT", dc) for dc in range(32)] + [("act", f) for f in range(12)] + \
                [("Yt", i) for i in range(2 * NSC)] + [("inf", i) for i in range(NSC)] + [("idx", i) for i in range(NSC)]
            P.add("dve", lambda e: e.engine_nop(), writes=oldk + newk)
            P.add("sp", lambda e: e.dma_start(out=iot[:], in_=iota_d), writes=["iot"], dma="c0")
            P.add("sp", lambda e: e.dma_start(out=tidf[:], in_=tid_d.rearrange("p (b c) -> p b c", c=5)), writes=["tidf"], dma="c1")
            P.add("dve", lambda e: e.tensor_copy(out=Rb[:], in_=tidf[:]), reads=["tidf"], writes=["Rb"])
            P.add("dve", lambda e: e.memset(onesr[:], 1.0), writes=["onesr"])
            P.add("dve", lambda e: e.memset(zt[:], 0.0), writes=[("Yt", 0)])
            Fk = [("Fd", c) for c in range(8)]
            for tb in range(NTOK // 128 + 1):
                rows = 128 if tb < 8 else 1
                for hf in range(8):
                    P.add("sp", lambda e, tb=tb, hf=hf, rows=rows: e.dma_start(
                        out=Fd[hf][tb * 128:tb * 128 + rows, :], in_=zt[0:rows, :]), reads=[("Yt", 0)],
                        writes=[Fk[hf]], dma="fz")
            wch = 0
            wdc = 0
            pbc = 0
            yrot = 0
            pending = []

            def flush_pending():
                while pending:
                    sci, M, yi, c0, fk = pending.pop(0)
                    P.add("pool", lambda e, sci=sci, M=M, yi=yi, c0=c0: e.indirect_dma_start(
                        out=Fd[c0 // 512][:, :], out_offset=bass.IndirectOffsetOnAxis(ap=idx[sci][0:M, 0:1], axis=0),
                        in_=Yt[yi][0:M, :], in_offset=None, compute_op=ALU.add),
                        reads=[("Yt", yi), ("idx", sci)], writes=[("Fd", fk)], dma=("scat", yi))

            slots = [(c * 128, 128) for c in range(NSC)]
            for ex_ in range(nexp):
                eb = ex_ % 2
                P.add("sp", lambda e, ex_=ex_, eb=eb: e.dma_start(out=bgu[eb][:], in_=bguT_d[ex_]), writes=[("bgu", eb)],
                      dma=("bgu", eb))
                P.add("dve", lambda e, ex_=ex_: e.tensor_copy(out=ghi[:], in_=G_all[:, :, ex_]), reads=["G_all"], writes=["ghi"])
                P.add("dve", lambda e: e.tensor_copy(out=ghf[:], in_=ghi[:]), reads=["ghi"], writes=["ghf"])
                P.add("dve", lambda e, ex_=ex_: e.tensor_tensor(out=ghf[:], in0=G_all[:, :, ex_], in1=ghf[:], op=ALU.subtract),
                      reads=["G_all", "ghf"], writes=["ghf"])
                P.add("dve", lambda e: e.tensor_copy(out=Rb[:, :, 3], in_=ghi[:]), reads=["ghi"], writes=["Rb"])
                P.add("dve", lambda e: e.tensor_copy(out=Rb[:, :, 4], in_=ghf[:]), reads=["ghf"], writes=["Rb"])
                for tb in range(8):
                    P.add("dve", lambda e, tb=tb, ex_=ex_: e.tensor_scalar(
                        out=sel[:, tb, :], in0=iot[:], scalar1=pos_all[:, tb, ex_:ex_ + 1], scalar2=mkf_all[:, tb, ex_:ex_ + 1],
                        op0=ALU.is_equal, op1=ALU.mult), reads=["iot", "pos_all", "mkf_all"], writes=[("sel", tb)])
                wv_gu = w_gu[ex_].rearrange("(kc p) n -> p kc n", p=128)
                gu_cols = []
                for pc_ in range(6):
                    gu_cols.append(pc_ * 256)
                    gu_cols.append(1536 + pc_ * 256)
                wv_dn = w_dn[ex_].rearrange("(fc p) n -> p fc n", p=128)

                def load_gu(j):
                    nonlocal wch
                    b = wch % 3
                    wch += 1
                    c0 = gu_cols[j]
                    P.add("pool", lambda e, b=b, c0=c0, wv_gu=wv_gu: e.dma_start(out=wbm[b][:], in_=wv_gu[:, :, c0:c0 + 256]),
                          writes=[("wbuf", b)], dma=("wbuf", b))
                    return b

                def load_dn(dc):
                    nonlocal wdc
                    b2 = wdc % 3
                    wdc += 1
                    P.add("pool", lambda e, b2=b2, dc=dc, wv_dn=wv_dn: e.dma_start(out=wdm[b2][:], in_=wv_dn[:, :, dc * 256:(dc + 1) * 256]),
                          writes=[("wd", b2)], dma=("wd", b2))
                    return b2

                gq = [load_gu(0), load_gu(1)]
                flush_pending()
                for dc in range(32):
                    pb = pbc % 6
                    pbc += 1
                    for tb in range(8):
                        P.add("pe", lambda e, dc=dc, tb=tb, pb=pb: e.matmul(
                            bank[pb][:, 0:CAP], lhsT=h2_all[:, tb, dc * 128:(dc + 1) * 128], rhs=sel[:, tb, :],
                            start=(tb == 0), stop=(tb == 7)), reads=[("h2", tb), ("sel", tb)], writes=[("bank", pb)])
                    if dc % 2 == 0:
                        P.add("act", lambda e, dc=dc, pb=pb: e.copy(out=XeT[:, dc, :], in_=bank[pb][:, 0:CAP]),
                              reads=[("bank", pb)], writes=[("XeT", dc)])
                    else:
                        P.add("dve", lambda e, dc=dc, pb=pb: e.tensor_copy(out=XeT[:, dc, :], in_=bank[pb][:, 0:CAP]),
                              reads=[("bank", pb)], writes=[("XeT", dc)])
                for sci, (s0, M) in enumerate(slots):
                    ib = 6 + sci % 2
                    for tb in range(8):
                        P.add("pe", lambda e, tb=tb, s0=s0, M=M, ib=ib: e.matmul(
                            bank[ib][0:M, 0:5], lhsT=sel[:, tb, s0:s0 + M], rhs=Rb[:, tb, :], start=(tb == 0), stop=(tb == 7)),
                            reads=[("sel", tb), "Rb"], writes=[("bank", ib)])
                    P.add("dve", lambda e, sci=sci, M=M, ib=ib: e.tensor_copy(out=inf[sci][0:M, 0:5], in_=bank[ib][0:M, 0:5]),
                          reads=[("bank", ib)], writes=[("inf", sci)])
                    P.add("dve", lambda e, sci=sci, M=M: e.scalar_tensor_tensor(
                        out=inf[sci][0:M, 5:6], in0=inf[sci][0:M, 0:1], scalar=128.0, in1=inf[sci][0:M, 1:2], op0=ALU.mult, op1=ALU.add),
                        reads=[("inf", sci)], writes=[("inf", sci)])
                    P.add("dve", lambda e, sci=sci, M=M: e.tensor_scalar(
                        out=inf[sci][0:M, 6:7], in0=inf[sci][0:M, 2:3], scalar1=-float(NTOK), scalar2=float(NTOK), op0=ALU.mult,
                        op1=ALU.add), reads=[("inf", sci)], writes=[("inf", sci)])
                    P.add("dve", lambda e, sci=sci, M=M: e.tensor_tensor(out=inf[sci][0:M, 5:6], in0=inf[sci][0:M, 5:6],
                                                                         in1=inf[sci][0:M, 6:7], op=ALU.add),
                          reads=[("inf", sci)], writes=[("inf", sci)])
                    P.add("dve", lambda e, sci=sci, M=M: e.tensor_copy(out=idx[sci][0:M, :], in_=inf[sci][0:M, 5:6]),
                          reads=[("inf", sci)], writes=[("idx", sci)])
                    P.add("dve", lambda e, sci=sci, M=M: e.tensor_tensor(out=inf[sci][0:M, 7:8], in0=inf[sci][0:M, 3:4],
                                                                         in1=inf[sci][0:M, 4:5], op=ALU.add),
                          reads=[("inf", sci)], writes=[("inf", sci)])
                dq = []
                for j in range(12):
                    b = gq.pop(0)
                    if j + 2 < 12:
                        gq.append(load_gu(j + 2))
                    else:
                        dq.append(load_dn(j + 2 - 12))
                    isg = j % 2 == 0
                    for sub in range(2):
                        fcb = (gu_cols[j] + sub * 128) // 128
                        f2 = (j // 2) * 2 + sub
                        pb = pbc % 6
                        pbc += 1
                        for kc in range(32):
                            P.add("pe", lambda e, b=b, sub=sub, kc=kc, pb=pb: e.matmul(
                                bank[pb][:, 0:CAP], lhsT=wbm[b][:, kc, sub * 128:(sub + 1) * 128], rhs=XeT[:, kc, :],
                                start=(kc == 0), stop=(kc == 31)), reads=[("wbuf", b), ("XeT", kc)], writes=[("bank", pb)])
                        w_ = sub
                        P.add("dve", lambda e, pb=pb, w_=w_, fcb=fcb, eb=eb: e.tensor_scalar(
                            out=gm[w_][:], in0=bank[pb][:, 0:CAP], scalar1=bgu[eb][:, fcb:fcb + 1], scalar2=7.0,
                            op0=ALU.add, op1=ALU.min), reads=[("bank", pb), ("bgu", eb)], writes=[("gm", w_)])
                        if isg:
                            P.add("act", lambda e, w_=w_: e.activation(out=sg[:], in_=gm[w_][:], func=AF.Sigmoid, scale=1.702),
                                  reads=[("gm", w_)], writes=["sg"])
                            P.add("dve", lambda e, w_=w_, sub=sub: e.tensor_tensor(out=gsT[:, sub, :], in0=gm[w_][:], in1=sg[:],
                                                                                  op=ALU.mult),
                                  reads=[("gm", w_), "sg"], writes=[("gs", sub)])
                        else:
                            P.add("dve", lambda e, w_=w_: e.tensor_scalar(out=gm[w_][:], in0=gm[w_][:], scalar1=-7.0, scalar2=1.0,
                                                                          op0=ALU.max, op1=ALU.add),
                                  reads=[("gm", w_)], writes=[("gm", w_)])
                            P.add("dve", lambda e, w_=w_, f2=f2, sub=sub: e.tensor_tensor(out=actT[:, f2, :], in0=gm[w_][:],
                                                                                         in1=gsT[:, sub, :], op=ALU.mult),
                                  reads=[("gm", w_), ("gs", sub)], writes=[("act", f2)])
                for dc in range(16):
                    b2 = dq.pop(0)
                    if dc + 2 < 16:
                        dq.append(load_dn(dc + 2))
                    flush_pending()
                    half = dc % 2
                    if half == 0:
                        bb = (dc // 2) % 2
                        P.add("pool", lambda e, ex_=ex_, bb=bb, dc=dc: e.dma_start(out=bd[bb][:], in_=b_dn[ex_:ex_ + 1, dc * 256:dc * 256 + 512]),
                              writes=[("bd", bb)], dma=("bd", bb))
                        yset = yrot % 2
                        yrot += 1
                    for sci, (s0, M) in enumerate(slots):
                        pb = pbc % 6
                        pbc += 1
                        yi = yset * NSC + sci
                        for fc in range(12):
                            P.add("pe", lambda e, b2=b2, fc=fc, pb=pb, s0=s0, M=M: e.matmul(
                                bank[pb][0:M, 0:256], lhsT=actT[:, fc, s0:s0 + M], rhs=wdm[b2][:, fc, :], start=(fc == 0), stop=False),
                                reads=[("wd", b2), ("act", fc)], writes=[("bank", pb)])
                        P.add("pe", lambda e, pb=pb, M=M, half=half, bb=bb: e.matmul(
                            bank[pb][0:M, 0:256], lhsT=onesr[0:1, 0:M], rhs=bd[bb][0:1, half * 256:(half + 1) * 256], start=False,
                            stop=True), reads=[("bd", bb), "onesr"], writes=[("bank", pb)])
                        P.add("act", lambda e, sci=sci, M=M, pb=pb, half=half, yi=yi: e.activation(
                            out=Yt[yi][0:M, half * 256:(half + 1) * 256], in_=bank[pb][0:M, 0:256], func=AF.Copy,
                            scale=inf[sci][0:M, 7:8]), reads=[("bank", pb), ("inf", sci)], writes=[("Yt", yi)])
                        if half == 1:
                            c0 = (dc // 2) * 512
                            pending.append((sci, M, yi, c0, dc // 2))
            flush_pending()
            if "F" in dbg:
                dump("F", [NTOK, D], F32,
                     [((lambda o, c=c: o[:, c * 512:(c + 1) * 512]), (lambda c=c: Fd[c][0:NTOK, :])) for c in range(8)],
                     Fk)

        if stage >= 7:
            A.top = hT_off
            g2t = A.alloc([128, D], F32, "g2t")
            fm_ = A.alloc([128, D], F32, "fm_")
            x1t = A.alloc([128, D], F32, "x1t")
            jk = A.alloc([128, D], BF16, "jk")
            P.add("dve", lambda e: e.engine_nop(), writes=[("h2", tb) for tb in range(8)] + ["g2t", "x1t", "jk"] + [("fm_", c) for c in range(8)])
            P.add("sp", lambda e: e.dma_start(out=g2t[:], in_=BC[3]), reads=[("BC", 3, c0) for c0 in range(0, D, 256)],
                  writes=["g2t"], dma="c0")
            for tb in range(8):
                for c in range(8):
                    P.add("sp", lambda e, tb=tb, c=c: e.dma_start(out=fm_[:, c * 512:(c + 1) * 512], in_=Fd[c][tb * 128:(tb + 1) * 128, :]),
                          reads=[("Fd", c)], writes=[("fm_", c)], dma=("fm_", c))
                P.add("sp", lambda e, tb=tb: e.dma_start(out=x1t[:], in_=X1d[tb * 128:(tb + 1) * 128, :]), reads=[("X1", tb)],
                      writes=["x1t"], dma="x1t")
                P.add("act", lambda e: e.activation(out=jk[:], in_=fm_[:], func=AF.Square, accum_out=small[:, 20:21]),
                      reads=[("fm_", c) for c in range(8)], writes=["jk", "s20"])
                P.add("act", lambda e: e.activation(out=small[:, 21:22], in_=small[:, 20:21], func=AF.Sqrt, scale=1.0 / D,
                                                    bias=1e-6), reads=["s20"], writes=["s21"])
                P.add("dve", lambda e: e.reciprocal(out=small[:, 22:23], in_=small[:, 21:22]), reads=["s21"], writes=["s22"])
                P.add("dve", lambda e: e.scalar_tensor_tensor(out=fm_[:], in0=fm_[:], scalar=small[:, 22:23], in1=g2t[:],
                                                              op0=ALU.mult, op1=ALU.mult),
                      reads=[("fm_", c) for c in range(8)] + ["s22", "g2t"], writes=[("fm_", c) for c in range(8)])
                P.add("dve", lambda e: e.tensor_tensor(out=x1t[:], in0=x1t[:], in1=fm_[:], op=ALU.add), reads=["x1t"] + [("fm_", c) for c in range(8)],
                      writes=["x1t"])
                P.add("sp", lambda e, tb=tb: e.dma_start(out=out_d[tb * 128:(tb + 1) * 128, :], in_=x1t[:]), reads=["x1t"],
                      writes=[("out", tb)], dma="outs")
                out_keys.append(("out", tb))

        if stage < 3:
            zt = A.alloc([128, D], F32, "zt")
            P.add("pool", lambda e: e.memset(zt[:], 0.0),
                  writes=["zt", "xt", "xs", ("stg", 0), ("stg", 1), ("stgk", 0), ("stgk", 1), ("stgv", 0), ("stgv", 1)])
            for tb in range(8):
                P.add("sp", lambda e, tb=tb: e.dma_start(out=out_d[tb * 128:(tb + 1) * 128, :], in_=zt[:]), reads=["zt"],
                      writes=[("out", tb)], dma=("out", tb % 2))
                out_keys.append(("out", tb))
        P.add("sp", lambda e: e.nop(), reads=out_keys)
        P.emit(nc)
    return nc, dbg_out


def _consts():
    i = np.arange(128)
    ones = np.ones((128, 128), np.float32)
    ntri = -(i[:, None] > i[None, :]).astype(np.float32)
    maskT = (i[None, :] > i[:, None]).astype(np.float32)
    triS = (i[:, None] < i[None, :]).astype(np.float32)
    return np.ascontiguousarray(np.stack([ones, ntri, maskT, -ones, triS, ones]))


def _tid():
    t = np.zeros((128, 8, 5), np.float32)
    t[:, :, 0] = np.arange(8, dtype=np.float32)[None, :]
    t[:, :, 1] = np.arange(128, dtype=np.float32)[:, None]
    t[:, :, 2] = 1.0
    return np.ascontiguousarray(t.reshape(128, 40))


def prep_inputs(inp):
    f = lambda a: np.ascontiguousarray(np.asarray(a, dtype=np.float32))
    x = f(inp["x"])
    c = f(inp["c"])
    b_ada = f(inp["b_ada"])[0]
    fm = lambda v: np.ascontiguousarray(v.reshape(-1, 128).T)
    bc = lambda v: np.broadcast_to(v[None, :], (128, v.shape[0]))
    vec_bc = np.ascontiguousarray(np.stack([
        bc(b_ada[2 * D:3 * D]), bc(b_ada[3 * D:4 * D]), bc(b_ada[4 * D:5 * D]), bc(b_ada[5 * D:6 * D]),
        bc(f(inp["g_post_mix"])[0]), bc(f(inp["g_pre_ffn"])[0]), bc(f(inp["g_post_ffn"])[0])]))
    shared = {
        "w_ada": f(inp["w_ada"])[0],
        "b_adaT": fm(b_ada[:2 * D]),
        "vec_bc": vec_bc,
        "gpreT": fm(f(inp["g_pre_mix"])[0]),
        "w_in": f(inp["w_in"])[0],
        "w_out": f(inp["w_out"])[0],
        "bgluT": fm(f(inp["b_glu"])[0]),
        "cwT": np.ascontiguousarray(f(inp["conv_w"])[0][:, 0, :].reshape(31, 16, 128).transpose(2, 1, 0).reshape(128, 16 * 31)),
        "cvec": np.ascontiguousarray(np.concatenate([fm(f(inp["conv_b"])[0]), fm(f(inp["conv_ln_g"])[0]),
                                                     fm(f(inp["conv_ln_b"])[0])], 1)),
        "consts": _consts(),
        "w_router": f(inp["w_router"])[0],
        "brt": np.ascontiguousarray(bc(f(inp["b_router"])[0])),
        "iota": np.ascontiguousarray(np.broadcast_to(np.arange(CAP, dtype=np.float32)[None, :], (128, CAP))),
        "tid": _tid(),
        "w_gu": f(inp["w_gate_up"])[0],
        "bguT": np.ascontiguousarray(f(inp["b_gate_up"])[0].reshape(32, 24, 128).transpose(0, 2, 1)),
        "w_dn": f(inp["w_down"])[0],
        "b_dn": f(inp["b_down"])[0],
    }
    zeros = np.zeros((NTOK, D), np.float32)
    maps = []
    for r in range(8):
        b, half = r // 2, r % 2
        m = dict(shared)
        m["xo"] = np.ascontiguousarray(x[b, half * NTOK:(half + 1) * NTOK])
        m["xp"] = np.ascontiguousarray(x[b, 0:NTOK]) if half == 1 else zeros
        m["flag"] = np.full((128, 1), float(half), np.float32)
        m["cT"] = fm(c[b])
        maps.append(m)
    return maps


def kernel(**inputs):
    nc, _ = build()
    maps = prep_inputs(inputs)
    res = run_bass_kernel_spmd(nc, maps, core_ids=list(range(8)))
    out = np.empty((4, 2048, D), np.float32)
    for r in range(8):
        b, half = r // 2, r % 2
        out[b, half * NTOK:(half + 1) * NTOK] = res.results[r]["out"]
    return out
```
